# Optimizing a Trainium2 kernel written in Bass

```python
import jax, jax.numpy as jnp
from jax import lax
import numpy as np

D_MODEL = 2048
BATCH = 2
SEQ = 4096
DEPTH = 1

MIX_WIDTH = D_MODEL
MOBA_WIDTH = D_MODEL // 2
MOBA_HEADS = 8
MOBA_HEAD_DIM = MOBA_WIDTH // MOBA_HEADS
MOBA_BLOCK = 256
MOBA_TOPK = 3
MOBA_QCHUNK = 64
ROPE_THETA = 500000.0
ROPE_DIMS = MOBA_HEAD_DIM // 4
RET_WIDTH = MIX_WIDTH - MOBA_WIDTH
RET_HEADS = 4
RET_HEAD_DIM = RET_WIDTH // RET_HEADS
RET_CHUNK = 128
RET_ROPE_BASE = 10000.0
IN_SPLITS = [MOBA_WIDTH] * 3 + [RET_WIDTH] * 4
IN_PROJ_DIM = sum(IN_SPLITS)
N_EXPERTS = 64
TOP_K = 8
N_GROUPS = 8
TOPK_GROUPS = 4
EXPERT_DIM = D_MODEL // 4
SHARED_DIM = EXPERT_DIM
ROUTE_SCALE = 2.5
MOE_BLOCK = 128
NORM_EPS = 1e-6
NEG = -1e30

kernel_name = "hymba_moba_retnet_moe_adaln"


def rms_norm(x, g):
    xf = x.astype(jnp.float32)
    y = xf * lax.rsqrt(jnp.mean(xf * xf, axis=-1, keepdims=True) + NORM_EPS)
    return (y * g.astype(jnp.float32)).astype(x.dtype)


def modulate(h, shift, scale):
    return h * (1.0 + scale[:, None, :]) + shift[:, None, :]


def rotary(x, positions, rot_dims, inv_freq):
    half = rot_dims // 2
    ang = positions[:, None, :, None].astype(jnp.float32) * inv_freq
    cos, sin = jnp.cos(ang), jnp.sin(ang)
    xr = x[..., :rot_dims].astype(jnp.float32)
    x1, x2 = xr[..., :half], xr[..., half:]
    rot = jnp.concatenate([x1 * cos - x2 * sin, x2 * cos + x1 * sin], axis=-1)
    return jnp.concatenate([rot.astype(x.dtype), x[..., rot_dims:]], axis=-1)


def moba_attention(q, k, v):
    B, H, T, hd = q.shape
    nb = -(-T // MOBA_BLOCK)
    Tp = nb * MOBA_BLOCK
    pad = ((0, 0), (0, 0), (0, Tp - T), (0, 0))
    q, k, v = jnp.pad(q, pad), jnp.pad(k, pad), jnp.pad(v, pad)
    kb = k.reshape(B, H, nb, MOBA_BLOCK, hd)
    vb = v.reshape(B, H, nb, MOBA_BLOCK, hd)
    kmean = jnp.mean(kb.astype(jnp.float32), axis=3)
    gate = jnp.einsum('bhtd,bhnd->bhtn', q.astype(jnp.float32), kmean)
    qblk = jnp.arange(Tp) // MOBA_BLOCK
    past = jnp.arange(nb)[None, :] < qblk[:, None]
    gate = jnp.where(past, gate, NEG)
    ksel = min(MOBA_TOPK, max(nb - 1, 1))
    gval, gidx = lax.top_k(gate, ksel)
    gvalid = gval > 0.5 * NEG
    nq = Tp // MOBA_QCHUNK
    def chunked(t):
        return jnp.moveaxis(t.reshape(B, H, nq, MOBA_QCHUNK, *t.shape[3:]), 2, 0)
    q_c, idx_c, val_c = chunked(q), chunked(gidx), chunked(gvalid)
    scale = hd ** -0.5
    bi = jnp.arange(B)[:, None, None, None]
    hi = jnp.arange(H)[None, :, None, None]

    def attend(args):
        c, qc, idx, valid = args
        kg = kb[bi, hi, idx].astype(jnp.float32)
        vg = vb[bi, hi, idx].astype(jnp.float32)
        blk = (c * MOBA_QCHUNK) // MOBA_BLOCK
        ko = lax.dynamic_index_in_dim(kb, blk, axis=2, keepdims=False).astype(jnp.float32)
        vo = lax.dynamic_index_in_dim(vb, blk, axis=2, keepdims=False).astype(jnp.float32)
        qf = qc.astype(jnp.float32) * scale
        s_sel = jnp.einsum('bhqd,bhqskd->bhqsk', qf, kg)
        s_sel = jnp.where(valid[..., None], s_sel, NEG).reshape(B, H, MOBA_QCHUNK, ksel * MOBA_BLOCK)
        s_own = jnp.einsum('bhqd,bhkd->bhqk', qf, ko)
        qpos = c * MOBA_QCHUNK + jnp.arange(MOBA_QCHUNK)
        kpos = blk * MOBA_BLOCK + jnp.arange(MOBA_BLOCK)
        s_own = jnp.where(kpos[None, :] <= qpos[:, None], s_own, NEG)
        p = jax.nn.softmax(jnp.concatenate([s_sel, s_own], axis=-1), axis=-1)
        p_sel = p[..., :ksel * MOBA_BLOCK].reshape(B, H, MOBA_QCHUNK, ksel, MOBA_BLOCK)
        p_own = p[..., ksel * MOBA_BLOCK:]
        o = jnp.einsum('bhqsk,bhqskd->bhqd', p_sel, vg) + jnp.einsum('bhqk,bhkd->bhqd', p_own, vo)
        return o.astype(q.dtype)

    out = lax.map(attend, (jnp.arange(nq), q_c, idx_c, val_c))
    out = jnp.moveaxis(out, 0, 2).reshape(B, H, Tp, hd)
    return out[:, :, :T]


def retention(q, k, v):
    B, H, T, d = q.shape
    C = RET_CHUNK
    n = T // C
    q = q.astype(jnp.float32)
    k = k.astype(jnp.float32) * (d ** -0.5)
    v = v.astype(jnp.float32)
    gamma = 1.0 - jnp.exp2(-5.0 - jnp.arange(H, dtype=jnp.float32))
    log_g = jnp.log(gamma)
    pos = jnp.arange(C, dtype=jnp.float32)
    diff = pos[:, None] - pos[None, :]
    dmask = jnp.where(diff >= 0, jnp.exp(jnp.maximum(diff, 0.0) * log_g[:, None, None]), 0.0)
    xi = jnp.exp((pos + 1.0) * log_g[:, None])
    zeta = jnp.exp((C - 1.0 - pos) * log_g[:, None])
    chunk_decay = jnp.exp(C * log_g)
    def chunked(t):
        return jnp.moveaxis(t.reshape(B, H, n, C, d), 2, 0)

    def step(S, inp):
        qi, ki, vi = inp
        inner = jnp.einsum('bhnd,bhmd->bhnm', qi, ki) * dmask
        o = jnp.einsum('bhnm,bhme->bhne', inner, vi) + jnp.einsum('bhnd,bhde->bhne', qi, S) * xi[None, :, :, None]
        S = S * chunk_decay[None, :, None, None] + jnp.einsum('bhmd,bhme->bhde', ki * zeta[None, :, :, None], vi)
        return S, o

    S0 = jnp.zeros((B, H, d, d), jnp.float32)
    _, o = lax.scan(step, S0, (chunked(q), chunked(k), chunked(v)))
    return jnp.moveaxis(o, 0, 2).reshape(B, H, T, d)


def moe_ffn(h, w_router, router_bias, w_gate, w_up, w_down, ws_gate, ws_up, ws_down):
    N, D = h.shape
    E, K, M = N_EXPERTS, TOP_K, MOE_BLOCK
    scores = jax.nn.sigmoid(h.astype(jnp.float32) @ w_router.astype(jnp.float32))
    biased = scores + router_bias.astype(jnp.float32)
    grp = biased.reshape(N, N_GROUPS, E // N_GROUPS)
    grp_score = jnp.sum(lax.top_k(grp, 2)[0], axis=-1)
    _, gsel = lax.top_k(grp_score, TOPK_GROUPS)
    gmask = jnp.sum(jax.nn.one_hot(gsel, N_GROUPS, dtype=jnp.float32), axis=1) > 0
    emask = jnp.repeat(gmask, E // N_GROUPS, axis=1)
    _, eidx = lax.top_k(jnp.where(emask, biased, NEG), K)
    w = jnp.take_along_axis(scores, eidx, axis=1)
    w = w / jnp.sum(w, axis=-1, keepdims=True) * ROUTE_SCALE
    e_flat = eidx.reshape(-1)
    tok_flat = jnp.repeat(jnp.arange(N, dtype=jnp.int32), K)
    w_flat = w.reshape(-1)
    order = jnp.argsort(e_flat)
    e_s, tok_s, w_s = e_flat[order], tok_flat[order], w_flat[order]
    counts = jnp.bincount(e_flat, length=E)
    pcounts = (counts + M - 1) // M * M
    start = jnp.cumsum(counts) - counts
    pend = jnp.cumsum(pcounts)
    pstart = pend - pcounts
    dest = pstart[e_s] + jnp.arange(N * K) - start[e_s]
    R = N * K + E * M
    nblk = R // M
    row_tok = jnp.zeros((R,), jnp.int32).at[dest].set(tok_s)
    row_w = jnp.zeros((R,), jnp.float32).at[dest].set(w_s)
    blk_expert = jnp.clip(jnp.searchsorted(pend, jnp.arange(nblk) * M, side='right'), 0, E - 1)

    def expert_block(args):
        tok, wt, e = args
        xb = h[tok]
        a = xb @ w_gate[e]
        u = xb @ w_up[e]
        return ((jax.nn.silu(a) * u) @ w_down[e]) * wt[:, None].astype(h.dtype)

    y = lax.map(expert_block, (row_tok.reshape(nblk, M), row_w.reshape(nblk, M), blk_expert))
    routed = jax.ops.segment_sum(y.reshape(R, D), row_tok, num_segments=N)
    shared = (jax.nn.silu(h @ ws_gate) * (h @ ws_up)) @ ws_down
    return routed + shared


def setup_inputs(seed: int = 0) -> dict:
    key = jax.random.key(seed)
    ks = jax.random.split(key, 17)
    f32 = jnp.float32
    D = D_MODEL
    def nrm(k, shape, fan_in, s=1.0):
        return jax.random.normal(k, shape, f32) * (s * fan_in ** -0.5)
    x = jax.random.normal(ks[0], (BATCH, SEQ, D), f32)
    c = jax.random.normal(ks[1], (BATCH, D), f32)
    positions = jnp.broadcast_to(jnp.arange(SEQ, dtype=jnp.int32)[None, :], (BATCH, SEQ))
    w_ada = nrm(ks[2], (DEPTH, D, 6 * D), D, 0.5)
    b_ada = 0.02 * jax.random.normal(ks[3], (DEPTH, 6 * D), f32)
    norm_mix = 1.0 + 0.02 * jax.random.normal(ks[4], (DEPTH, D), f32)
    norm_ffn = 1.0 + 0.02 * jax.random.normal(ks[5], (DEPTH, D), f32)
    norm_out = 1.0 + 0.02 * jax.random.normal(ks[6], (D,), f32)
    w_in = nrm(ks[7], (DEPTH, D, IN_PROJ_DIM), D)
    w_out = nrm(ks[8], (DEPTH, MIX_WIDTH, D), MIX_WIDTH)
    w_router = nrm(ks[9], (DEPTH, D, N_EXPERTS), D)
    router_bias = 0.01 * jax.random.normal(ks[10], (DEPTH, N_EXPERTS), f32)
    w_gate = nrm(ks[11], (DEPTH, N_EXPERTS, D, EXPERT_DIM), D)
    w_up = nrm(ks[12], (DEPTH, N_EXPERTS, D, EXPERT_DIM), D)
    w_down = nrm(ks[13], (DEPTH, N_EXPERTS, EXPERT_DIM, D), EXPERT_DIM)
    w_sh_gate = nrm(ks[14], (DEPTH, D, SHARED_DIM), D)
    w_sh_up = nrm(ks[15], (DEPTH, D, SHARED_DIM), D)
    w_sh_down = nrm(ks[16], (DEPTH, SHARED_DIM, D), SHARED_DIM)
    return {"x": x, "c": c, "positions": positions, "w_ada": w_ada, "b_ada": b_ada,
            "norm_mix": norm_mix, "norm_ffn": norm_ffn, "norm_out": norm_out,
            "w_in": w_in, "w_out": w_out, "w_router": w_router, "router_bias": router_bias,
            "w_gate": w_gate, "w_up": w_up, "w_down": w_down,
            "w_sh_gate": w_sh_gate, "w_sh_up": w_sh_up, "w_sh_down": w_sh_down}


def reference(x, c, positions, w_ada, b_ada, norm_mix, norm_ffn, norm_out, w_in, w_out,
              w_router, router_bias, w_gate, w_up, w_down, w_sh_gate, w_sh_up, w_sh_down):
    B, T, D = x.shape
    moba_inv = ROPE_THETA ** (-(jnp.arange(ROPE_DIMS // 2, dtype=jnp.float32) * 2.0 / ROPE_DIMS))
    ret_inv = RET_ROPE_BASE ** (-jnp.linspace(0.0, 1.0, RET_HEAD_DIM // 2, dtype=jnp.float32))
    split_idx = [int(s) for s in np.cumsum(IN_SPLITS)[:-1]]
    def to_heads(t, H):
        return t.reshape(B, T, H, -1).transpose(0, 2, 1, 3)
    def from_heads(t):
        return t.transpose(0, 2, 1, 3).reshape(B, T, -1)
    for l in range(DEPTH):
        mod = jax.nn.silu(c) @ w_ada[l] + b_ada[l]
        shift_a, scale_a, gate_a, shift_f, scale_f, gate_f = jnp.split(mod, 6, axis=-1)
        h = modulate(rms_norm(x, norm_mix[l]), shift_a, scale_a)
        proj = h @ w_in[l]
        qa, ka, va, qr, kr, vr, gr = jnp.split(proj, split_idx, axis=-1)
        qa = rotary(to_heads(qa, MOBA_HEADS), positions, ROPE_DIMS, moba_inv)
        ka = rotary(to_heads(ka, MOBA_HEADS), positions, ROPE_DIMS, moba_inv)
        o_a = from_heads(moba_attention(qa, ka, to_heads(va, MOBA_HEADS)))
        qr = rotary(to_heads(qr, RET_HEADS), positions, RET_HEAD_DIM, ret_inv)
        kr = rotary(to_heads(kr, RET_HEADS), positions, RET_HEAD_DIM, ret_inv)
        o_r = retention(qr, kr, to_heads(vr, RET_HEADS))
        mu = jnp.mean(o_r, axis=-1, keepdims=True)
        var = jnp.mean(jnp.square(o_r - mu), axis=-1, keepdims=True)
        o_r = ((o_r - mu) * lax.rsqrt(var + NORM_EPS)).astype(x.dtype)
        o_r = from_heads(o_r) * jax.nn.silu(gr)
        mix = jnp.concatenate([o_a, o_r], axis=-1) @ w_out[l]
        x = x + gate_a[:, None, :] * mix
        h = modulate(rms_norm(x, norm_ffn[l]), shift_f, scale_f)
        y = moe_ffn(h.reshape(B * T, D), w_router[l], router_bias[l], w_gate[l], w_up[l], w_down[l],
                    w_sh_gate[l], w_sh_up[l], w_sh_down[l]).reshape(B, T, D)
        x = x + gate_f[:, None, :] * y
    return rms_norm(x, norm_out)
```

```python
import contextlib
import os
import numpy as np
import concourse.bass as bass
import concourse.mybir as mybir
from concourse.bass_utils import run_bass_kernel_spmd

F32 = mybir.dt.float32
BF16 = mybir.dt.bfloat16
AF = mybir.ActivationFunctionType
ALU = mybir.AluOpType
AX = mybir.AxisListType

D = 2048
NKC = 16
NE = 64
EPS = 1e-6
NT = 8
NSLOT_T = 32


class Buf:
    __slots__ = ("name", "w", "r")

    def __init__(self, name):
        self.name = name
        self.w = None
        self.r = []


class Trk:
    def __init__(self, nc, n_dma_sems=28, n_fixed=8):
        self.nc = nc
        self.sems = {}
        self.seen = {}
        self.engines = {"pe": nc.tensor, "act": nc.scalar, "dve": nc.vector, "pool": nc.gpsimd, "sp": nc.sync}
        self._ctx = []
        for k in ("pe", "act", "dve", "pool"):
            self._mk(k)
        self.n_dma = 0
        self.n_dma_pool = 0
        self.n_fixed = n_fixed
        self.n_dma_sems = n_dma_sems
        for i in range(n_dma_sems):
            self._mk(("dma", i))

    def _mk(self, key):
        nm = "s_" + "".join(ch for ch in str(key) if ch.isalnum())
        cm = self.nc.semaphore(nm)
        h = cm.__enter__()
        self._ctx.append(cm)
        self.sems[key] = [h, 0]
        for e in self.engines:
            self.seen.setdefault(e, {})[key] = 0

    def close(self):
        for cm in reversed(self._ctx):
            cm.__exit__(None, None, None)

    def _wait(self, ename, dep):
        if dep is None:
            return
        key, val = dep
        if self.seen[ename].get(key, 0) >= val:
            return
        self.engines[ename].wait_ge(self.sems[key][0], val)
        self.seen[ename][key] = val

    def _deps(self, ename, reads, writes):
        for b in reads:
            if b.w is not None and not (ename == "pe" and b.w[0] == "pe"):
                self._wait(ename, b.w)
        for b in writes:
            if b.w is not None and not (ename == "pe" and b.w[0] == "pe"):
                self._wait(ename, b.w)
            for d in b.r:
                if not (ename == "pe" and d[0] == "pe"):
                    self._wait(ename, d)

    def _record(self, key, dep, reads, writes):
        for b in writes:
            b.w = dep
            b.r = []
        for b in reads:
            b.r = [d for d in b.r if d[0] != key] + [dep]

    def op(self, ename, fn, reads=(), writes=(), inc=True):
        self._deps(ename, reads, writes)
        ins = fn(self.engines[ename])
        s = self.sems[ename]
        if inc:
            s[1] += 1
            ins.then_inc(s[0], 1)
            val = s[1]
        else:
            val = s[1] + 1
        self._record(ename, (ename, val), reads, writes)
        return ins

    def dma(self, qname, out, in_, reads=(), writes=(), sem=None):
        if sem is None:
            if qname == "pool":
                sem = 4 + self.n_dma_pool % 4
                self.n_dma_pool += 1
            else:
                sem = self.n_fixed + self.n_dma % (self.n_dma_sems - self.n_fixed)
                self.n_dma += 1
        key = ("dma", sem)
        s = self.sems[key]
        if s[1] > 0:
            self._wait(qname, (key, s[1]))
        self._deps(qname, reads, writes)
        ins = self.engines[qname].dma_start(out=out, in_=in_)
        s[1] += 16
        ins.then_inc(s[0], 16)
        self._record(key, (key, s[1]), reads, writes)
        return ins

    def drain(self, ename, bufs):
        for b in bufs:
            self._wait(ename, b.w)

    def barrier(self):
        for e in self.engines:
            for key, s in self.sems.items():
                if s[1] > 0 and key != e:
                    self._wait(e, (key, s[1]))


class Prog:
    def __init__(self, mode="full", ne=NE):
        self.mode = mode
        self.ne = ne
        self.stop = os.environ.get("K_STOP", "")
        self.nc = bass.Bass("TRN2", target_bir_lowering=False)
        self.es = contextlib.ExitStack()
        self.ins = {}

    def din(self, name, shape, dt=F32):
        ap = self.nc.dram_tensor(name, list(shape), dt, kind="ExternalInput").ap()
        self.ins[name] = ap
        return ap

    def sb(self, name, shape, dt, es=None):
        return (es or self.es).enter_context(self.nc.sbuf_tensor("sb_" + name, list(shape), dt))

    def phase0_gen(self, t, ps, pb, ring, rb, es):
        I = self.ins
        ccol = self.sb("ccol", [128, NKC], F32, es)
        csil = self.sb("csil", [128, NKC], F32, es)
        crep = self.sb("crep", [128, NKC, 128], BF16, es)
        rowt = [self.sb(f"rowt{i}", [128, D], F32, es) for i in range(2)]
        nrow = self.sb("nrow", [128, D], F32, es)
        b_c, b_crep = Buf("ccol"), Buf("crep")
        b_rowt = [Buf("rowt0"), Buf("rowt1")]
        b_nrow = Buf("nrow")
        t.dma("sp", ccol[:], I["c_col"][:, :], writes=[b_c])
        t.op("act", lambda e: e.activation(out=csil[:], in_=ccol[:], func=AF.Silu), reads=[b_c], writes=[b_c])
        t.op("dve", lambda e: e.tensor_copy(out=crep[:], in_=csil[:].unsqueeze(2).broadcast_to([128, NKC, 128])),
             reads=[b_c], writes=[b_crep])

        def load(i):
            col0 = i * 512
            t.dma("pool", ring[i % 4][:].rearrange("p (k n) -> p k n", n=512),
                  I["w_ada"][:, col0:col0 + 512].rearrange("(k p) n -> p k n", p=128),
                  writes=[rb[i % 4]], sem=i % 4)
        for i in range(3):
            load(i)
        for v in range(6):
            rt = rowt[v % 2]
            t.dma("sp", rt[:], I["b_ada"][0:1, v * D:(v + 1) * D].partition_broadcast(128), writes=[b_rowt[v % 2]])
            if v in (1, 4):
                src = I["norm_mix"] if v == 1 else I["norm_ffn"]
                t.dma("sp", nrow[:], src[0:1, :].partition_broadcast(128), writes=[b_nrow])
            for cg in range(4):
                i = v * 4 + cg
                if i + 3 < 24:
                    load(i + 3)
                slot = i % 4
                bank = 6 + cg % 2
                for k in range(NKC):
                    t.op("pe", lambda e, k=k: e.matmul(ps[bank][:], crep[:, k, :], ring[slot][:, k * 512:(k + 1) * 512],
                                                       start=(k == 0), stop=(k == NKC - 1)),
                         reads=[b_crep, rb[slot]], writes=[pb[bank]], inc=(k == NKC - 1))
                t.op("dve", lambda e: e.tensor_tensor(out=rt[:, cg * 512:(cg + 1) * 512], in0=ps[bank][:],
                                                      in1=rt[:, cg * 512:(cg + 1) * 512], op=ALU.add),
                     reads=[pb[bank], b_rowt[v % 2]], writes=[b_rowt[v % 2]])
                if cg == 3:
                    if v in (1, 4):
                        t.op("dve", lambda e: e.scalar_tensor_tensor(out=rt[:], in0=rt[:], scalar=1.0, in1=nrow[:],
                                                                     op0=ALU.add, op1=ALU.mult),
                             reads=[b_rowt[v % 2], b_nrow], writes=[b_rowt[v % 2]])
                    t.dma("sp", self.modrows[v:v + 1, :], rt[0:1, :], reads=[b_rowt[v % 2]], writes=[self.b_modrows[v]])
                yield (v, cg)

    def routing(self, t, R, lg_ap, bl, rbrow, b_rb, wn_out, bw):
        sc, bi, m1, eq, g2, m2, t8, gm, bm, e8, ws = (R[k] for k in ("sc", "bi", "m1", "eq", "g2", "m2", "t8", "gm", "bm", "e8", "ws"))
        B = R["B"]

        def v3(x):
            return x[:].rearrange("p (g k) -> p g k", k=8)

        def bc(x):
            return x[:].unsqueeze(2).broadcast_to([128, 8, 8])
        t.op("act", lambda e: e.activation(out=sc[:], in_=lg_ap, func=AF.Sigmoid), reads=[bl], writes=[B["sc"]])
        t.op("dve", lambda e: e.tensor_tensor(out=bi[:], in0=sc[:], in1=rbrow[:], op=ALU.add), reads=[B["sc"], b_rb], writes=[B["bi"]])
        t.op("dve", lambda e: e.tensor_reduce(out=m1[:], in_=v3(bi), axis=AX.X, op=ALU.max), reads=[B["bi"]], writes=[B["m1"]])
        t.op("dve", lambda e: e.tensor_tensor(out=v3(eq), in0=v3(bi), in1=bc(m1), op=ALU.is_equal), reads=[B["bi"], B["m1"]], writes=[B["eq"]])
        t.op("dve", lambda e: e.scalar_tensor_tensor(out=g2[:], in0=eq[:], scalar=-1e30, in1=bi[:], op0=ALU.mult, op1=ALU.add),
             reads=[B["eq"], B["bi"]], writes=[B["g2"]])
        t.op("dve", lambda e: e.tensor_reduce(out=m2[:], in_=v3(g2), axis=AX.X, op=ALU.max), reads=[B["g2"]], writes=[B["m2"]])
        t.op("dve", lambda e: e.tensor_tensor(out=m2[:], in0=m2[:], in1=m1[:], op=ALU.add), reads=[B["m2"], B["m1"]], writes=[B["m2"]])
        t.op("dve", lambda e: e.max(out=t8[:], in_=m2[:]), reads=[B["m2"]], writes=[B["t8"]])
        t.op("dve", lambda e: e.tensor_scalar(out=gm[:], in0=m2[:], scalar1=t8[:, 3:4], scalar2=None, op0=ALU.is_ge),
             reads=[B["m2"], B["t8"]], writes=[B["gm"]])
        t.op("dve", lambda e: e.tensor_scalar(out=t8[:], in0=gm[:], scalar1=-1.0, scalar2=1e30, op0=ALU.add, op1=ALU.mult),
             reads=[B["gm"]], writes=[B["t8"]])
        t.op("dve", lambda e: e.tensor_tensor(out=v3(bm), in0=v3(bi), in1=bc(gm), op=ALU.mult), reads=[B["bi"], B["gm"]], writes=[B["bm"]])
        t.op("dve", lambda e: e.tensor_tensor(out=v3(bm), in0=v3(bm), in1=bc(t8), op=ALU.add), reads=[B["bm"], B["t8"]], writes=[B["bm"]])
        t.op("dve", lambda e: e.max(out=e8[:], in_=bm[:]), reads=[B["bm"]], writes=[B["e8"]])
        t.op("dve", lambda e: e.tensor_scalar(out=eq[:], in0=bm[:], scalar1=e8[:, 7:8], scalar2=None, op0=ALU.is_ge),
             reads=[B["bm"], B["e8"]], writes=[B["eq"]])
        t.op("dve", lambda e: e.tensor_tensor(out=g2[:], in0=eq[:], in1=sc[:], op=ALU.mult), reads=[B["eq"], B["sc"]], writes=[B["g2"]])
        t.op("dve", lambda e: e.tensor_reduce(out=ws[:], in_=g2[:], axis=AX.X, op=ALU.add), reads=[B["g2"]], writes=[B["ws"]])
        t.op("dve", lambda e: e.reciprocal(out=ws[:], in_=ws[:]), reads=[B["ws"]], writes=[B["ws"]])
        t.op("dve", lambda e: e.tensor_scalar(out=wn_out, in0=g2[:], scalar1=ws[:, 0:1], scalar2=2.5, op0=ALU.mult, op1=ALU.mult),
             reads=[B["g2"], B["ws"]], writes=[bw])

    def phaseB(self, t, ps, pb, ring, rb, ident32, b_id):
        nc = self.nc
        I = self.ins
        ne = self.ne
        with contextlib.ExitStack() as es:
            h2T = self.sb("h2T", [128, NKC, NT * 128], BF16, es)
            wn = self.sb("wn", [128, NT, 64], F32, es)
            ones1 = self.sb("ones1", [128, 1], F32, es)
            b_y = [[Buf(f"y{i}_{c}") for c in range(4)] for i in range(NT)]
            b_h2T = [Buf(f"h2T{i}") for i in range(NT)]
            b_wn = [Buf(f"wn{i}") for i in range(NT)]
            b_act = [[Buf(f"act{h}_{f}") for f in range(4)] for h in range(2)]
            b_sil = [Buf("sil0"), Buf("sil1")]
            b_one = Buf("ones1")
            t.op("dve", lambda e: e.memset(ones1[:], 1.0), writes=[b_one])
            mats = []
            for e_ in range(ne):
                mats += [("g", I["w_gate"][e_]), ("g", I["w_up"][e_]), ("d", I["w_down"][e_])]
            mats += [("g", I["w_sh_gate"]), ("g", I["w_sh_up"]), ("d", I["w_sh_down"])]
            state = {"issued": 0, "consumed": 0}

            def pump():
                while state["issued"] < len(mats) and state["issued"] - 4 < state["consumed"]:
                    i = state["issued"]
                    kind, src = mats[i]
                    slot = i % 4
                    if kind == "g":
                        t.dma("pool", ring[slot][:].rearrange("p (k n) -> p k n", n=512),
                              src.rearrange("(k p) n -> p k n", p=128), writes=[rb[slot]], sem=slot)
                    else:
                        t.dma("pool", ring[slot][:].rearrange("p (k n) -> p k n", n=D),
                              src.rearrange("(k p) n -> p k n", p=128), writes=[rb[slot]], sem=slot)
                    state["issued"] += 1
            pump()
            with contextlib.ExitStack() as es1:
                g2row = self.sb("g2row", [128, D], F32, es1)
                shfrow = self.sb("shfrow", [128, D], F32, es1)
                xt = [self.sb(f"xtB{i}", [128, D], F32, es1) for i in range(2)]
                hf = self.sb("hfB", [128, D], F32, es1)
                sq = self.sb("sqB", [128, D], BF16, es1)
                hT32 = self.sb("hT32", [128, NKC, 128], F32, es1)
                wr32 = self.sb("wr32", [128, NKC, 64], F32, es1)
                rbrow = self.sb("rbrow", [128, 64], F32, es1)
                ss = self.sb("ssB", [128, 2], F32, es1)
                R = {k: self.sb("rt_" + k, [128, n], F32, es1) for k, n in
                     (("sc", 64), ("bi", 64), ("m1", 8), ("eq", 64), ("g2", 64), ("m2", 8), ("t8", 8), ("gm", 8),
                      ("bm", 64), ("e8", 8), ("ws", 1))}
                R["B"] = {k: Buf("rt_" + k) for k in ("sc", "bi", "m1", "eq", "g2", "m2", "t8", "gm", "bm", "e8", "ws")}
                b_g2, b_shf, b_wr, b_rbr = Buf("g2row"), Buf("shfrow"), Buf("wr32"), Buf("rbrow")
                b_xt = [Buf("xt0"), Buf("xt1")]
                b_hf, b_sq, b_hT32, b_ss = Buf("hf"), Buf("sq"), Buf("hT32"), Buf("ss")
                t.dma("sp", g2row[:], self.modrows[4:5, :].partition_broadcast(128), reads=[self.b_modrows[4]], writes=[b_g2])
                t.dma("sp", shfrow[:], self.modrows[3:4, :].partition_broadcast(128), reads=[self.b_modrows[3]], writes=[b_shf])
                t.dma("sp", wr32[:], I["w_router"].rearrange("(k p) n -> p k n", p=128), writes=[b_wr])
                t.dma("sp", rbrow[:], I["router_bias"][0:1, :].partition_broadcast(128), writes=[b_rbr])
                hfs = [hf, self.sb("hfB1", [128, D], F32, es1)]
                b_hfs = [b_hf, Buf("hf1")]
                ss4 = self.sb("ssB4", [128, 4], F32, es1)
                b_ss4 = [Buf("ssB40"), Buf("ssB41")]

                def normB(i):
                    if i >= NT:
                        return
                    x_, bx = xt[i % 2], b_xt[i % 2]
                    hf_, bhf = hfs[i % 2], b_hfs[i % 2]
                    sa, sb2, bss = ss4[:, 2 * (i % 2):2 * (i % 2) + 1], ss4[:, 2 * (i % 2) + 1:2 * (i % 2) + 2], b_ss4[i % 2]
                    t.dma("sp", x_[:], self.x1s[i * 128:(i + 1) * 128, :], reads=[self.b_x1s], writes=[bx])
                    t.op("act", lambda e: e.activation(out=sq[:], in_=x_[:], func=AF.Square, accum_out=sa), reads=[bx], writes=[b_sq, bss])
                    t.op("act", lambda e: e.activation(out=sb2, in_=sa, func=AF.Sqrt, bias=EPS, scale=1.0 / D), reads=[bss], writes=[bss])
                    t.op("dve", lambda e: e.reciprocal(out=sb2, in_=sb2), reads=[bss], writes=[bss])
                    t.op("dve", lambda e: e.scalar_tensor_tensor(out=hf_[:], in0=x_[:], scalar=sb2, in1=g2row[:], op0=ALU.mult, op1=ALU.mult),
                         reads=[bx, bss, b_g2], writes=[bhf])
                    t.op("pool", lambda e: e.tensor_tensor(out=hf_[:], in0=hf_[:], in1=shfrow[:], op=ALU.add), reads=[bhf, b_shf], writes=[bhf])
                normB(0)
                for i in range(NT):
                    hf, b_hf = hfs[i % 2], b_hfs[i % 2]
                    lvl = int(os.environ.get("K_B1", "3"))
                    if lvl < 2:
                        t.dma("sp", self.out[i * 128:(i + 1) * 128, :], hf[:], reads=[b_hf], writes=[Buf("o")])
                        continue
                    for j in range(4):
                        for r in range(4):
                            c = 4 * j + r
                            t.op("pe", lambda e, c=c, r=r: e.matmul(ps[j][:, r * 128:(r + 1) * 128],
                                                                    hf[:, c * 128:(c + 1) * 128], ident32[:],
                                                                    start=True, stop=True),
                                 reads=[b_hf, b_id], writes=[pb[j]], inc=(r == 3))
                        skip = os.environ.get("K_SKIP", "")
                        if "dve" not in skip:
                          t.op("dve", lambda e: e.tensor_copy(out=hT32[:, 4 * j:4 * j + 4, :],
                                                            in_=ps[j][:].rearrange("p (r n) -> p r n", n=128)),
                             reads=[pb[j]], writes=[b_hT32])
                        if "act" not in skip:
                          t.op("act", lambda e: e.activation(out=h2T[:, 4 * j:4 * j + 4, i * 128:(i + 1) * 128],
                                                           in_=hT32[:, 4 * j:4 * j + 4, :], func=AF.Copy),
                             reads=[b_hT32], writes=[b_h2T[i]])
                    if lvl < 3:
                        t.dma("sp", self.out[i * 128:(i + 1) * 128, :], hT32[:].rearrange("p k n -> p (k n)"), reads=[b_hT32], writes=[Buf("o")])
                        continue
                    for c in range(NKC):
                        t.op("pe", lambda e, c=c: e.matmul(ps[4][:, 0:64], hT32[:, c, :], wr32[:, c, :],
                                                           start=(c == 0), stop=(c == NKC - 1)),
                             reads=[b_hT32, b_wr], writes=[pb[4]], inc=(c == NKC - 1))
                    normB(i + 1)
                    self.routing(t, R, ps[4][:, 0:64], pb[4], rbrow, b_rbr, wn[:, i, :], b_wn[i])
            t.barrier()
            if self.stop == "B1":
                bo = Buf("o")
                for i in range(NT if lvl == 3 else 0):
                    t.dma("sp", self.out[i * 128:(i + 1) * 128, 0:64], wn[:, i, :], reads=[b_wn[i]], writes=[bo])
                t.barrier()
                return
            yacc = self.sb("yacc", [128, NT, D], F32, es)
            act = self.sb("actT", [128, 4, NT * 128], BF16, es)
            sil = [self.sb(f"sil{i}", [128, 512], BF16, es) for i in range(2)]
            nyb = 0
            for e_ in range(ne + 1):
                sg, su, sd = (3 * e_) % 4, (3 * e_ + 1) % 4, (3 * e_ + 2) % 4
                wg, wu, wd = ring[sg], ring[su], ring[sd]
                nau = 0
                for half in range(2):
                    hbufs = b_h2T[4 * half:4 * half + 4]
                    for fc in range(4):
                        pa, pu = (nau % 2) * 2, (nau % 2) * 2 + 1
                        nau += 1
                        for w_, slot_, bank in ((wg, sg, pa), (wu, su, pu)):
                            for k in range(NKC):
                                t.op("pe", lambda e, k=k, w_=w_, bank=bank: e.matmul(
                                    ps[bank][:], w_[:, k * 512 + fc * 128:k * 512 + (fc + 1) * 128],
                                    h2T[:, k, half * 512:(half + 1) * 512], start=(k == 0), stop=(k == NKC - 1)),
                                    reads=[rb[slot_]] + hbufs, writes=[pb[bank]], inc=(k == NKC - 1))
                        sl, bsl = sil[nau % 2], b_sil[nau % 2]
                        t.op("act", lambda e: e.activation(out=sl[:], in_=ps[pa][:], func=AF.Silu), reads=[pb[pa]], writes=[bsl])
                        t.op("dve", lambda e: e.tensor_tensor(out=act[:, fc, half * 512:(half + 1) * 512], in0=sl[:],
                                                              in1=ps[pu][:], op=ALU.mult),
                             reads=[bsl, pb[pu]], writes=[b_act[half][fc]])
                state["consumed"] += 2
                pump()
                for i in range(NT):
                    for cg in range(4):
                        bank = 4 + nyb % 4
                        nyb += 1
                        for fc in range(4):
                            t.op("pe", lambda e, fc=fc: e.matmul(ps[bank][:], act[:, fc, i * 128:(i + 1) * 128],
                                                                 wd[:, fc * D + cg * 512:fc * D + (cg + 1) * 512],
                                                                 start=(fc == 0), stop=(fc == 3)),
                                 reads=[rb[sd], b_act[i // 4][fc]], writes=[pb[bank]], inc=(fc == 3))
                        ysl = yacc[:, i, cg * 512:(cg + 1) * 512]
                        wcol = wn[:, i, e_:e_ + 1] if e_ < ne else ones1[:, 0:1]
                        wbuf = b_wn[i] if e_ < ne else b_one
                        if e_ == 0:
                            t.op("dve", lambda e: e.tensor_scalar(out=ysl, in0=ps[bank][:], scalar1=wcol, scalar2=None, op0=ALU.mult),
                                 reads=[pb[bank], wbuf], writes=[b_y[i][cg]])
                        else:
                            t.op("dve", lambda e: e.scalar_tensor_tensor(out=ysl, in0=ps[bank][:], scalar=wcol, in1=ysl,
                                                                         op0=ALU.mult, op1=ALU.add),
                                 reads=[pb[bank], wbuf, b_y[i][cg]], writes=[b_y[i][cg]])
                state["consumed"] += 1
                pump()
            if self.stop == "B2":
                bo = Buf("o")
                for i in range(NT):
                    t.dma("sp", self.out[i * 128:(i + 1) * 128, :], yacc[:, i, :], reads=b_y[i], writes=[bo])
                t.barrier()
                return
            t.barrier()
            with contextlib.ExitStack() as es3:
                r0 = ring[0][:].bitcast(F32)
                r1 = ring[1][:].bitcast(F32)
                gfrow = r0[:, 0:D]
                norow = r0[:, D:2 * D]
                xt = [r1[:, 0:D], r1[:, D:2 * D]]
                sq = ring[2][:, 0:D]
                ss = self.sb("ssC", [128, 2 * NT], F32, es3)
                b_gf, b_no = Buf("gfrow"), Buf("norow")
                b_xt = [Buf("xtC0"), Buf("xtC1")]
                b_sq = Buf("sqC")
                b_ss = [Buf(f"ssC{i}") for i in range(NT)]
                t.dma("sp", gfrow[:], self.modrows[5:6, :].partition_broadcast(128), reads=[self.b_modrows[5]], writes=[b_gf])
                t.dma("sp", norow[:], I["norm_out"][0:1, :].partition_broadcast(128), writes=[b_no])
                outs = []

                def load_x1(i):
                    if i < NT:
                        t.dma("sp", xt[i % 2][:], self.x1s[i * 128:(i + 1) * 128, :], reads=[self.b_x1s], writes=[b_xt[i % 2]])
                load_x1(0)
                load_x1(1)
                for i in range(NT):
                    x_, bx = xt[i % 2], b_xt[i % 2]
                    yb = b_y[i]
                    t.op("pool", lambda e: e.tensor_tensor(out=yacc[:, i, :], in0=yacc[:, i, :], in1=gfrow[:], op=ALU.mult),
                         reads=yb + [b_gf], writes=yb)
                    t.op("dve", lambda e: e.tensor_tensor(out=yacc[:, i, :], in0=yacc[:, i, :], in1=x_[:], op=ALU.add),
                         reads=yb + [bx], writes=yb)
                    t.op("act", lambda e: e.activation(out=sq[:], in_=yacc[:, i, :], func=AF.Square, accum_out=ss[:, 2 * i:2 * i + 1]),
                         reads=yb, writes=[b_sq, b_ss[i]])
                    t.op("act", lambda e: e.activation(out=ss[:, 2 * i + 1:2 * i + 2], in_=ss[:, 2 * i:2 * i + 1], func=AF.Sqrt,
                                                       bias=EPS, scale=1.0 / D), reads=[b_ss[i]], writes=[b_ss[i]])
                    t.op("dve", lambda e: e.reciprocal(out=ss[:, 2 * i + 1:2 * i + 2], in_=ss[:, 2 * i + 1:2 * i + 2]),
                         reads=[b_ss[i]], writes=[b_ss[i]])
                    t.op("dve", lambda e: e.scalar_tensor_tensor(out=yacc[:, i, :], in0=yacc[:, i, :], scalar=ss[:, 2 * i + 1:2 * i + 2],
                                                                 in1=norow[:], op0=ALU.mult, op1=ALU.mult),
                         reads=yb + [b_ss[i], b_no], writes=yb)
                    bo = Buf(f"out{i}")
                    t.dma("sp", self.out[i * 128:(i + 1) * 128, :], yacc[:, i, :], reads=yb, writes=[bo])
                    outs.append(bo)
                    load_x1(i + 2)
                t.drain("sp", outs)
                t.barrier()

    def phaseA(self, t, ps, pb, ring, rb, ident32, b_id):
        nc = self.nc
        I = self.ins
        SCALE = 128.0 ** -0.5
        PI = float(np.pi)
        psb = [p_[:].bitcast(BF16) for p_ in ps]
        hTs = nc.dram_tensor("hTs", [NSLOT_T, 128, D], BF16, kind="Internal").ap()
        b_hTs = [Buf(f"hTs{i}") for i in range(NSLOT_T)]
        hTt = [ring[3][:, q * D:(q + 1) * D] for q in range(4)]
        b_hTt = [Buf(f"hTt{q}") for q in range(4)]

        def load_hT(tile):
            if 0 <= tile < NSLOT_T:
                t.dma("sp", hTt[tile % 4], hTs[tile], reads=[b_hTs[tile]], writes=[b_hTt[tile % 4]])

        def project(tile, wslot, bslot, ncols, bank):
            for c in range(NKC):
                t.op("pe", lambda e, c=c: e.matmul(ps[bank][:, 0:ncols], hTt[tile % 4][:, c * 128:(c + 1) * 128],
                                                   wslot[:, c * 512:c * 512 + ncols], start=(c == 0), stop=(c == NKC - 1)),
                     reads=[b_hTt[tile % 4], bslot], writes=[pb[bank]], inc=(c == NKC - 1))

        def load_w(slot, col0, ncols=512):
            t.dma("pool", ring[slot][:].rearrange("p (k n) -> p k n", n=512)[:, :, 0:ncols],
                  I["w_in"][:, col0:col0 + ncols].rearrange("(k p) n -> p k n", p=128), writes=[rb[slot]], sem=slot)

        with contextlib.ExitStack() as es:
            OT = self.sb("OT", [128, NKC, NT * 128], BF16, es)
            b_OT = [Buf(f"OT{i}") for i in range(NT)]
            identb = self.sb("identb", [128, 128], BF16, es)
            onesb = self.sb("onesb", [128, 128], BF16, es)
            posi = self.sb("posi", [128, NSLOT_T], mybir.dt.int32, es)
            posf = self.sb("posf", [128, NSLOT_T], F32, es)
            vmask = self.sb("vmask", [128, NSLOT_T], F32, es)
            b_idb, b_ones, b_pos, b_vm = (Buf(n) for n in ("identb", "onesb", "pos", "vmask"))
            t.op("dve", lambda e: e.tensor_copy(out=identb[:], in_=ident32[:]), reads=[b_id], writes=[b_idb])
            t.op("dve", lambda e: e.memset(onesb[:], 1.0), writes=[b_ones])
            t.dma("sp", posi[:], I["pos_i"][:, :], writes=[b_pos])
            t.op("dve", lambda e: e.tensor_copy(out=posf[:], in_=posi[:]), reads=[b_pos], writes=[b_pos])
            t.dma("sp", vmask[:], I["vmask"][:, :], writes=[b_vm])

            with contextlib.ExitStack() as es0:
                p0 = self.phase0_gen(t, ps, pb, ring, rb, es0)
                for _ in range(8):
                    next(p0)
                g1row = self.sb("g1row", [128, D], F32, es0)
                sharow = self.sb("sharow", [128, D], F32, es0)
                xt = [self.sb(f"xtA{i}", [128, D], F32, es0) for i in range(3)]
                hb = [self.sb(f"hbA{i}", [128, D], BF16, es0) for i in range(2)]
                hst = [self.sb(f"hstA{i}", [128, D], BF16, es0) for i in range(2)]
                sq = self.sb("sqA", [128, D], BF16, es0)
                ssA = self.sb("ssA", [128, 4], F32, es0)
                b_g1, b_sha, b_sq = Buf("g1row"), Buf("sharow"), Buf("sq")
                b_xt = [Buf("xtA0"), Buf("xtA1"), Buf("xtA2")]
                b_hb = [Buf("hb0"), Buf("hb1")]
                b_hst = [Buf("hst0"), Buf("hst1")]

                def load_x(tile):
                    if tile < NSLOT_T:
                        t.dma("sp", xt[tile % 3][:], I["xs"][tile * 128:(tile + 1) * 128, :], writes=[b_xt[tile % 3]])
                b_ss = [Buf("ssA0"), Buf("ssA1")]
                t.dma("sp", g1row[:], self.modrows[1:2, :].partition_broadcast(128), reads=[self.b_modrows[1]], writes=[b_g1])
                t.dma("sp", sharow[:], self.modrows[0:1, :].partition_broadcast(128), reads=[self.b_modrows[0]], writes=[b_sha])
                load_x(0)
                load_x(1)
                for tile in range(NSLOT_T):
                    if tile % 2 == 1:
                        next(p0, None)
                    k2 = tile % 2
                    x_, bx = xt[tile % 3], b_xt[tile % 3]
                    sa, sb2 = ssA[:, 2 * k2:2 * k2 + 1], ssA[:, 2 * k2 + 1:2 * k2 + 2]
                    t.op("act", lambda e: e.activation(out=sq[:], in_=x_[:], func=AF.Square, accum_out=sa), reads=[bx], writes=[b_sq, b_ss[k2]])
                    t.op("act", lambda e: e.activation(out=sb2, in_=sa, func=AF.Sqrt, bias=EPS, scale=1.0 / D), reads=[b_ss[k2]], writes=[b_ss[k2]])
                    t.op("dve", lambda e: e.reciprocal(out=sb2, in_=sb2), reads=[b_ss[k2]], writes=[b_ss[k2]])
                    t.op("dve", lambda e: e.scalar_tensor_tensor(out=x_[:], in0=x_[:], scalar=sb2, in1=g1row[:], op0=ALU.mult, op1=ALU.mult),
                         reads=[bx, b_ss[k2], b_g1], writes=[bx])
                    t.op("dve", lambda e: e.tensor_tensor(out=hb[k2][:], in0=x_[:], in1=sharow[:], op=ALU.add),
                         reads=[bx, b_sha], writes=[b_hb[k2]])
                    for j in range(2):
                        bank = 2 * k2 + j
                        for r in range(8):
                            c = 8 * j + r
                            t.op("pe", lambda e, c=c, r=r: e.transpose(psb[bank][:, r * 128:(r + 1) * 128], hb[k2][:, c * 128:(c + 1) * 128], identb[:]),
                                 reads=[b_hb[k2], b_idb], writes=[pb[bank]], inc=(r == 7))
                    t.op("act", lambda e: e.activation(out=hst[k2][:, 0:1024], in_=psb[2 * k2][:], func=AF.Copy), reads=[pb[2 * k2]], writes=[b_hst[k2]])
                    t.op("dve", lambda e: e.tensor_copy(out=hst[k2][:, 1024:2048], in_=psb[2 * k2 + 1][:]), reads=[pb[2 * k2 + 1]], writes=[b_hst[k2]])
                    load_x(tile + 2)
                    t.dma("sp", hTs[tile], hst[k2][:], reads=[b_hst[k2]], writes=[b_hTs[tile]])
                for _ in p0:
                    pass
                t.barrier()

            def sincos(ang, n, sin_out, cos_out, b_ang, b_out):
                kf = self._sc_tmp[:, 0:n]
                ki = self._sc_ki[:, 0:n]
                bt = self._b_sc
                t.op("dve", lambda e: e.tensor_scalar(out=cos_out, in0=ang, scalar1=0.5 * PI, scalar2=None, op0=ALU.add),
                     reads=[b_ang], writes=[b_out])
                for r_, br in ((cos_out, b_out), (ang, b_ang)):
                    t.op("dve", lambda e: e.tensor_scalar(out=kf, in0=r_, scalar1=1.0 / (2 * PI), scalar2=None, op0=ALU.mult), reads=[br], writes=[bt])
                    t.op("dve", lambda e: e.tensor_copy(out=ki, in_=kf), reads=[bt], writes=[bt])
                    t.op("dve", lambda e: e.tensor_copy(out=kf, in_=ki), reads=[bt], writes=[bt])
                    t.op("dve", lambda e: e.scalar_tensor_tensor(out=r_, in0=kf, scalar=-2 * PI, in1=r_, op0=ALU.mult, op1=ALU.add),
                         reads=[bt, br], writes=[br])
                    t.op("dve", lambda e: e.tensor_scalar(out=kf, in0=r_, scalar1=PI, scalar2=-2 * PI, op0=ALU.is_gt, op1=ALU.mult), reads=[br], writes=[bt])
                    t.op("dve", lambda e: e.tensor_tensor(out=r_, in0=r_, in1=kf, op=ALU.add), reads=[bt, br], writes=[br])
                    t.op("dve", lambda e: e.tensor_scalar(out=r_, in0=r_, scalar1=-PI, scalar2=PI, op0=ALU.max, op1=ALU.min), reads=[br], writes=[br])
                t.op("act", lambda e: e.activation(out=cos_out, in_=cos_out, func=AF.Sin), reads=[b_out], writes=[b_out])
                t.op("act", lambda e: e.activation(out=sin_out, in_=ang, func=AF.Sin), reads=[b_ang], writes=[b_out])

            self._sc_ki = self.sb("sc_ki", [128, 1024], mybir.dt.int32, es)
            self._sc_tmp = self.sb("sc_tmp", [128, 1024], F32, es)
            self._b_sc = Buf("sc_tmp")

            NH = 4
            with contextlib.ExitStack() as esm:
                cosA = self.sb("cosA", [128, NSLOT_T, 16], F32, esm)
                sinA = self.sb("sinA", [128, NSLOT_T, 16], F32, esm)
                angA = self.sb("angA", [128, NSLOT_T, 16], F32, esm)
                invfA = self.sb("invfA", [128, 16], F32, esm)
                KT = self.sb("KT", [128, NH, NSLOT_T * 128], BF16, esm)
                V = self.sb("Vm", [128, NSLOT_T, NH * 128], BF16, esm)
                QT = self.sb("QT", [128, NH, NT * 128], BF16, esm)
                TT = self.sb("TT", [16, NH, NT * 128], BF16, esm)
                kbs = [self.sb(f"kbm{i}", [128, NH, 128], BF16, esm) for i in range(2)]
                tm1 = self.sb("tm1", [128, NH, 16], F32, esm)
                tm2 = self.sb("tm2", [128, NH, 16], F32, esm)
                kmT = self.sb("kmT", [128, NH, 16], F32, esm)
                kmTb = self.sb("kmTb", [128, NH, 16], BF16, esm)
                gbias = self.sb("gbias", [128, 4, 16], F32, esm)
                diag = self.sb("diag", [128, 4, 16], F32, esm)
                oh16 = self.sb("oh16", [16, 16, 128], BF16, esm)
                causal = self.sb("causal", [128, 4, 512], BF16, esm)
                gm = self.sb("gmA", [128, NH, 16], F32, esm)
                t8 = self.sb("t8A", [128, NH, 8], F32, esm)
                thr = self.sb("thrA", [128, NH], F32, esm)
                sel = self.sb("selA", [128, NH, 16], F32, esm)
                selb = self.sb("selbA", [128, NH, 16], BF16, esm)
                Pb = [self.sb(f"Pb{i}", [128, 512], BF16, esm) for i in range(3)]
                rl = self._sc_tmp[:, 0:512]
                b_cs, b_ang, b_ifa = Buf("cosinA"), Buf("angA"), Buf("invfA")
                b_KT = [Buf(f"KT{i}") for i in range(NSLOT_T)]
                b_V = [Buf(f"V{i}") for i in range(NSLOT_T)]
                b_QT = [Buf(f"QT{i}") for i in range(NT)]
                b_TT = [Buf(f"TT{i}") for i in range(NT)]
                b_kbs = [Buf("kb0"), Buf("kb1")]
                b_tm1, b_tm2, b_km, b_kmb = (Buf(n) for n in ("tm1", "tm2", "kmT", "kmTb"))
                b_gb, b_dg, b_oh, b_ca, b_gm, b_t8, b_thr, b_sel, b_selb, b_rl = (
                    Buf(n) for n in ("gbias", "diag", "oh16", "causal", "gm", "t8", "thr", "sel", "selb", "rl"))
                b_P = [Buf(f"P{i}") for i in range(3)]
                t.dma("sp", invfA[:], I["invfA"][:, :], writes=[b_ifa])
                t.dma("sp", gbias[:], I["gatebias"][:, :, :], writes=[b_gb])
                t.dma("sp", diag[:], I["diag"][:, :, :], writes=[b_dg])
                t.dma("pool", oh16[:], I["oh16"][:, :, :], writes=[b_oh])
                t.dma("pool", causal[:], I["causal"][:, :, :], writes=[b_ca])
                t.op("dve", lambda e: e.tensor_tensor(out=angA[:], in0=posf[:].unsqueeze(2).broadcast_to([128, NSLOT_T, 16]),
                                                      in1=invfA[:].unsqueeze(1).broadcast_to([128, NSLOT_T, 16]), op=ALU.mult),
                     reads=[b_pos, b_ifa], writes=[b_ang])
                sincos(angA[:].rearrange("p a b -> p (a b)"), NSLOT_T * 16, sinA[:].rearrange("p a b -> p (a b)"),
                       cosA[:].rearrange("p a b -> p (a b)"), b_ang, b_cs)

                def rotaryA(bank, tile, dst, b_dst):
                    src3 = ps[bank][:].rearrange("p (h d) -> p h d", d=128)
                    cb = cosA[:, tile, :].unsqueeze(1).broadcast_to([128, NH, 16])
                    sn = sinA[:, tile, :].unsqueeze(1).broadcast_to([128, NH, 16])
                    x1, x2 = src3[:, :, 0:16], src3[:, :, 16:32]
                    t.op("dve", lambda e: e.tensor_tensor(out=tm1[:], in0=x1, in1=cb, op=ALU.mult), reads=[pb[bank], b_cs], writes=[b_tm1])
                    t.op("dve", lambda e: e.tensor_tensor(out=tm2[:], in0=x2, in1=sn, op=ALU.mult), reads=[pb[bank], b_cs], writes=[b_tm2])
                    t.op("dve", lambda e: e.tensor_tensor(out=dst[:, :, 0:16], in0=tm1[:], in1=tm2[:], op=ALU.subtract),
                         reads=[b_tm1, b_tm2], writes=[b_dst])
                    t.op("dve", lambda e: e.tensor_tensor(out=tm1[:], in0=x2, in1=cb, op=ALU.mult), reads=[pb[bank], b_cs], writes=[b_tm1])
                    t.op("dve", lambda e: e.tensor_tensor(out=tm2[:], in0=x1, in1=sn, op=ALU.mult), reads=[pb[bank], b_cs], writes=[b_tm2])
                    t.op("dve", lambda e: e.tensor_tensor(out=dst[:, :, 16:32], in0=tm1[:], in1=tm2[:], op=ALU.add),
                         reads=[b_tm1, b_tm2], writes=[b_dst])
                    t.op("dve", lambda e: e.tensor_copy(out=dst[:, :, 32:128], in_=src3[:, :, 32:128]), reads=[pb[bank]], writes=[b_dst])

                def transp4(src, b_src, dstT, b_dstT):
                    for h in range(NH):
                        t.op("pe", lambda e, h=h: e.transpose(psb[4][:, h * 128:(h + 1) * 128], src[:, h, :], identb[:]),
                             reads=[b_src, b_idb], writes=[pb[4]], inc=(h == NH - 1))
                    t.op("act", lambda e: e.activation(out=dstT, in_=psb[4][:, 0:NH * 128].rearrange("p (h n) -> p h n", n=128), func=AF.Copy),
                         reads=[pb[4]], writes=[b_dstT])

                def load_moba_w(p):
                    load_w(0, 1024 + 512 * p)
                    load_w(1, 2048 + 512 * p)
                    load_w(2, 512 * p)
                load_moba_w(0)
                for p in range(2):
                    for q in range(3):
                        load_hT(q)

                    def post(tile):
                        k2 = tile % 2
                        rotaryA(k2, tile, kbs[0], b_kbs[0])
                        t.op("act", lambda e: e.activation(out=V[:, tile, :], in_=ps[2 + k2][:], func=AF.Copy), reads=[pb[2 + k2]], writes=[b_V[tile]])
                        transp4(kbs[0], b_kbs[0], KT[:, :, tile * 128:(tile + 1) * 128], b_KT[tile])
                        if tile >= 24:
                            i = tile - 24
                            rotaryA(5 + k2, tile, kbs[1], b_kbs[1])
                            transp4(kbs[1], b_kbs[1], QT[:, :, i * 128:(i + 1) * 128], b_QT[i])
                        if tile % 8 == 7:
                            sg = tile // 8
                            t.op("dve", lambda e: e.tensor_reduce(out=kmT[:, :, 4 * sg:4 * sg + 4],
                                                                  in_=KT[:, :, sg * 1024:(sg + 1) * 1024].rearrange("p h (b k) -> p h b k", k=256),
                                                                  axis=AX.X, op=ALU.add),
                                 reads=b_KT[sg * 8:(sg + 1) * 8], writes=[b_km])

                    for tile in range(NSLOT_T):
                        k2 = tile % 2
                        load_hT(tile + 3)
                        project(tile, ring[0], rb[0], 512, k2)
                        project(tile, ring[1], rb[1], 512, 2 + k2)
                        if tile >= 24:
                            project(tile, ring[2], rb[2], 512, 5 + k2)
                        if tile >= 1:
                            post(tile - 1)
                    post(NSLOT_T - 1)
                    t.op("dve", lambda e: e.tensor_scalar(out=kmTb[:], in0=kmT[:], scalar1=1.0 / 256, scalar2=None, op0=ALU.mult),
                         reads=[b_km], writes=[b_kmb])
                    if p == 0:
                        load_moba_w(1)
                    else:
                        load_w(0, 4096)
                        load_w(1, 5120)
                        load_w(2, 3072)
                    for i in range(NT):
                        r = i // 2
                        for h in range(NH):
                            t.op("pe", lambda e, h=h: e.matmul(ps[4][:, h * 16:(h + 1) * 16], QT[:, h, i * 128:(i + 1) * 128], kmTb[:, h, :],
                                                               start=True, stop=True),
                                 reads=[b_QT[i], b_kmb], writes=[pb[4]], inc=(h == NH - 1))
                        t.op("dve", lambda e: e.tensor_tensor(out=gm[:], in0=ps[4][:, 0:NH * 16].rearrange("p (h n) -> p h n", n=16),
                                                              in1=gbias[:, r, :].unsqueeze(1).broadcast_to([128, NH, 16]), op=ALU.add),
                             reads=[pb[4], b_gb], writes=[b_gm])
                        for h in range(NH):
                            t.op("dve", lambda e, h=h: e.max(out=t8[:, h, :], in_=gm[:, h, :]), reads=[b_gm], writes=[b_t8])
                        t.op("dve", lambda e: e.tensor_scalar(out=thr[:], in0=t8[:, :, 2], scalar1=-1e29, scalar2=None, op0=ALU.max),
                             reads=[b_t8], writes=[b_thr])
                        t.op("dve", lambda e: e.tensor_tensor(out=sel[:], in0=gm[:], in1=thr[:].unsqueeze(2).broadcast_to([128, NH, 16]),
                                                              op=ALU.is_ge), reads=[b_gm, b_thr], writes=[b_sel])
                        t.op("dve", lambda e: e.tensor_tensor(out=sel[:], in0=sel[:], in1=diag[:, r, :].unsqueeze(1).broadcast_to([128, NH, 16]),
                                                              op=ALU.max), reads=[b_sel, b_dg], writes=[b_sel])
                        t.op("dve", lambda e: e.tensor_scalar(out=selb[:], in0=sel[:], scalar1=-1.0, scalar2=30000.0, op0=ALU.add, op1=ALU.mult),
                             reads=[b_sel], writes=[b_selb])
                        for h in range(NH):
                            t.op("pe", lambda e, h=h: e.transpose(psb[4][0:16, 512 + h * 128:512 + (h + 1) * 128], selb[:, h, :], identb[:]),
                                 reads=[b_selb, b_idb], writes=[pb[4]], inc=(h == NH - 1))
                        t.op("act", lambda e: e.activation(out=TT[:, :, i * 128:(i + 1) * 128],
                                                           in_=psb[4][0:16, 512:512 + NH * 128].rearrange("p (h n) -> p h n", n=128), func=AF.Copy),
                             reads=[pb[4]], writes=[b_TT[i]])
                    nS = 0
                    for h in range(NH):
                        for half in range(2):
                            qsl = slice(half * 512, (half + 1) * 512)
                            bq = b_QT[4 * half:4 * half + 4]
                            btt = b_TT[4 * half:4 * half + 4]
                            nkt = 28 + 4 * half
                            ob, lb = (5, 6) if (2 * h + half) % 2 == 0 else (7, 4)
                            def emit_pv(kt, Pt, bP):
                                t.op("pe", lambda e: e.matmul(ps[ob][:], V[:, kt, h * 128:(h + 1) * 128], Pt[:], start=(kt == 0), stop=(kt == nkt - 1)),
                                     reads=[b_V[kt], bP], writes=[pb[ob]], inc=False)
                                t.op("pe", lambda e: e.matmul(ps[lb][:], onesb[:], Pt[:], start=(kt == 0), stop=(kt == nkt - 1)),
                                     reads=[b_ones, bP], writes=[pb[lb]], inc=True)
                            pend = None
                            for kt in range(nkt):
                                sb_ = nS % 4
                                Pt, bP = Pb[nS % 3], b_P[nS % 3]
                                nS += 1
                                dg = kt >= 24 + 4 * half
                                t.op("pe", lambda e: e.matmul(ps[sb_][:], KT[:, h, kt * 128:(kt + 1) * 128], QT[:, h, qsl], start=True, stop=False),
                                     reads=[b_KT[kt]] + bq, writes=[pb[sb_]], inc=False)
                                t.op("pe", lambda e: e.matmul(ps[sb_][:], oh16[:, kt // 2, :], TT[:, h, qsl], start=False, stop=not dg),
                                     reads=[b_oh] + btt, writes=[pb[sb_]], inc=not dg)
                                if dg:
                                    t.op("pe", lambda e: e.matmul(ps[sb_][:], identb[:], causal[:, kt - 24 - 4 * half, :], start=False, stop=True),
                                         reads=[b_idb, b_ca], writes=[pb[sb_]], inc=True)
                                t.op("act", lambda e: e.activation(out=Pt[:], in_=ps[sb_][:], func=AF.Exp, scale=SCALE), reads=[pb[sb_]], writes=[bP])
                                if pend is not None:
                                    emit_pv(*pend)
                                pend = (kt, Pt, bP)
                            emit_pv(*pend)
                            t.op("dve", lambda e: e.reciprocal(out=rl[:], in_=ps[lb][:]), reads=[pb[lb]], writes=[b_rl])
                            t.op("dve", lambda e: e.tensor_tensor(out=OT[:, 4 * p + h, qsl], in0=ps[ob][:], in1=rl[:], op=ALU.mult),
                                 reads=[pb[ob], b_rl], writes=b_OT[4 * half:4 * half + 4])
                t.barrier()

            with contextlib.ExitStack() as esr:
                cosRs = [self.sb(f"cosR{i}", [128, 8, 128], F32, esr) for i in range(2)]
                sinRs = [self.sb(f"sinR{i}", [128, 8, 128], F32, esr) for i in range(2)]
                rtabs = nc.dram_tensor("rtabs", [4, 2, 128, 1024], F32, kind="Internal").ap()
                b_rtabs = [Buf(f"rtabs{i}") for i in range(4)]
                angR = self.sb("angR", [128, 8, 128], F32, esr)
                invfR = self.sb("invfR", [128, 128], F32, esr)
                kB = self.sb("kB", [128, 8, 512], BF16, esr)
                vB = self.sb("vB", [128, 8, 512], BF16, esr)
                qB = self.sb("qB", [128, 8, 512], BF16, esr)
                gB = self.sb("gB", [128, 8, 512], BF16, esr)
                Sf = self.sb("Sf", [128, 2, 512], F32, esr)
                Sb = self.sb("Sb", [128, 2, 512], BF16, esr)
                rtbl = self.sb("rtbl", [128, 4, 128], F32, esr)
                rxi = self.sb("rxi", [128, 4], F32, esr)
                rkz = self.sb("rkz", [128, 4], F32, esr)
                r1 = self.sb("r1", [128, 2, 128], F32, esr)
                r2 = self.sb("r2", [128, 2, 128], F32, esr)
                r3 = self.sb("r3", [128, 2, 128], F32, esr)
                r4 = self.sb("r4", [128, 2, 128], F32, esr)
                kqTs = [self.sb(f"kqT{i}", [128, 8, 128], BF16, esr) for i in range(2)]
                ATps = [self.sb(f"ATp{i}", [128, 2, 128], BF16, esr) for i in range(2)]
                osb = self.sb("osb", [128, 512], F32, esr)
                osq = self.sb("osq", [128, 512], BF16, esr)
                onb = self.sb("onb", [128, 512], BF16, esr)
                Kz = self.sb("Kz", [128, 512], BF16, esr)
                st = self.sb("stR", [128, 5, 2], F32, esr)
                b_csrs, b_angr, b_ifr = [Buf("cosinR0"), Buf("cosinR1")], Buf("angR"), Buf("invfR")
                b_kB = [Buf(f"kB{i}") for i in range(8)]
                b_vB = [Buf(f"vB{i}") for i in range(8)]
                b_qB = [Buf(f"qB{i}") for i in range(8)]
                b_gB = [Buf(f"gB{i}") for i in range(8)]
                b_Sf = [Buf("Sf0"), Buf("Sf1")]
                b_Sb = [Buf("Sb0"), Buf("Sb1")]
                b_rc = Buf("retconst")
                b_r = [Buf(f"r{i}") for i in range(4)]
                b_osb, b_osq, b_onb, b_Kz, b_st = (Buf(n) for n in ("osb", "osq", "onb", "Kz", "stR"))
                b_kqTs = [Buf("kqT0"), Buf("kqT1")]
                b_ATps = [Buf("ATp0"), Buf("ATp1")]
                t.dma("sp", invfR[:], I["invfR"][:, :], writes=[b_ifr])
                t.dma("sp", rtbl[:], I["rettbl"][:, :, :], writes=[b_rc])
                t.dma("sp", rxi[:], I["retxi"][:, :], writes=[b_rc])
                t.dma("sp", rkz[:], I["retkz"][:, :], writes=[b_rc])
                gam = [1.0 - 2.0 ** (-5 - h) for h in range(4)]

                def make_tables(rp, sg):
                    if sg > 3:
                        return
                    cR, sR, bcs = cosRs[sg % 2], sinRs[sg % 2], b_csrs[sg % 2]
                    cf, sf = cR[:].rearrange("p a b -> p (a b)"), sR[:].rearrange("p a b -> p (a b)")
                    if rp == 0:
                        t.op("dve", lambda e: e.tensor_tensor(out=angR[:], in0=posf[:, sg * 8:(sg + 1) * 8].unsqueeze(2).broadcast_to([128, 8, 128]),
                                                              in1=invfR[:].unsqueeze(1).broadcast_to([128, 8, 128]), op=ALU.mult),
                             reads=[b_pos, b_ifr], writes=[b_angr])
                        sincos(angR[:].rearrange("p a b -> p (a b)"), 1024, sf, cf, b_angr, bcs)
                        t.dma("sp", rtabs[sg, 0], cf, reads=[bcs], writes=[b_rtabs[sg]])
                        t.dma("sp", rtabs[sg, 1], sf, reads=[bcs], writes=[b_rtabs[sg]])
                    else:
                        t.dma("sp", cf, rtabs[sg, 0], reads=[b_rtabs[sg]], writes=[bcs])
                        t.dma("sp", sf, rtabs[sg, 1], reads=[b_rtabs[sg]], writes=[bcs])

                def rotaryR(bank, tile_in_group, dst, b_dst, sg):
                    cosR, sinR, b_csr = cosRs[sg % 2], sinRs[sg % 2], b_csrs[sg % 2]
                    src = ps[bank][:].rearrange("p (h s d) -> p h s d", s=2, d=128)
                    d4 = dst.rearrange("p (h s d) -> p h s d", s=2, d=128)
                    cb = cosR[:, tile_in_group, :].unsqueeze(1).broadcast_to([128, 2, 128])
                    sn = sinR[:, tile_in_group, :].unsqueeze(1).broadcast_to([128, 2, 128])
                    x1, x2 = src[:, :, 0, :], src[:, :, 1, :]
                    t.op("dve", lambda e: e.tensor_tensor(out=r1[:], in0=x1, in1=cb, op=ALU.mult), reads=[pb[bank], b_csr], writes=[b_r[0]])
                    t.op("dve", lambda e: e.tensor_tensor(out=r2[:], in0=x2, in1=sn, op=ALU.mult), reads=[pb[bank], b_csr], writes=[b_r[1]])
                    t.op("dve", lambda e: e.tensor_tensor(out=r3[:], in0=x2, in1=cb, op=ALU.mult), reads=[pb[bank], b_csr], writes=[b_r[2]])
                    t.op("dve", lambda e: e.tensor_tensor(out=r4[:], in0=x1, in1=sn, op=ALU.mult), reads=[pb[bank], b_csr], writes=[b_r[3]])
                    t.op("pool", lambda e: e.tensor_tensor(out=d4[:, :, 0, :], in0=r1[:], in1=r2[:], op=ALU.subtract),
                         reads=[b_r[0], b_r[1]], writes=[b_dst])
                    t.op("pool", lambda e: e.tensor_tensor(out=d4[:, :, 1, :], in0=r3[:], in1=r4[:], op=ALU.add),
                         reads=[b_r[2], b_r[3]], writes=[b_dst])

                for rp in range(2):
                    t.op("dve", lambda e: e.memset(Sf[:], 0.0), writes=b_Sf)
                    t.op("dve", lambda e: e.memset(Sb[:], 0.0), writes=b_Sb)
                    for q in range(3):
                        load_hT(q)
                    make_tables(rp, 0)
                    for sg in range(4):

                        def postkv(i, tile):
                            k2 = tile % 2
                            rotaryR(k2, i, kB[:, i, :], b_kB[i], sg)
                            t.op("act", lambda e: e.activation(out=vB[:, i, :], in_=ps[2 + k2][:], func=AF.Copy, scale=vmask[:, tile:tile + 1]),
                                 reads=[pb[2 + k2], b_vm], writes=[b_vB[i]])

                        def postqg(i, tile):
                            k2 = tile % 2
                            rotaryR(k2, i, qB[:, i, :], b_qB[i], sg)
                            t.op("act", lambda e: e.activation(out=gB[:, i, :], in_=ps[2 + k2][:], func=AF.Silu), reads=[pb[2 + k2]], writes=[b_gB[i]])

                        for i in range(8):
                            tile = sg * 8 + i
                            k2 = tile % 2
                            load_hT(tile + 3 if not (sg == 3 and i >= 5) else -1)
                            project(tile, ring[0], rb[0], 512, k2)
                            project(tile, ring[1], rb[1], 512, 2 + k2)
                            if i >= 1:
                                postkv(i - 1, tile - 1)
                            if i == 3:
                                make_tables(rp, sg + 1)
                        postkv(7, sg * 8 + 7)
                        if sg == 3:
                            load_w(0, 6144 + 512 * rp)
                            for q in range(3):
                                load_hT(24 + q)
                            for i in range(8):
                                tile = 24 + i
                                k2 = tile % 2
                                load_hT(tile + 3)
                                project(tile, ring[2], rb[2], 512, k2)
                                project(tile, ring[0], rb[0], 512, 2 + k2)
                                if i >= 1:
                                    postqg(i - 1, tile - 1)
                            postqg(7, 31)
                            if rp == 0:
                                load_w(0, 4096 + 512)
                                load_w(1, 5120 + 512)
                                load_w(2, 3072 + 512)
                            else:
                                for cg in range(4):
                                    t.dma("pool", ring[cg][:].rearrange("p (k n) -> p k n", n=512),
                                          I["w_out"][:, cg * 512:(cg + 1) * 512].rearrange("(k p) n -> p k n", p=128),
                                          writes=[rb[cg]] + (b_hTt if cg == 3 else []), sem=cg)
                        h0 = 2 * rp

                        def partA(i):
                            for hh in range(2):
                                for q_, (src_, bsrc) in enumerate(((kB, b_kB[i]), (qB, b_qB[i]))):
                                    for dc in range(2):
                                        col = (4 * hh + 2 * q_ + dc) * 128
                                        t.op("pe", lambda e, col=col, dc=dc, src_=src_, hh=hh: e.transpose(
                                            psb[4][:, col:col + 128], src_[:, i, hh * 256 + dc * 128:hh * 256 + (dc + 1) * 128], identb[:]),
                                            reads=[bsrc, b_idb], writes=[pb[4]], inc=(hh == 1 and q_ == 1 and dc == 1))
                            t.op("act", lambda e: e.activation(out=kqTs[i % 2][:], in_=psb[4][:].rearrange("p (a n) -> p a n", n=128), func=AF.Copy),
                                 reads=[pb[4]], writes=[b_kqTs[i % 2]])
                            for hh in range(2):
                                for dc in range(2):
                                    t.op("pe", lambda e, dc=dc, hh=hh: e.matmul(ps[5][:, hh * 128:(hh + 1) * 128], kqTs[i % 2][:, 4 * hh + dc, :], kqTs[i % 2][:, 4 * hh + 2 + dc, :],
                                                                                start=(dc == 0), stop=(dc == 1)),
                                         reads=[b_kqTs[i % 2]], writes=[pb[5]], inc=(hh == 1 and dc == 1))
                            t.op("dve", lambda e: e.tensor_tensor(out=ATps[i % 2][:], in0=ps[5][:, 0:256].rearrange("p (a n) -> p a n", n=128),
                                                                  in1=rtbl[:, h0:h0 + 2, :], op=ALU.mult),
                                 reads=[pb[5], b_rc], writes=[b_ATps[i % 2]])

                        def partB(i):
                            for hh in range(2):
                                hs = slice(hh * 256, (hh + 1) * 256)
                                t.op("pe", lambda e: e.matmul(ps[6][:, hs], ATps[i % 2][:, hh, :], vB[:, i, hs], start=True, stop=False),
                                     reads=[b_ATps[i % 2], b_vB[i]], writes=[pb[6]], inc=False)
                                for dc in range(2):
                                    t.op("pe", lambda e, dc=dc: e.matmul(ps[6][:, hs], kqTs[i % 2][:, 4 * hh + 2 + dc, :], Sb[:, hh, dc * 256:(dc + 1) * 256],
                                                                         start=False, stop=(dc == 1)),
                                         reads=[b_kqTs[i % 2], b_Sb[hh]], writes=[pb[6]], inc=(dc == 1))
                            for hh in range(2):
                                hs = slice(hh * 256, (hh + 1) * 256)
                                t.op("act", lambda e: e.activation(out=osb[:, hs], in_=ps[6][:, hs], func=AF.Copy, scale=rxi[:, h0 + hh:h0 + hh + 1],
                                                                   accum_out=st[:, 0, hh:hh + 1]), reads=[pb[6], b_rc], writes=[b_osb, b_st])
                            for hh in range(2):
                                hs = slice(hh * 256, (hh + 1) * 256)
                                t.op("act", lambda e: e.activation(out=osq[:, hs], in_=osb[:, hs], func=AF.Square, accum_out=st[:, 1, hh:hh + 1]),
                                     reads=[b_osb], writes=[b_osq, b_st])
                            t.op("dve", lambda e: e.tensor_scalar(out=st[:, 2, :], in0=st[:, 0, :], scalar1=1.0 / 256, scalar2=None, op0=ALU.mult),
                                 reads=[b_st], writes=[b_st])
                            t.op("dve", lambda e: e.tensor_tensor(out=st[:, 3, :], in0=st[:, 2, :], in1=st[:, 2, :], op=ALU.mult),
                                 reads=[b_st], writes=[b_st])
                            t.op("dve", lambda e: e.scalar_tensor_tensor(out=st[:, 3, :], in0=st[:, 1, :], scalar=1.0 / 256, in1=st[:, 3, :],
                                                                         op0=ALU.mult, op1=ALU.subtract), reads=[b_st], writes=[b_st])
                            t.op("act", lambda e: e.activation(out=st[:, 4, :], in_=st[:, 3, :], func=AF.Sqrt, bias=EPS, scale=1.0),
                                 reads=[b_st], writes=[b_st])
                            t.op("dve", lambda e: e.reciprocal(out=st[:, 4, :], in_=st[:, 4, :]), reads=[b_st], writes=[b_st])
                            for hh in range(2):
                                hs = slice(hh * 256, (hh + 1) * 256)
                                t.op("dve", lambda e: e.tensor_scalar(out=osb[:, hs], in0=osb[:, hs], scalar1=st[:, 2, hh:hh + 1], scalar2=st[:, 4, hh:hh + 1],
                                                                      op0=ALU.subtract, op1=ALU.mult), reads=[b_osb, b_st], writes=[b_osb])
                            t.op("pool", lambda e: e.tensor_tensor(out=onb[:], in0=osb[:], in1=gB[:, i, :], op=ALU.mult),
                                 reads=[b_osb, b_gB[i]], writes=[b_onb])
                            for a_ in range(4):
                                t.op("pe", lambda e, a_=a_: e.transpose(psb[1][:, a_ * 128:(a_ + 1) * 128], onb[:, a_ * 128:(a_ + 1) * 128], identb[:]),
                                     reads=[b_onb, b_idb], writes=[pb[1]], inc=(a_ == 3))
                            t.op("act", lambda e: e.activation(out=OT[:, 8 + 2 * h0:8 + 2 * h0 + 4, i * 128:(i + 1) * 128],
                                                               in_=psb[1][:, 0:512].rearrange("p (a n) -> p a n", n=128), func=AF.Copy),
                                 reads=[pb[1]], writes=[b_OT[i]])

                        def upd(i):
                            t.op("pool", lambda e: e.tensor_tensor(out=Kz[:].rearrange("p (a n) -> p a n", n=256),
                                                                   in0=kB[:, i, :].rearrange("p (a n) -> p a n", n=256),
                                                                   in1=rkz[:, h0:h0 + 2].unsqueeze(2).broadcast_to([128, 2, 256]), op=ALU.mult),
                                 reads=[b_kB[i], b_rc], writes=[b_Kz])
                            for hh in range(2):
                                hs = slice(hh * 256, (hh + 1) * 256)
                                sbank = 7 if hh == 0 else 3
                                for dc in range(2):
                                    t.op("pe", lambda e, dc=dc: e.matmul(ps[sbank][:, dc * 256:(dc + 1) * 256], Kz[:, hh * 256 + dc * 128:hh * 256 + (dc + 1) * 128],
                                                                         vB[:, i, hs], start=True, stop=True),
                                         reads=[b_Kz, b_vB[i]], writes=[pb[sbank]], inc=(dc == 1))
                            for hh in range(2):
                                sbank = 7 if hh == 0 else 3
                                t.op("dve", lambda e: e.scalar_tensor_tensor(out=Sf[:, hh, :], in0=Sf[:, hh, :], scalar=float(gam[h0 + hh] ** 128), in1=ps[sbank][:],
                                                                             op0=ALU.mult, op1=ALU.add), reads=[pb[sbank], b_Sf[hh]], writes=[b_Sf[hh]])
                            for hh in range(2):
                                t.op("act", lambda e: e.activation(out=Sb[:, hh, :], in_=Sf[:, hh, :], func=AF.Copy), reads=[b_Sf[hh]], writes=[b_Sb[hh]])

                        if sg == 3:
                            partA(0)
                            for i in range(8):
                                if i + 1 < 8:
                                    partA(i + 1)
                                partB(i)
                                upd(i)
                        else:
                            for i in range(8):
                                upd(i)
                t.barrier()

            with contextlib.ExitStack() as eso:
                garow = self.sb("garow", [128, D], F32, eso)
                xo = [self.sb(f"xo{i}", [128, D], F32, eso) for i in range(2)]
                tmpo = [self.sb(f"tmpo{i}", [128, 512], F32, eso) for i in range(2)]
                b_ga = Buf("garow")
                b_xo = [Buf("xo0"), Buf("xo1")]
                b_tmpo = [Buf("tmpo0"), Buf("tmpo1")]
                t.dma("sp", garow[:], self.modrows[2:3, :].partition_broadcast(128), reads=[self.b_modrows[2]], writes=[b_ga])
                nb = 0
                def load_xo(i):
                    if i < NT:
                        t.dma("sp", xo[i % 2][:], I["xs"][(24 + i) * 128:(25 + i) * 128, :], writes=[b_xo[i % 2]])
                load_xo(0)
                load_xo(1)
                for i in range(NT):
                    x_, bx = xo[i % 2], b_xo[i % 2]
                    for cg in range(4):
                        bank = nb % 4
                        tm, btm = tmpo[nb % 2], b_tmpo[nb % 2]
                        nb += 1
                        for c in range(NKC):
                            t.op("pe", lambda e, c=c: e.matmul(ps[bank][:], OT[:, c, i * 128:(i + 1) * 128], ring[cg][:, c * 512:(c + 1) * 512],
                                                               start=(c == 0), stop=(c == NKC - 1)),
                                 reads=[b_OT[i], rb[cg]], writes=[pb[bank]], inc=(c == NKC - 1))
                        t.op("dve", lambda e: e.tensor_tensor(out=tm[:], in0=ps[bank][:], in1=garow[:, cg * 512:(cg + 1) * 512], op=ALU.mult),
                             reads=[pb[bank], b_ga], writes=[btm])
                        t.op("pool", lambda e: e.tensor_tensor(out=x_[:, cg * 512:(cg + 1) * 512], in0=x_[:, cg * 512:(cg + 1) * 512], in1=tm[:], op=ALU.add),
                             reads=[btm, bx], writes=[bx])
                    t.dma("sp", self.x1s[i * 128:(i + 1) * 128, :], x_[:], reads=[bx], writes=[self.b_x1s])
                    load_xo(i + 2)
                t.barrier()

    def build(self):
        nc = self.nc
        I = self.ins
        ne = self.ne
        self.din("c_col", [128, NKC])
        self.din("w_ada", [D, 6 * D])
        self.din("b_ada", [1, 6 * D])
        self.din("norm_mix", [1, D])
        self.din("norm_ffn", [1, D])
        self.din("norm_out", [1, D])
        self.din("w_router", [D, 64])
        self.din("router_bias", [1, 64])
        self.din("w_gate", [ne, D, 512])
        self.din("w_up", [ne, D, 512])
        self.din("w_down", [ne, 512, D])
        self.din("w_sh_gate", [D, 512])
        self.din("w_sh_up", [D, 512])
        self.din("w_sh_down", [512, D])
        self.din("ident", [128, 128])
        if self.mode in ("testA", "full"):
            self.din("xs", [NSLOT_T * 128, D])
            self.din("pos_i", [128, NSLOT_T], mybir.dt.int32)
            self.din("vmask", [128, NSLOT_T])
            self.din("w_in", [D, 7168])
            self.din("w_out", [D, D])
            self.din("invfA", [128, 16])
            self.din("invfR", [128, 128])
            self.din("gatebias", [128, 4, 16])
            self.din("diag", [128, 4, 16])
            self.din("oh16", [16, 16, 128])
            self.din("causal", [128, 4, 512])
            self.din("rettbl", [128, 4, 128])
            self.din("retxi", [128, 4])
            self.din("retkz", [128, 4])
        if self.mode == "testB":
            self.x1s = self.din("x1s", [NT * 128, D])
        elif self.mode == "testA":
            self.x1s = nc.dram_tensor("x1s", [NT * 128, D], F32, kind="ExternalOutput").ap()
        else:
            self.x1s = nc.dram_tensor("x1s", [NT * 128, D], F32, kind="Internal").ap()
        self.b_x1s = Buf("x1s")
        self.modrows = nc.dram_tensor("modrows", [8, D], F32, kind="Internal").ap()
        self.b_modrows = [Buf(f"modrows{v}") for v in range(6)]
        if self.mode != "testA":
            self.out = nc.dram_tensor("out", [NT * 128, D], F32, kind="ExternalOutput").ap()
        es = self.es
        ps = [es.enter_context(nc.psum_tensor(f"ps{i}", [128, 512], F32)) for i in range(8)]
        pb = [Buf(f"ps{i}") for i in range(8)]
        ring = [self.sb(f"ring{i}", [128, NKC * 512], BF16) for i in range(4)]
        rb = [Buf(f"ring{i}") for i in range(4)]
        ident32 = self.sb("ident32", [128, 128], F32)
        b_id = Buf("ident")
        t = Trk(nc)
        self.t = t
        t.dma("sp", ident32[:], I["ident"][:, :], writes=[b_id])
        if self.mode not in ("testA", "full"):
            with contextlib.ExitStack() as es0:
                for _ in self.phase0_gen(t, ps, pb, ring, rb, es0):
                    pass
                t.barrier()
        if self.mode == "test0":
            bo = Buf("o")
            t.dma("sp", self.out[0:6, :], self.modrows[0:6, :], reads=self.b_modrows, writes=[bo])
            t.drain("sp", [bo])
        if self.mode in ("testA", "full"):
            self.phaseA(t, ps, pb, ring, rb, ident32, b_id)
        if self.mode in ("testB", "full"):
            self.phaseB(t, ps, pb, ring, rb, ident32, b_id)
        t.close()
        es.close()
        return nc


def _common_inputs(inputs, b, ne=NE):
    f = np.ascontiguousarray
    return {
        "c_col": f(inputs["c"][b].reshape(NKC, 128).T),
        "w_ada": inputs["w_ada"][0],
        "b_ada": inputs["b_ada"][0:1],
        "norm_mix": inputs["norm_mix"][0:1],
        "norm_ffn": inputs["norm_ffn"][0:1],
        "norm_out": inputs["norm_out"].reshape(1, D),
        "w_router": inputs["w_router"][0],
        "router_bias": inputs["router_bias"][0:1],
        "w_gate": inputs["w_gate"][0][:ne],
        "w_up": inputs["w_up"][0][:ne],
        "w_down": inputs["w_down"][0][:ne],
        "w_sh_gate": inputs["w_sh_gate"][0],
        "w_sh_up": inputs["w_sh_up"][0],
        "w_sh_down": inputs["w_sh_down"][0],
        "ident": np.eye(128, dtype=np.float32),
    }


def _phaseA_inputs(inputs, b, j):
    f32 = np.float32
    own_end = 1024 * (j + 1)
    start = own_end - 4096
    lo = max(start, 0)
    xs = np.zeros((4096, D), f32)
    xs[lo - start:] = inputs["x"][b, lo:own_end]
    pos = np.zeros((4096,), np.int32)
    pos[lo - start:] = inputs["positions"][b, lo:own_end]
    tile_valid = ((np.arange(NSLOT_T) * 128 + start) >= 0).astype(f32)
    r = np.arange(4)[:, None]
    kb = np.arange(16)[None, :]
    gatebias = np.where((kb < 12 + r) & (kb >= 12 - 4 * j), 0.0, -1e30).astype(f32)
    diag = (kb >= 12 + r).astype(f32)
    oh16 = (np.arange(16)[:, None, None] == np.arange(16)[None, :, None]) * np.ones((1, 1, 128))
    pp = np.arange(128)[:, None, None]
    dd = np.arange(4)[None, :, None]
    cc = np.arange(512)[None, None, :]
    causal = np.where(128 * dd + pp <= cc, 0.0, -30000.0)
    invfA = (np.float32(500000.0) ** (-(np.arange(16, dtype=f32) * f32(2.0) / f32(32.0)))).astype(f32)
    invfR = (np.float32(10000.0) ** (-np.linspace(0.0, 1.0, 128, dtype=f32))).astype(f32)
    gam = 1.0 - 2.0 ** (-5.0 - np.arange(4))
    m = np.arange(128)
    rettbl = (gam[None, :, None] ** (-(m[:, None, None] + 1.0))) / 16.0 * (m[None, None, :] >= m[:, None, None])
    retxi = gam[None, :] ** (m[:, None] + 1.0)
    retkz = gam[None, :] ** (127.0 - m[:, None]) / 16.0
    c = np.ascontiguousarray
    return {
        "xs": xs,
        "pos_i": c(pos.reshape(NSLOT_T, 128).T),
        "vmask": c(np.broadcast_to(tile_valid[None, :], (128, NSLOT_T))).astype(f32),
        "w_in": inputs["w_in"][0],
        "w_out": inputs["w_out"][0],
        "invfA": c(np.broadcast_to(invfA[None, :], (128, 16))).astype(f32),
        "invfR": c(np.broadcast_to(invfR[None, :], (128, 128))).astype(f32),
        "gatebias": c(np.broadcast_to(gatebias[None], (128, 4, 16))).astype(f32),
        "diag": c(np.broadcast_to(diag[None], (128, 4, 16))).astype(f32),
        "oh16": c(oh16).astype(f32),
        "causal": c(causal).astype(f32),
        "rettbl": c(rettbl).astype(f32),
        "retxi": c(retxi).astype(f32),
        "retkz": c(retkz).astype(f32),
    }


_PROG = {}


def kernel(**inputs):
    inputs = {k: np.asarray(v) for k, v in inputs.items()}
    if "full" not in _PROG:
        _PROG["full"] = Prog(mode="full").build()
    nc = _PROG["full"]
    in_maps = []
    for core in range(8):
        b, j = core // 4, core % 4
        im = _common_inputs(inputs, b)
        im.update(_phaseA_inputs(inputs, b, j))
        in_maps.append(im)
    res = run_bass_kernel_spmd(nc, in_maps, core_ids=list(range(8)))
    out = np.empty((2, 4096, D), np.float32)
    for core in range(8):
        b, j = core // 4, core % 4
        out[b, 1024 * j:1024 * (j + 1)] = res.results[core]["out"]
    return out
```

```python
import contextlib
import numpy as np
import concourse.bass as bass
import concourse.mybir as mybir
from concourse.bass_utils import run_bass_kernel_spmd

F32 = mybir.dt.float32
BF16 = mybir.dt.bfloat16
AF = mybir.ActivationFunctionType
ALU = mybir.AluOpType
AX = mybir.AxisListType

D = 2048
NKC = 16
NE = 64
EPS = 1e-6
NT = 8
NSLOT_T = 32


class Buf:
    __slots__ = ("name", "w", "r")

    def __init__(self, name):
        self.name = name
        self.w = None
        self.r = []


class Trk:
    def __init__(self, nc, n_dma_sems=28, n_fixed=8):
        self.nc = nc
        self.sems = {}
        self.seen = {}
        self.engines = {"pe": nc.tensor, "act": nc.scalar, "dve": nc.vector, "pool": nc.gpsimd, "sp": nc.sync}
        self._ctx = []
        for k in ("pe", "act", "dve", "pool"):
            self._mk(k)
        self.n_dma = 0
        self.n_dma_pool = 0
        self.n_fixed = n_fixed
        self.n_dma_sems = n_dma_sems
        for i in range(n_dma_sems):
            self._mk(("dma", i))

    def _mk(self, key):
        nm = "s_" + "".join(ch for ch in str(key) if ch.isalnum())
        cm = self.nc.semaphore(nm)
        h = cm.__enter__()
        self._ctx.append(cm)
        self.sems[key] = [h, 0]
        for e in self.engines:
            self.seen.setdefault(e, {})[key] = 0

    def close(self):
        for cm in reversed(self._ctx):
            cm.__exit__(None, None, None)

    def _wait(self, ename, dep):
        if dep is None:
            return
        key, val = dep
        if self.seen[ename].get(key, 0) >= val:
            return
        self.engines[ename].wait_ge(self.sems[key][0], val)
        self.seen[ename][key] = val

    def _deps(self, ename, reads, writes):
        for b in reads:
            if b.w is not None and not (ename == "pe" and b.w[0] == "pe"):
                self._wait(ename, b.w)
        for b in writes:
            if b.w is not None and not (ename == "pe" and b.w[0] == "pe"):
                self._wait(ename, b.w)
            for d in b.r:
                if not (ename == "pe" and d[0] == "pe"):
                    self._wait(ename, d)

    def _record(self, key, dep, reads, writes):
        for b in writes:
            b.w = dep
            b.r = []
        for b in reads:
            b.r = [d for d in b.r if d[0] != key] + [dep]

    def op(self, ename, fn, reads=(), writes=(), inc=True):
        self._deps(ename, reads, writes)
        ins = fn(self.engines[ename])
        s = self.sems[ename]
        if inc:
            s[1] += 1
            ins.then_inc(s[0], 1)
            val = s[1]
        else:
            val = s[1] + 1
        self._record(ename, (ename, val), reads, writes)
        return ins

    def dma(self, qname, out, in_, reads=(), writes=(), sem=None):
        if sem is None:
            if qname == "pool":
                sem = 4 + self.n_dma_pool % 4
                self.n_dma_pool += 1
            else:
                sem = self.n_fixed + self.n_dma % (self.n_dma_sems - self.n_fixed)
                self.n_dma += 1
        key = ("dma", sem)
        s = self.sems[key]
        if s[1] > 0:
            self._wait(qname, (key, s[1]))
        self._deps(qname, reads, writes)
        ins = self.engines[qname].dma_start(out=out, in_=in_)
        s[1] += 16
        ins.then_inc(s[0], 16)
        self._record(key, (key, s[1]), reads, writes)
        return ins

    def drain(self, ename, bufs):
        for b in bufs:
            self._wait(ename, b.w)

    def barrier(self):
        for e in self.engines:
            for key, s in self.sems.items():
                if s[1] > 0 and key != e:
                    self._wait(e, (key, s[1]))


class Prog:
    def __init__(self, mode="full", ne=NE):
        self.mode = mode
        self.ne = ne
        self.nc = bass.Bass("TRN2", target_bir_lowering=False)
        self.es = contextlib.ExitStack()
        self.ins = {}

    def din(self, name, shape, dt=F32):
        ap = self.nc.dram_tensor(name, list(shape), dt, kind="ExternalInput").ap()
        self.ins[name] = ap
        return ap

    def sb(self, name, shape, dt, es=None):
        return (es or self.es).enter_context(self.nc.sbuf_tensor("sb_" + name, list(shape), dt))

    def phase0_gen(self, t, ps, pb, ring, rb, es):
        I = self.ins
        ccol = self.sb("ccol", [128, NKC], F32, es)
        csil = self.sb("csil", [128, NKC], F32, es)
        crep = self.sb("crep", [128, NKC, 128], BF16, es)
        rowt = [self.sb(f"rowt{i}", [128, D], F32, es) for i in range(2)]
        nrow = self.sb("nrow", [128, D], F32, es)
        b_c, b_crep = Buf("ccol"), Buf("crep")
        b_rowt = [Buf("rowt0"), Buf("rowt1")]
        b_nrow = Buf("nrow")
        t.dma("sp", ccol[:], I["c_col"][:, :], writes=[b_c])
        t.op("act", lambda e: e.activation(out=csil[:], in_=ccol[:], func=AF.Silu), reads=[b_c], writes=[b_c])
        t.op("dve", lambda e: e.tensor_copy(out=crep[:], in_=csil[:].unsqueeze(2).broadcast_to([128, NKC, 128])),
             reads=[b_c], writes=[b_crep])

        def load(i):
            col0 = i * 512
            t.dma("pool", ring[i % 4][:].rearrange("p (k n) -> p k n", n=512),
                  I["w_ada"][:, col0:col0 + 512].rearrange("(k p) n -> p k n", p=128),
                  writes=[rb[i % 4]], sem=i % 4)
        for i in range(3):
            load(i)
        for v in range(6):
            rt = rowt[v % 2]
            t.dma("sp", rt[:], I["b_ada"][0:1, v * D:(v + 1) * D].partition_broadcast(128), writes=[b_rowt[v % 2]])
            if v in (1, 4):
                src = I["norm_mix"] if v == 1 else I["norm_ffn"]
                t.dma("sp", nrow[:], src[0:1, :].partition_broadcast(128), writes=[b_nrow])
            for cg in range(4):
                i = v * 4 + cg
                if i + 3 < 24:
                    load(i + 3)
                slot = i % 4
                bank = 6 + cg % 2
                for k in range(NKC):
                    t.op("pe", lambda e, k=k: e.matmul(ps[bank][:], crep[:, k, :], ring[slot][:, k * 512:(k + 1) * 512],
                                                       start=(k == 0), stop=(k == NKC - 1)),
                         reads=[b_crep, rb[slot]], writes=[pb[bank]], inc=(k == NKC - 1))
                t.op("dve", lambda e: e.tensor_tensor(out=rt[:, cg * 512:(cg + 1) * 512], in0=ps[bank][:],
                                                      in1=rt[:, cg * 512:(cg + 1) * 512], op=ALU.add),
                     reads=[pb[bank], b_rowt[v % 2]], writes=[b_rowt[v % 2]])
                if cg == 3:
                    if v in (1, 4):
                        t.op("dve", lambda e: e.scalar_tensor_tensor(out=rt[:], in0=rt[:], scalar=1.0, in1=nrow[:],
                                                                     op0=ALU.add, op1=ALU.mult),
                             reads=[b_rowt[v % 2], b_nrow], writes=[b_rowt[v % 2]])
                    t.dma("sp", self.modrows[v:v + 1, :], rt[0:1, :], reads=[b_rowt[v % 2]], writes=[self.b_modrows[v]])
                yield (v, cg)

    def routing(self, t, R, lg_ap, bl, rbrow, b_rb, wn_out, bw):
        sc, bi, m1, eq, g2, m2, t8, gm, bm, e8, ws = (R[k] for k in ("sc", "bi", "m1", "eq", "g2", "m2", "t8", "gm", "bm", "e8", "ws"))
        B = R["B"]

        def v3(x):
            return x[:].rearrange("p (g k) -> p g k", k=8)

        def bc(x):
            return x[:].unsqueeze(2).broadcast_to([128, 8, 8])
        t.op("act", lambda e: e.activation(out=sc[:], in_=lg_ap, func=AF.Sigmoid), reads=[bl], writes=[B["sc"]])
        t.op("dve", lambda e: e.tensor_tensor(out=bi[:], in0=sc[:], in1=rbrow[:], op=ALU.add), reads=[B["sc"], b_rb], writes=[B["bi"]])
        t.op("dve", lambda e: e.tensor_reduce(out=m1[:], in_=v3(bi), axis=AX.X, op=ALU.max), reads=[B["bi"]], writes=[B["m1"]])
        t.op("dve", lambda e: e.tensor_tensor(out=v3(eq), in0=v3(bi), in1=bc(m1), op=ALU.is_equal), reads=[B["bi"], B["m1"]], writes=[B["eq"]])
        t.op("dve", lambda e: e.scalar_tensor_tensor(out=g2[:], in0=eq[:], scalar=-1e30, in1=bi[:], op0=ALU.mult, op1=ALU.add),
             reads=[B["eq"], B["bi"]], writes=[B["g2"]])
        t.op("dve", lambda e: e.tensor_reduce(out=m2[:], in_=v3(g2), axis=AX.X, op=ALU.max), reads=[B["g2"]], writes=[B["m2"]])
        t.op("dve", lambda e: e.tensor_tensor(out=m2[:], in0=m2[:], in1=m1[:], op=ALU.add), reads=[B["m2"], B["m1"]], writes=[B["m2"]])
        t.op("dve", lambda e: e.max(out=t8[:], in_=m2[:]), reads=[B["m2"]], writes=[B["t8"]])
        t.op("dve", lambda e: e.tensor_scalar(out=gm[:], in0=m2[:], scalar1=t8[:, 3:4], scalar2=None, op0=ALU.is_ge),
             reads=[B["m2"], B["t8"]], writes=[B["gm"]])
        t.op("dve", lambda e: e.tensor_scalar(out=t8[:], in0=gm[:], scalar1=-1.0, scalar2=1e30, op0=ALU.add, op1=ALU.mult),
             reads=[B["gm"]], writes=[B["t8"]])
        t.op("dve", lambda e: e.tensor_tensor(out=v3(bm), in0=v3(bi), in1=bc(gm), op=ALU.mult), reads=[B["bi"], B["gm"]], writes=[B["bm"]])
        t.op("dve", lambda e: e.tensor_tensor(out=v3(bm), in0=v3(bm), in1=bc(t8), op=ALU.add), reads=[B["bm"], B["t8"]], writes=[B["bm"]])
        t.op("dve", lambda e: e.max(out=e8[:], in_=bm[:]), reads=[B["bm"]], writes=[B["e8"]])
        t.op("dve", lambda e: e.tensor_scalar(out=eq[:], in0=bm[:], scalar1=e8[:, 7:8], scalar2=None, op0=ALU.is_ge),
             reads=[B["bm"], B["e8"]], writes=[B["eq"]])
        t.op("dve", lambda e: e.tensor_tensor(out=g2[:], in0=eq[:], in1=sc[:], op=ALU.mult), reads=[B["eq"], B["sc"]], writes=[B["g2"]])
        t.op("dve", lambda e: e.tensor_reduce(out=ws[:], in_=g2[:], axis=AX.X, op=ALU.add), reads=[B["g2"]], writes=[B["ws"]])
        t.op("dve", lambda e: e.reciprocal(out=ws[:], in_=ws[:]), reads=[B["ws"]], writes=[B["ws"]])
        t.op("dve", lambda e: e.tensor_scalar(out=wn_out, in0=g2[:], scalar1=ws[:, 0:1], scalar2=2.5, op0=ALU.mult, op1=ALU.mult),
             reads=[B["g2"], B["ws"]], writes=[bw])

    def phaseB(self, t, ps, pb, ring, rb, ident32, b_id):
        nc = self.nc
        I = self.ins
        ne = self.ne
        with contextlib.ExitStack() as es:
            h2T = self.sb("h2T", [128, NKC, NT * 128], BF16, es)
            wn = self.sb("wn", [128, NT, 64], F32, es)
            ones1 = self.sb("ones1", [128, 1], F32, es)
            b_y = [[Buf(f"y{i}_{c}") for c in range(4)] for i in range(NT)]
            b_h2T = [Buf(f"h2T{i}") for i in range(NT)]
            b_wn = [Buf(f"wn{i}") for i in range(NT)]
            b_act = [[Buf(f"act{h}_{f}") for f in range(4)] for h in range(2)]
            b_sil = [Buf("sil0"), Buf("sil1")]
            b_one = Buf("ones1")
            t.op("dve", lambda e: e.memset(ones1[:], 1.0), writes=[b_one])
            mats = []
            for e_ in range(ne):
                mats += [("g", I["w_gate"][e_]), ("g", I["w_up"][e_]), ("d", I["w_down"][e_])]
            mats += [("g", I["w_sh_gate"]), ("g", I["w_sh_up"]), ("d", I["w_sh_down"])]
            state = {"issued": 0, "consumed": 0}

            def pump():
                while state["issued"] < len(mats) and state["issued"] - 4 < state["consumed"]:
                    i = state["issued"]
                    kind, src = mats[i]
                    slot = i % 4
                    if kind == "g":
                        t.dma("pool", ring[slot][:].rearrange("p (k n) -> p k n", n=512),
                              src.rearrange("(k p) n -> p k n", p=128), writes=[rb[slot]], sem=slot)
                    else:
                        t.dma("pool", ring[slot][:].rearrange("p (k n) -> p k n", n=D),
                              src.rearrange("(k p) n -> p k n", p=128), writes=[rb[slot]], sem=slot)
                    state["issued"] += 1
            pump()
            with contextlib.ExitStack() as es1:
                g2row = self.sb("g2row", [128, D], F32, es1)
                shfrow = self.sb("shfrow", [128, D], F32, es1)
                xt = [self.sb(f"xtB{i}", [128, D], F32, es1) for i in range(2)]
                hf = self.sb("hfB", [128, D], F32, es1)
                sq = self.sb("sqB", [128, D], BF16, es1)
                hT32 = self.sb("hT32", [128, NKC, 128], F32, es1)
                wr32 = self.sb("wr32", [128, NKC, 64], F32, es1)
                rbrow = self.sb("rbrow", [128, 64], F32, es1)
                ss = self.sb("ssB", [128, 2], F32, es1)
                R = {k: self.sb("rt_" + k, [128, n], F32, es1) for k, n in
                     (("sc", 64), ("bi", 64), ("m1", 8), ("eq", 64), ("g2", 64), ("m2", 8), ("t8", 8), ("gm", 8),
                      ("bm", 64), ("e8", 8), ("ws", 1))}
                R["B"] = {k: Buf("rt_" + k) for k in ("sc", "bi", "m1", "eq", "g2", "m2", "t8", "gm", "bm", "e8", "ws")}
                b_g2, b_shf, b_wr, b_rbr = Buf("g2row"), Buf("shfrow"), Buf("wr32"), Buf("rbrow")
                b_xt = [Buf("xt0"), Buf("xt1")]
                b_hf, b_sq, b_hT32, b_ss = Buf("hf"), Buf("sq"), Buf("hT32"), Buf("ss")
                t.dma("sp", g2row[:], self.modrows[4:5, :].partition_broadcast(128), reads=[self.b_modrows[4]], writes=[b_g2])
                t.dma("sp", shfrow[:], self.modrows[3:4, :].partition_broadcast(128), reads=[self.b_modrows[3]], writes=[b_shf])
                t.dma("sp", wr32[:], I["w_router"].rearrange("(k p) n -> p k n", p=128), writes=[b_wr])
                t.dma("sp", rbrow[:], I["router_bias"][0:1, :].partition_broadcast(128), writes=[b_rbr])
                hfs = [hf, self.sb("hfB1", [128, D], F32, es1)]
                b_hfs = [b_hf, Buf("hf1")]
                ss4 = self.sb("ssB4", [128, 4], F32, es1)
                b_ss4 = [Buf("ssB40"), Buf("ssB41")]

                def normB(i):
                    if i >= NT:
                        return
                    x_, bx = xt[i % 2], b_xt[i % 2]
                    hf_, bhf = hfs[i % 2], b_hfs[i % 2]
                    sa, sb2, bss = ss4[:, 2 * (i % 2):2 * (i % 2) + 1], ss4[:, 2 * (i % 2) + 1:2 * (i % 2) + 2], b_ss4[i % 2]
                    t.dma("sp", x_[:], self.x1s[i * 128:(i + 1) * 128, :], reads=[self.b_x1s], writes=[bx])
                    t.op("act", lambda e: e.activation(out=sq[:], in_=x_[:], func=AF.Square, accum_out=sa), reads=[bx], writes=[b_sq, bss])
                    t.op("act", lambda e: e.activation(out=sb2, in_=sa, func=AF.Sqrt, bias=EPS, scale=1.0 / D), reads=[bss], writes=[bss])
                    t.op("dve", lambda e: e.reciprocal(out=sb2, in_=sb2), reads=[bss], writes=[bss])
                    t.op("dve", lambda e: e.scalar_tensor_tensor(out=hf_[:], in0=x_[:], scalar=sb2, in1=g2row[:], op0=ALU.mult, op1=ALU.mult),
                         reads=[bx, bss, b_g2], writes=[bhf])
                    t.op("pool", lambda e: e.tensor_tensor(out=hf_[:], in0=hf_[:], in1=shfrow[:], op=ALU.add), reads=[bhf, b_shf], writes=[bhf])
                normB(0)
                for i in range(NT):
                    hf, b_hf = hfs[i % 2], b_hfs[i % 2]
                    for j in range(4):
                        for r in range(4):
                            c = 4 * j + r
                            t.op("pe", lambda e, c=c, r=r: e.matmul(ps[j][:, r * 128:(r + 1) * 128],
                                                                    hf[:, c * 128:(c + 1) * 128], ident32[:],
                                                                    start=True, stop=True),
                                 reads=[b_hf, b_id], writes=[pb[j]], inc=(r == 3))
                        t.op("dve", lambda e: e.tensor_copy(out=hT32[:, 4 * j:4 * j + 4, :],
                                                            in_=ps[j][:].rearrange("p (r n) -> p r n", n=128)),
                             reads=[pb[j]], writes=[b_hT32])
                        t.op("act", lambda e: e.activation(out=h2T[:, 4 * j:4 * j + 4, i * 128:(i + 1) * 128],
                                                           in_=hT32[:, 4 * j:4 * j + 4, :], func=AF.Copy),
                             reads=[b_hT32], writes=[b_h2T[i]])
                    for c in range(NKC):
                        t.op("pe", lambda e, c=c: e.matmul(ps[4][:, 0:64], hT32[:, c, :], wr32[:, c, :],
                                                           start=(c == 0), stop=(c == NKC - 1)),
                             reads=[b_hT32, b_wr], writes=[pb[4]], inc=(c == NKC - 1))
                    normB(i + 1)
                    self.routing(t, R, ps[4][:, 0:64], pb[4], rbrow, b_rbr, wn[:, i, :], b_wn[i])
            t.barrier()
            yacc = self.sb("yacc", [128, NT, D], F32, es)
            act = self.sb("actT", [128, 4, NT * 128], BF16, es)
            sil = [self.sb(f"sil{i}", [128, 512], BF16, es) for i in range(2)]
            nyb = 0
            for e_ in range(ne + 1):
                sg, su, sd = (3 * e_) % 4, (3 * e_ + 1) % 4, (3 * e_ + 2) % 4
                wg, wu, wd = ring[sg], ring[su], ring[sd]
                nau = 0
                for half in range(2):
                    hbufs = b_h2T[4 * half:4 * half + 4]
                    for fc in range(4):
                        pa, pu = (nau % 2) * 2, (nau % 2) * 2 + 1
                        nau += 1
                        for w_, slot_, bank in ((wg, sg, pa), (wu, su, pu)):
                            for k in range(NKC):
                                t.op("pe", lambda e, k=k, w_=w_, bank=bank: e.matmul(
                                    ps[bank][:], w_[:, k * 512 + fc * 128:k * 512 + (fc + 1) * 128],
                                    h2T[:, k, half * 512:(half + 1) * 512], start=(k == 0), stop=(k == NKC - 1)),
                                    reads=[rb[slot_]] + hbufs, writes=[pb[bank]], inc=(k == NKC - 1))
                        sl, bsl = sil[nau % 2], b_sil[nau % 2]
                        t.op("act", lambda e: e.activation(out=sl[:], in_=ps[pa][:], func=AF.Silu), reads=[pb[pa]], writes=[bsl])
                        t.op("dve", lambda e: e.tensor_tensor(out=act[:, fc, half * 512:(half + 1) * 512], in0=sl[:],
                                                              in1=ps[pu][:], op=ALU.mult),
                             reads=[bsl, pb[pu]], writes=[b_act[half][fc]])
                state["consumed"] += 2
                pump()
                for i in range(NT):
                    for cg in range(4):
                        bank = 4 + nyb % 4
                        nyb += 1
                        for fc in range(4):
                            t.op("pe", lambda e, fc=fc: e.matmul(ps[bank][:], act[:, fc, i * 128:(i + 1) * 128],
                                                                 wd[:, fc * D + cg * 512:fc * D + (cg + 1) * 512],
                                                                 start=(fc == 0), stop=(fc == 3)),
                                 reads=[rb[sd], b_act[i // 4][fc]], writes=[pb[bank]], inc=(fc == 3))
                        ysl = yacc[:, i, cg * 512:(cg + 1) * 512]
                        wcol = wn[:, i, e_:e_ + 1] if e_ < ne else ones1[:, 0:1]
                        wbuf = b_wn[i] if e_ < ne else b_one
                        if e_ == 0:
                            t.op("dve", lambda e: e.tensor_scalar(out=ysl, in0=ps[bank][:], scalar1=wcol, scalar2=None, op0=ALU.mult),
                                 reads=[pb[bank], wbuf], writes=[b_y[i][cg]])
                        else:
                            t.op("dve", lambda e: e.scalar_tensor_tensor(out=ysl, in0=ps[bank][:], scalar=wcol, in1=ysl,
                                                                         op0=ALU.mult, op1=ALU.add),
                                 reads=[pb[bank], wbuf, b_y[i][cg]], writes=[b_y[i][cg]])
                state["consumed"] += 1
                pump()
            t.barrier()
            with contextlib.ExitStack() as es3:
                r0 = ring[0][:].bitcast(F32)
                r1 = ring[1][:].bitcast(F32)
                gfrow = r0[:, 0:D]
                norow = r0[:, D:2 * D]
                xt = [r1[:, 0:D], r1[:, D:2 * D]]
                sq = ring[2][:, 0:D]
                ss = self.sb("ssC", [128, 2 * NT], F32, es3)
                b_gf, b_no = Buf("gfrow"), Buf("norow")
                b_xt = [Buf("xtC0"), Buf("xtC1")]
                b_sq = Buf("sqC")
                b_ss = [Buf(f"ssC{i}") for i in range(NT)]
                t.dma("sp", gfrow[:], self.modrows[5:6, :].partition_broadcast(128), reads=[self.b_modrows[5]], writes=[b_gf])
                t.dma("sp", norow[:], I["norm_out"][0:1, :].partition_broadcast(128), writes=[b_no])
                outs = []

                def load_x1(i):
                    if i < NT:
                        t.dma("sp", xt[i % 2][:], self.x1s[i * 128:(i + 1) * 128, :], reads=[self.b_x1s], writes=[b_xt[i % 2]])
                load_x1(0)
                load_x1(1)
                for i in range(NT):
                    x_, bx = xt[i % 2], b_xt[i % 2]
                    yb = b_y[i]
                    t.op("pool", lambda e: e.tensor_tensor(out=yacc[:, i, :], in0=yacc[:, i, :], in1=gfrow[:], op=ALU.mult),
                         reads=yb + [b_gf], writes=yb)
                    t.op("dve", lambda e: e.tensor_tensor(out=yacc[:, i, :], in0=yacc[:, i, :], in1=x_[:], op=ALU.add),
                         reads=yb + [bx], writes=yb)
                    t.op("act", lambda e: e.activation(out=sq[:], in_=yacc[:, i, :], func=AF.Square, accum_out=ss[:, 2 * i:2 * i + 1]),
                         reads=yb, writes=[b_sq, b_ss[i]])
                    t.op("act", lambda e: e.activation(out=ss[:, 2 * i + 1:2 * i + 2], in_=ss[:, 2 * i:2 * i + 1], func=AF.Sqrt,
                                                       bias=EPS, scale=1.0 / D), reads=[b_ss[i]], writes=[b_ss[i]])
                    t.op("dve", lambda e: e.reciprocal(out=ss[:, 2 * i + 1:2 * i + 2], in_=ss[:, 2 * i + 1:2 * i + 2]),
                         reads=[b_ss[i]], writes=[b_ss[i]])
                    t.op("dve", lambda e: e.scalar_tensor_tensor(out=yacc[:, i, :], in0=yacc[:, i, :], scalar=ss[:, 2 * i + 1:2 * i + 2],
                                                                 in1=norow[:], op0=ALU.mult, op1=ALU.mult),
                         reads=yb + [b_ss[i], b_no], writes=yb)
                    bo = Buf(f"out{i}")
                    t.dma("sp", self.out[i * 128:(i + 1) * 128, :], yacc[:, i, :], reads=yb, writes=[bo])
                    outs.append(bo)
                    load_x1(i + 2)
                t.drain("sp", outs)
                t.barrier()

    def phaseA(self, t, ps, pb, ring, rb, ident32, b_id):
        nc = self.nc
        I = self.ins
        SCALE = 128.0 ** -0.5
        PI = float(np.pi)
        psb = [p_[:].bitcast(BF16) for p_ in ps]
        hTs = nc.dram_tensor("hTs", [NSLOT_T, 128, D], BF16, kind="Internal").ap()
        b_hTs = [Buf(f"hTs{i}") for i in range(NSLOT_T)]
        hTt = [ring[3][:, q * D:(q + 1) * D] for q in range(4)]
        b_hTt = [Buf(f"hTt{q}") for q in range(4)]

        def load_hT(tile):
            if 0 <= tile < NSLOT_T:
                t.dma("sp", hTt[tile % 4], hTs[tile], reads=[b_hTs[tile]], writes=[b_hTt[tile % 4]])

        def project(tile, wslot, bslot, ncols, bank):
            for c in range(NKC):
                t.op("pe", lambda e, c=c: e.matmul(ps[bank][:, 0:ncols], hTt[tile % 4][:, c * 128:(c + 1) * 128],
                                                   wslot[:, c * 512:c * 512 + ncols], start=(c == 0), stop=(c == NKC - 1)),
                     reads=[b_hTt[tile % 4], bslot], writes=[pb[bank]], inc=(c == NKC - 1))

        def load_w(slot, col0, ncols=512):
            t.dma("pool", ring[slot][:].rearrange("p (k n) -> p k n", n=512)[:, :, 0:ncols],
                  I["w_in"][:, col0:col0 + ncols].rearrange("(k p) n -> p k n", p=128), writes=[rb[slot]], sem=slot)

        with contextlib.ExitStack() as es:
            OT = self.sb("OT", [128, NKC, NT * 128], BF16, es)
            b_OT = [Buf(f"OT{i}") for i in range(NT)]
            identb = self.sb("identb", [128, 128], BF16, es)
            onesb = self.sb("onesb", [128, 128], BF16, es)
            posi = self.sb("posi", [128, NSLOT_T], mybir.dt.int32, es)
            posf = self.sb("posf", [128, NSLOT_T], F32, es)
            vmask = self.sb("vmask", [128, NSLOT_T], F32, es)
            b_idb, b_ones, b_pos, b_vm = (Buf(n) for n in ("identb", "onesb", "pos", "vmask"))
            t.op("dve", lambda e: e.tensor_copy(out=identb[:], in_=ident32[:]), reads=[b_id], writes=[b_idb])
            t.op("dve", lambda e: e.memset(onesb[:], 1.0), writes=[b_ones])
            t.dma("sp", posi[:], I["pos_i"][:, :], writes=[b_pos])
            t.op("dve", lambda e: e.tensor_copy(out=posf[:], in_=posi[:]), reads=[b_pos], writes=[b_pos])
            t.dma("sp", vmask[:], I["vmask"][:, :], writes=[b_vm])

            with contextlib.ExitStack() as es0:
                p0 = self.phase0_gen(t, ps, pb, ring, rb, es0)
                for _ in range(8):
                    next(p0)
                g1row = self.sb("g1row", [128, D], F32, es0)
                sharow = self.sb("sharow", [128, D], F32, es0)
                xt = [self.sb(f"xtA{i}", [128, D], F32, es0) for i in range(3)]
                hb = [self.sb(f"hbA{i}", [128, D], BF16, es0) for i in range(2)]
                hst = [self.sb(f"hstA{i}", [128, D], BF16, es0) for i in range(2)]
                sq = self.sb("sqA", [128, D], BF16, es0)
                ssA = self.sb("ssA", [128, 4], F32, es0)
                b_g1, b_sha, b_sq = Buf("g1row"), Buf("sharow"), Buf("sq")
                b_xt = [Buf("xtA0"), Buf("xtA1"), Buf("xtA2")]
                b_hb = [Buf("hb0"), Buf("hb1")]
                b_hst = [Buf("hst0"), Buf("hst1")]

                def load_x(tile):
                    if tile < NSLOT_T:
                        t.dma("sp", xt[tile % 3][:], I["xs"][tile * 128:(tile + 1) * 128, :], writes=[b_xt[tile % 3]])
                b_ss = [Buf("ssA0"), Buf("ssA1")]
                t.dma("sp", g1row[:], self.modrows[1:2, :].partition_broadcast(128), reads=[self.b_modrows[1]], writes=[b_g1])
                t.dma("sp", sharow[:], self.modrows[0:1, :].partition_broadcast(128), reads=[self.b_modrows[0]], writes=[b_sha])
                load_x(0)
                load_x(1)
                for tile in range(NSLOT_T):
                    if tile % 2 == 1:
                        next(p0, None)
                    k2 = tile % 2
                    x_, bx = xt[tile % 3], b_xt[tile % 3]
                    sa, sb2 = ssA[:, 2 * k2:2 * k2 + 1], ssA[:, 2 * k2 + 1:2 * k2 + 2]
                    t.op("act", lambda e: e.activation(out=sq[:], in_=x_[:], func=AF.Square, accum_out=sa), reads=[bx], writes=[b_sq, b_ss[k2]])
                    t.op("act", lambda e: e.activation(out=sb2, in_=sa, func=AF.Sqrt, bias=EPS, scale=1.0 / D), reads=[b_ss[k2]], writes=[b_ss[k2]])
                    t.op("dve", lambda e: e.reciprocal(out=sb2, in_=sb2), reads=[b_ss[k2]], writes=[b_ss[k2]])
                    t.op("dve", lambda e: e.scalar_tensor_tensor(out=x_[:], in0=x_[:], scalar=sb2, in1=g1row[:], op0=ALU.mult, op1=ALU.mult),
                         reads=[bx, b_ss[k2], b_g1], writes=[bx])
                    t.op("dve", lambda e: e.tensor_tensor(out=hb[k2][:], in0=x_[:], in1=sharow[:], op=ALU.add),
                         reads=[bx, b_sha], writes=[b_hb[k2]])
                    for j in range(2):
                        bank = 2 * k2 + j
                        for r in range(8):
                            c = 8 * j + r
                            t.op("pe", lambda e, c=c, r=r: e.transpose(psb[bank][:, r * 128:(r + 1) * 128], hb[k2][:, c * 128:(c + 1) * 128], identb[:]),
                                 reads=[b_hb[k2], b_idb], writes=[pb[bank]], inc=(r == 7))
                    t.op("act", lambda e: e.activation(out=hst[k2][:, 0:1024], in_=psb[2 * k2][:], func=AF.Copy), reads=[pb[2 * k2]], writes=[b_hst[k2]])
                    t.op("dve", lambda e: e.tensor_copy(out=hst[k2][:, 1024:2048], in_=psb[2 * k2 + 1][:]), reads=[pb[2 * k2 + 1]], writes=[b_hst[k2]])
                    load_x(tile + 2)
                    t.dma("sp", hTs[tile], hst[k2][:], reads=[b_hst[k2]], writes=[b_hTs[tile]])
                for _ in p0:
                    pass
                t.barrier()

            def sincos(ang, n, sin_out, cos_out, b_ang, b_out):
                kf = self._sc_tmp[:, 0:n]
                ki = self._sc_ki[:, 0:n]
                bt = self._b_sc
                t.op("dve", lambda e: e.tensor_scalar(out=cos_out, in0=ang, scalar1=0.5 * PI, scalar2=None, op0=ALU.add),
                     reads=[b_ang], writes=[b_out])
                for r_, br in ((cos_out, b_out), (ang, b_ang)):
                    t.op("dve", lambda e: e.tensor_scalar(out=kf, in0=r_, scalar1=1.0 / (2 * PI), scalar2=None, op0=ALU.mult), reads=[br], writes=[bt])
                    t.op("dve", lambda e: e.tensor_copy(out=ki, in_=kf), reads=[bt], writes=[bt])
                    t.op("dve", lambda e: e.tensor_copy(out=kf, in_=ki), reads=[bt], writes=[bt])
                    t.op("dve", lambda e: e.scalar_tensor_tensor(out=r_, in0=kf, scalar=-2 * PI, in1=r_, op0=ALU.mult, op1=ALU.add),
                         reads=[bt, br], writes=[br])
                    t.op("dve", lambda e: e.tensor_scalar(out=kf, in0=r_, scalar1=PI, scalar2=-2 * PI, op0=ALU.is_gt, op1=ALU.mult), reads=[br], writes=[bt])
                    t.op("dve", lambda e: e.tensor_tensor(out=r_, in0=r_, in1=kf, op=ALU.add), reads=[bt, br], writes=[br])
                    t.op("dve", lambda e: e.tensor_scalar(out=r_, in0=r_, scalar1=-PI, scalar2=PI, op0=ALU.max, op1=ALU.min), reads=[br], writes=[br])
                t.op("act", lambda e: e.activation(out=cos_out, in_=cos_out, func=AF.Sin), reads=[b_out], writes=[b_out])
                t.op("act", lambda e: e.activation(out=sin_out, in_=ang, func=AF.Sin), reads=[b_ang], writes=[b_out])

            self._sc_ki = self.sb("sc_ki", [128, 1024], mybir.dt.int32, es)
            self._sc_tmp = self.sb("sc_tmp", [128, 1024], F32, es)
            self._b_sc = Buf("sc_tmp")

            NH = 4
            with contextlib.ExitStack() as esm:
                cosA = self.sb("cosA", [128, NSLOT_T, 16], F32, esm)
                sinA = self.sb("sinA", [128, NSLOT_T, 16], F32, esm)
                angA = self.sb("angA", [128, NSLOT_T, 16], F32, esm)
                invfA = self.sb("invfA", [128, 16], F32, esm)
                KT = self.sb("KT", [128, NH, NSLOT_T * 128], BF16, esm)
                V = self.sb("Vm", [128, NSLOT_T, NH * 128], BF16, esm)
                QT = self.sb("QT", [128, NH, NT * 128], BF16, esm)
                TT = self.sb("TT", [16, NH, NT * 128], BF16, esm)
                kbs = [self.sb(f"kbm{i}", [128, NH, 128], BF16, esm) for i in range(2)]
                tm1 = self.sb("tm1", [128, NH, 16], F32, esm)
                tm2 = self.sb("tm2", [128, NH, 16], F32, esm)
                kmT = self.sb("kmT", [128, NH, 16], F32, esm)
                kmTb = self.sb("kmTb", [128, NH, 16], BF16, esm)
                gbias = self.sb("gbias", [128, 4, 16], F32, esm)
                diag = self.sb("diag", [128, 4, 16], F32, esm)
                oh16 = self.sb("oh16", [16, 16, 128], BF16, esm)
                causal = self.sb("causal", [128, 4, 512], BF16, esm)
                gm = self.sb("gmA", [128, NH, 16], F32, esm)
                t8 = self.sb("t8A", [128, NH, 8], F32, esm)
                thr = self.sb("thrA", [128, NH], F32, esm)
                sel = self.sb("selA", [128, NH, 16], F32, esm)
                selb = self.sb("selbA", [128, NH, 16], BF16, esm)
                Pb = [self.sb(f"Pb{i}", [128, 512], BF16, esm) for i in range(3)]
                rl = self._sc_tmp[:, 0:512]
                b_cs, b_ang, b_ifa = Buf("cosinA"), Buf("angA"), Buf("invfA")
                b_KT = [Buf(f"KT{i}") for i in range(NSLOT_T)]
                b_V = [Buf(f"V{i}") for i in range(NSLOT_T)]
                b_QT = [Buf(f"QT{i}") for i in range(NT)]
                b_TT = [Buf(f"TT{i}") for i in range(NT)]
                b_kbs = [Buf("kb0"), Buf("kb1")]
                b_tm1, b_tm2, b_km, b_kmb = (Buf(n) for n in ("tm1", "tm2", "kmT", "kmTb"))
                b_gb, b_dg, b_oh, b_ca, b_gm, b_t8, b_thr, b_sel, b_selb, b_rl = (
                    Buf(n) for n in ("gbias", "diag", "oh16", "causal", "gm", "t8", "thr", "sel", "selb", "rl"))
                b_P = [Buf(f"P{i}") for i in range(3)]
                t.dma("sp", invfA[:], I["invfA"][:, :], writes=[b_ifa])
                t.dma("sp", gbias[:], I["gatebias"][:, :, :], writes=[b_gb])
                t.dma("sp", diag[:], I["diag"][:, :, :], writes=[b_dg])
                t.dma("pool", oh16[:], I["oh16"][:, :, :], writes=[b_oh])
                t.dma("pool", causal[:], I["causal"][:, :, :], writes=[b_ca])
                t.op("dve", lambda e: e.tensor_tensor(out=angA[:], in0=posf[:].unsqueeze(2).broadcast_to([128, NSLOT_T, 16]),
                                                      in1=invfA[:].unsqueeze(1).broadcast_to([128, NSLOT_T, 16]), op=ALU.mult),
                     reads=[b_pos, b_ifa], writes=[b_ang])
                sincos(angA[:].rearrange("p a b -> p (a b)"), NSLOT_T * 16, sinA[:].rearrange("p a b -> p (a b)"),
                       cosA[:].rearrange("p a b -> p (a b)"), b_ang, b_cs)

                def rotaryA(bank, tile, dst, b_dst):
                    src3 = ps[bank][:].rearrange("p (h d) -> p h d", d=128)
                    cb = cosA[:, tile, :].unsqueeze(1).broadcast_to([128, NH, 16])
                    sn = sinA[:, tile, :].unsqueeze(1).broadcast_to([128, NH, 16])
                    x1, x2 = src3[:, :, 0:16], src3[:, :, 16:32]
                    t.op("dve", lambda e: e.tensor_tensor(out=tm1[:], in0=x1, in1=cb, op=ALU.mult), reads=[pb[bank], b_cs], writes=[b_tm1])
                    t.op("dve", lambda e: e.tensor_tensor(out=tm2[:], in0=x2, in1=sn, op=ALU.mult), reads=[pb[bank], b_cs], writes=[b_tm2])
                    t.op("dve", lambda e: e.tensor_tensor(out=dst[:, :, 0:16], in0=tm1[:], in1=tm2[:], op=ALU.subtract),
                         reads=[b_tm1, b_tm2], writes=[b_dst])
                    t.op("dve", lambda e: e.tensor_tensor(out=tm1[:], in0=x2, in1=cb, op=ALU.mult), reads=[pb[bank], b_cs], writes=[b_tm1])
                    t.op("dve", lambda e: e.tensor_tensor(out=tm2[:], in0=x1, in1=sn, op=ALU.mult), reads=[pb[bank], b_cs], writes=[b_tm2])
                    t.op("dve", lambda e: e.tensor_tensor(out=dst[:, :, 16:32], in0=tm1[:], in1=tm2[:], op=ALU.add),
                         reads=[b_tm1, b_tm2], writes=[b_dst])
                    t.op("dve", lambda e: e.tensor_copy(out=dst[:, :, 32:128], in_=src3[:, :, 32:128]), reads=[pb[bank]], writes=[b_dst])

                def transp4(src, b_src, dstT, b_dstT):
                    for h in range(NH):
                        t.op("pe", lambda e, h=h: e.transpose(psb[4][:, h * 128:(h + 1) * 128], src[:, h, :], identb[:]),
                             reads=[b_src, b_idb], writes=[pb[4]], inc=(h == NH - 1))
                    t.op("act", lambda e: e.activation(out=dstT, in_=psb[4][:, 0:NH * 128].rearrange("p (h n) -> p h n", n=128), func=AF.Copy),
                         reads=[pb[4]], writes=[b_dstT])

                def load_moba_w(p):
                    load_w(0, 1024 + 512 * p)
                    load_w(1, 2048 + 512 * p)
                    load_w(2, 512 * p)
                load_moba_w(0)
                for p in range(2):
                    for q in range(3):
                        load_hT(q)

                    def post(tile):
                        k2 = tile % 2
                        rotaryA(k2, tile, kbs[0], b_kbs[0])
                        t.op("act", lambda e: e.activation(out=V[:, tile, :], in_=ps[2 + k2][:], func=AF.Copy), reads=[pb[2 + k2]], writes=[b_V[tile]])
                        transp4(kbs[0], b_kbs[0], KT[:, :, tile * 128:(tile + 1) * 128], b_KT[tile])
                        if tile >= 24:
                            i = tile - 24
                            rotaryA(5 + k2, tile, kbs[1], b_kbs[1])
                            transp4(kbs[1], b_kbs[1], QT[:, :, i * 128:(i + 1) * 128], b_QT[i])
                        if tile % 8 == 7:
                            sg = tile // 8
                            t.op("dve", lambda e: e.tensor_reduce(out=kmT[:, :, 4 * sg:4 * sg + 4],
                                                                  in_=KT[:, :, sg * 1024:(sg + 1) * 1024].rearrange("p h (b k) -> p h b k", k=256),
                                                                  axis=AX.X, op=ALU.add),
                                 reads=b_KT[sg * 8:(sg + 1) * 8], writes=[b_km])

                    for tile in range(NSLOT_T):
                        k2 = tile % 2
                        load_hT(tile + 3)
                        project(tile, ring[0], rb[0], 512, k2)
                        project(tile, ring[1], rb[1], 512, 2 + k2)
                        if tile >= 24:
                            project(tile, ring[2], rb[2], 512, 5 + k2)
                        if tile >= 1:
                            post(tile - 1)
                    post(NSLOT_T - 1)
                    t.op("dve", lambda e: e.tensor_scalar(out=kmTb[:], in0=kmT[:], scalar1=1.0 / 256, scalar2=None, op0=ALU.mult),
                         reads=[b_km], writes=[b_kmb])
                    if p == 0:
                        load_moba_w(1)
                    else:
                        load_w(0, 4096)
                        load_w(1, 5120)
                        load_w(2, 3072)
                    for i in range(NT):
                        r = i // 2
                        for h in range(NH):
                            t.op("pe", lambda e, h=h: e.matmul(ps[4][:, h * 16:(h + 1) * 16], QT[:, h, i * 128:(i + 1) * 128], kmTb[:, h, :],
                                                               start=True, stop=True),
                                 reads=[b_QT[i], b_kmb], writes=[pb[4]], inc=(h == NH - 1))
                        t.op("dve", lambda e: e.tensor_tensor(out=gm[:], in0=ps[4][:, 0:NH * 16].rearrange("p (h n) -> p h n", n=16),
                                                              in1=gbias[:, r, :].unsqueeze(1).broadcast_to([128, NH, 16]), op=ALU.add),
                             reads=[pb[4], b_gb], writes=[b_gm])
                        for h in range(NH):
                            t.op("dve", lambda e, h=h: e.max(out=t8[:, h, :], in_=gm[:, h, :]), reads=[b_gm], writes=[b_t8])
                        t.op("dve", lambda e: e.tensor_scalar(out=thr[:], in0=t8[:, :, 2], scalar1=-1e29, scalar2=None, op0=ALU.max),
                             reads=[b_t8], writes=[b_thr])
                        t.op("dve", lambda e: e.tensor_tensor(out=sel[:], in0=gm[:], in1=thr[:].unsqueeze(2).broadcast_to([128, NH, 16]),
                                                              op=ALU.is_ge), reads=[b_gm, b_thr], writes=[b_sel])
                        t.op("dve", lambda e: e.tensor_tensor(out=sel[:], in0=sel[:], in1=diag[:, r, :].unsqueeze(1).broadcast_to([128, NH, 16]),
                                                              op=ALU.max), reads=[b_sel, b_dg], writes=[b_sel])
                        t.op("dve", lambda e: e.tensor_scalar(out=selb[:], in0=sel[:], scalar1=-1.0, scalar2=30000.0, op0=ALU.add, op1=ALU.mult),
                             reads=[b_sel], writes=[b_selb])
                        for h in range(NH):
                            t.op("pe", lambda e, h=h: e.transpose(psb[4][0:16, 512 + h * 128:512 + (h + 1) * 128], selb[:, h, :], identb[:]),
                                 reads=[b_selb, b_idb], writes=[pb[4]], inc=(h == NH - 1))
                        t.op("act", lambda e: e.activation(out=TT[:, :, i * 128:(i + 1) * 128],
                                                           in_=psb[4][0:16, 512:512 + NH * 128].rearrange("p (h n) -> p h n", n=128), func=AF.Copy),
                             reads=[pb[4]], writes=[b_TT[i]])
                    nS = 0
                    for h in range(NH):
                        for half in range(2):
                            qsl = slice(half * 512, (half + 1) * 512)
                            bq = b_QT[4 * half:4 * half + 4]
                            btt = b_TT[4 * half:4 * half + 4]
                            nkt = 28 + 4 * half
                            ob, lb = (5, 6) if (2 * h + half) % 2 == 0 else (7, 4)
                            def emit_pv(kt, Pt, bP):
                                t.op("pe", lambda e: e.matmul(ps[ob][:], V[:, kt, h * 128:(h + 1) * 128], Pt[:], start=(kt == 0), stop=(kt == nkt - 1)),
                                     reads=[b_V[kt], bP], writes=[pb[ob]], inc=False)
                                t.op("pe", lambda e: e.matmul(ps[lb][:], onesb[:], Pt[:], start=(kt == 0), stop=(kt == nkt - 1)),
                                     reads=[b_ones, bP], writes=[pb[lb]], inc=True)
                            pend = None
                            for kt in range(nkt):
                                sb_ = nS % 4
                                Pt, bP = Pb[nS % 3], b_P[nS % 3]
                                nS += 1
                                dg = kt >= 24 + 4 * half
                                t.op("pe", lambda e: e.matmul(ps[sb_][:], KT[:, h, kt * 128:(kt + 1) * 128], QT[:, h, qsl], start=True, stop=False),
                                     reads=[b_KT[kt]] + bq, writes=[pb[sb_]], inc=False)
                                t.op("pe", lambda e: e.matmul(ps[sb_][:], oh16[:, kt // 2, :], TT[:, h, qsl], start=False, stop=not dg),
                                     reads=[b_oh] + btt, writes=[pb[sb_]], inc=not dg)
                                if dg:
                                    t.op("pe", lambda e: e.matmul(ps[sb_][:], identb[:], causal[:, kt - 24 - 4 * half, :], start=False, stop=True),
                                         reads=[b_idb, b_ca], writes=[pb[sb_]], inc=True)
                                t.op("act", lambda e: e.activation(out=Pt[:], in_=ps[sb_][:], func=AF.Exp, scale=SCALE), reads=[pb[sb_]], writes=[bP])
                                if pend is not None:
                                    emit_pv(*pend)
                                pend = (kt, Pt, bP)
                            emit_pv(*pend)
                            t.op("dve", lambda e: e.reciprocal(out=rl[:], in_=ps[lb][:]), reads=[pb[lb]], writes=[b_rl])
                            t.op("dve", lambda e: e.tensor_tensor(out=OT[:, 4 * p + h, qsl], in0=ps[ob][:], in1=rl[:], op=ALU.mult),
                                 reads=[pb[ob], b_rl], writes=b_OT[4 * half:4 * half + 4])
                t.barrier()

            with contextlib.ExitStack() as esr:
                cosRs = [self.sb(f"cosR{i}", [128, 8, 128], F32, esr) for i in range(2)]
                sinRs = [self.sb(f"sinR{i}", [128, 8, 128], F32, esr) for i in range(2)]
                rtabs = nc.dram_tensor("rtabs", [4, 2, 128, 1024], F32, kind="Internal").ap()
                b_rtabs = [Buf(f"rtabs{i}") for i in range(4)]
                angR = self.sb("angR", [128, 8, 128], F32, esr)
                invfR = self.sb("invfR", [128, 128], F32, esr)
                kB = self.sb("kB", [128, 8, 512], BF16, esr)
                vB = self.sb("vB", [128, 8, 512], BF16, esr)
                qB = self.sb("qB", [128, 8, 512], BF16, esr)
                gB = self.sb("gB", [128, 8, 512], BF16, esr)
                Sf = self.sb("Sf", [128, 2, 512], F32, esr)
                Sb = self.sb("Sb", [128, 2, 512], BF16, esr)
                rtbl = self.sb("rtbl", [128, 4, 128], F32, esr)
                rxi = self.sb("rxi", [128, 4], F32, esr)
                rkz = self.sb("rkz", [128, 4], F32, esr)
                r1 = self.sb("r1", [128, 2, 128], F32, esr)
                r2 = self.sb("r2", [128, 2, 128], F32, esr)
                r3 = self.sb("r3", [128, 2, 128], F32, esr)
                r4 = self.sb("r4", [128, 2, 128], F32, esr)
                kqTs = [self.sb(f"kqT{i}", [128, 8, 128], BF16, esr) for i in range(2)]
                ATps = [self.sb(f"ATp{i}", [128, 2, 128], BF16, esr) for i in range(2)]
                osb = self.sb("osb", [128, 512], F32, esr)
                osq = self.sb("osq", [128, 512], BF16, esr)
                onb = self.sb("onb", [128, 512], BF16, esr)
                Kz = self.sb("Kz", [128, 512], BF16, esr)
                st = self.sb("stR", [128, 5, 2], F32, esr)
                b_csrs, b_angr, b_ifr = [Buf("cosinR0"), Buf("cosinR1")], Buf("angR"), Buf("invfR")
                b_kB = [Buf(f"kB{i}") for i in range(8)]
                b_vB = [Buf(f"vB{i}") for i in range(8)]
                b_qB = [Buf(f"qB{i}") for i in range(8)]
                b_gB = [Buf(f"gB{i}") for i in range(8)]
                b_Sf = [Buf("Sf0"), Buf("Sf1")]
                b_Sb = [Buf("Sb0"), Buf("Sb1")]
                b_rc = Buf("retconst")
                b_r = [Buf(f"r{i}") for i in range(4)]
                b_osb, b_osq, b_onb, b_Kz, b_st = (Buf(n) for n in ("osb", "osq", "onb", "Kz", "stR"))
                b_kqTs = [Buf("kqT0"), Buf("kqT1")]
                b_ATps = [Buf("ATp0"), Buf("ATp1")]
                t.dma("sp", invfR[:], I["invfR"][:, :], writes=[b_ifr])
                t.dma("sp", rtbl[:], I["rettbl"][:, :, :], writes=[b_rc])
                t.dma("sp", rxi[:], I["retxi"][:, :], writes=[b_rc])
                t.dma("sp", rkz[:], I["retkz"][:, :], writes=[b_rc])
                gam = [1.0 - 2.0 ** (-5 - h) for h in range(4)]

                def make_tables(rp, sg):
                    if sg > 3:
                        return
                    cR, sR, bcs = cosRs[sg % 2], sinRs[sg % 2], b_csrs[sg % 2]
                    cf, sf = cR[:].rearrange("p a b -> p (a b)"), sR[:].rearrange("p a b -> p (a b)")
                    if rp == 0:
                        t.op("dve", lambda e: e.tensor_tensor(out=angR[:], in0=posf[:, sg * 8:(sg + 1) * 8].unsqueeze(2).broadcast_to([128, 8, 128]),
                                                              in1=invfR[:].unsqueeze(1).broadcast_to([128, 8, 128]), op=ALU.mult),
                             reads=[b_pos, b_ifr], writes=[b_angr])
                        sincos(angR[:].rearrange("p a b -> p (a b)"), 1024, sf, cf, b_angr, bcs)
                        t.dma("sp", rtabs[sg, 0], cf, reads=[bcs], writes=[b_rtabs[sg]])
                        t.dma("sp", rtabs[sg, 1], sf, reads=[bcs], writes=[b_rtabs[sg]])
                    else:
                        t.dma("sp", cf, rtabs[sg, 0], reads=[b_rtabs[sg]], writes=[bcs])
                        t.dma("sp", sf, rtabs[sg, 1], reads=[b_rtabs[sg]], writes=[bcs])

                def rotaryR(bank, tile_in_group, dst, b_dst, sg):
                    cosR, sinR, b_csr = cosRs[sg % 2], sinRs[sg % 2], b_csrs[sg % 2]
                    src = ps[bank][:].rearrange("p (h s d) -> p h s d", s=2, d=128)
                    d4 = dst.rearrange("p (h s d) -> p h s d", s=2, d=128)
                    cb = cosR[:, tile_in_group, :].unsqueeze(1).broadcast_to([128, 2, 128])
                    sn = sinR[:, tile_in_group, :].unsqueeze(1).broadcast_to([128, 2, 128])
                    x1, x2 = src[:, :, 0, :], src[:, :, 1, :]
                    t.op("dve", lambda e: e.tensor_tensor(out=r1[:], in0=x1, in1=cb, op=ALU.mult), reads=[pb[bank], b_csr], writes=[b_r[0]])
                    t.op("dve", lambda e: e.tensor_tensor(out=r2[:], in0=x2, in1=sn, op=ALU.mult), reads=[pb[bank], b_csr], writes=[b_r[1]])
                    t.op("dve", lambda e: e.tensor_tensor(out=r3[:], in0=x2, in1=cb, op=ALU.mult), reads=[pb[bank], b_csr], writes=[b_r[2]])
                    t.op("dve", lambda e: e.tensor_tensor(out=r4[:], in0=x1, in1=sn, op=ALU.mult), reads=[pb[bank], b_csr], writes=[b_r[3]])
                    t.op("pool", lambda e: e.tensor_tensor(out=d4[:, :, 0, :], in0=r1[:], in1=r2[:], op=ALU.subtract),
                         reads=[b_r[0], b_r[1]], writes=[b_dst])
                    t.op("pool", lambda e: e.tensor_tensor(out=d4[:, :, 1, :], in0=r3[:], in1=r4[:], op=ALU.add),
                         reads=[b_r[2], b_r[3]], writes=[b_dst])

                for rp in range(2):
                    t.op("dve", lambda e: e.memset(Sf[:], 0.0), writes=b_Sf)
                    t.op("dve", lambda e: e.memset(Sb[:], 0.0), writes=b_Sb)
                    for q in range(3):
                        load_hT(q)
                    make_tables(rp, 0)
                    for sg in range(4):

                        def postkv(i, tile):
                            k2 = tile % 2
                            rotaryR(k2, i, kB[:, i, :], b_kB[i], sg)
                            t.op("act", lambda e: e.activation(out=vB[:, i, :], in_=ps[2 + k2][:], func=AF.Copy, scale=vmask[:, tile:tile + 1]),
                                 reads=[pb[2 + k2], b_vm], writes=[b_vB[i]])

                        def postqg(i, tile):
                            k2 = tile % 2
                            rotaryR(k2, i, qB[:, i, :], b_qB[i], sg)
                            t.op("act", lambda e: e.activation(out=gB[:, i, :], in_=ps[2 + k2][:], func=AF.Silu), reads=[pb[2 + k2]], writes=[b_gB[i]])

                        for i in range(8):
                            tile = sg * 8 + i
                            k2 = tile % 2
                            load_hT(tile + 3 if not (sg == 3 and i >= 5) else -1)
                            project(tile, ring[0], rb[0], 512, k2)
                            project(tile, ring[1], rb[1], 512, 2 + k2)
                            if i >= 1:
                                postkv(i - 1, tile - 1)
                            if i == 3:
                                make_tables(rp, sg + 1)
                        postkv(7, sg * 8 + 7)
                        if sg == 3:
                            load_w(0, 6144 + 512 * rp)
                            for q in range(3):
                                load_hT(24 + q)
                            for i in range(8):
                                tile = 24 + i
                                k2 = tile % 2
                                load_hT(tile + 3)
                                project(tile, ring[2], rb[2], 512, k2)
                                project(tile, ring[0], rb[0], 512, 2 + k2)
                                if i >= 1:
                                    postqg(i - 1, tile - 1)
                            postqg(7, 31)
                            if rp == 0:
                                load_w(0, 4096 + 512)
                                load_w(1, 5120 + 512)
                                load_w(2, 3072 + 512)
                            else:
                                for cg in range(4):
                                    t.dma("pool", ring[cg][:].rearrange("p (k n) -> p k n", n=512),
                                          I["w_out"][:, cg * 512:(cg + 1) * 512].rearrange("(k p) n -> p k n", p=128),
                                          writes=[rb[cg]] + (b_hTt if cg == 3 else []), sem=cg)
                        h0 = 2 * rp

                        def partA(i):
                            for hh in range(2):
                                for q_, (src_, bsrc) in enumerate(((kB, b_kB[i]), (qB, b_qB[i]))):
                                    for dc in range(2):
                                        col = (4 * hh + 2 * q_ + dc) * 128
                                        t.op("pe", lambda e, col=col, dc=dc, src_=src_, hh=hh: e.transpose(
                                            psb[4][:, col:col + 128], src_[:, i, hh * 256 + dc * 128:hh * 256 + (dc + 1) * 128], identb[:]),
                                            reads=[bsrc, b_idb], writes=[pb[4]], inc=(hh == 1 and q_ == 1 and dc == 1))
                            t.op("act", lambda e: e.activation(out=kqTs[i % 2][:], in_=psb[4][:].rearrange("p (a n) -> p a n", n=128), func=AF.Copy),
                                 reads=[pb[4]], writes=[b_kqTs[i % 2]])
                            for hh in range(2):
                                for dc in range(2):
                                    t.op("pe", lambda e, dc=dc, hh=hh: e.matmul(ps[5][:, hh * 128:(hh + 1) * 128], kqTs[i % 2][:, 4 * hh + dc, :], kqTs[i % 2][:, 4 * hh + 2 + dc, :],
                                                                                start=(dc == 0), stop=(dc == 1)),
                                         reads=[b_kqTs[i % 2]], writes=[pb[5]], inc=(hh == 1 and dc == 1))
                            t.op("dve", lambda e: e.tensor_tensor(out=ATps[i % 2][:], in0=ps[5][:, 0:256].rearrange("p (a n) -> p a n", n=128),
                                                                  in1=rtbl[:, h0:h0 + 2, :], op=ALU.mult),
                                 reads=[pb[5], b_rc], writes=[b_ATps[i % 2]])

                        def partB(i):
                            for hh in range(2):
                                hs = slice(hh * 256, (hh + 1) * 256)
                                t.op("pe", lambda e: e.matmul(ps[6][:, hs], ATps[i % 2][:, hh, :], vB[:, i, hs], start=True, stop=False),
                                     reads=[b_ATps[i % 2], b_vB[i]], writes=[pb[6]], inc=False)
                                for dc in range(2):
                                    t.op("pe", lambda e, dc=dc: e.matmul(ps[6][:, hs], kqTs[i % 2][:, 4 * hh + 2 + dc, :], Sb[:, hh, dc * 256:(dc + 1) * 256],
                                                                         start=False, stop=(dc == 1)),
                                         reads=[b_kqTs[i % 2], b_Sb[hh]], writes=[pb[6]], inc=(dc == 1))
                            for hh in range(2):
                                hs = slice(hh * 256, (hh + 1) * 256)
                                t.op("act", lambda e: e.activation(out=osb[:, hs], in_=ps[6][:, hs], func=AF.Copy, scale=rxi[:, h0 + hh:h0 + hh + 1],
                                                                   accum_out=st[:, 0, hh:hh + 1]), reads=[pb[6], b_rc], writes=[b_osb, b_st])
                            for hh in range(2):
                                hs = slice(hh * 256, (hh + 1) * 256)
                                t.op("act", lambda e: e.activation(out=osq[:, hs], in_=osb[:, hs], func=AF.Square, accum_out=st[:, 1, hh:hh + 1]),
                                     reads=[b_osb], writes=[b_osq, b_st])
                            t.op("dve", lambda e: e.tensor_scalar(out=st[:, 2, :], in0=st[:, 0, :], scalar1=1.0 / 256, scalar2=None, op0=ALU.mult),
                                 reads=[b_st], writes=[b_st])
                            t.op("dve", lambda e: e.tensor_tensor(out=st[:, 3, :], in0=st[:, 2, :], in1=st[:, 2, :], op=ALU.mult),
                                 reads=[b_st], writes=[b_st])
                            t.op("dve", lambda e: e.scalar_tensor_tensor(out=st[:, 3, :], in0=st[:, 1, :], scalar=1.0 / 256, in1=st[:, 3, :],
                                                                         op0=ALU.mult, op1=ALU.subtract), reads=[b_st], writes=[b_st])
                            t.op("act", lambda e: e.activation(out=st[:, 4, :], in_=st[:, 3, :], func=AF.Sqrt, bias=EPS, scale=1.0),
                                 reads=[b_st], writes=[b_st])
                            t.op("dve", lambda e: e.reciprocal(out=st[:, 4, :], in_=st[:, 4, :]), reads=[b_st], writes=[b_st])
                            for hh in range(2):
                                hs = slice(hh * 256, (hh + 1) * 256)
                                t.op("dve", lambda e: e.tensor_scalar(out=osb[:, hs], in0=osb[:, hs], scalar1=st[:, 2, hh:hh + 1], scalar2=st[:, 4, hh:hh + 1],
                                                                      op0=ALU.subtract, op1=ALU.mult), reads=[b_osb, b_st], writes=[b_osb])
                            t.op("pool", lambda e: e.tensor_tensor(out=onb[:], in0=osb[:], in1=gB[:, i, :], op=ALU.mult),
                                 reads=[b_osb, b_gB[i]], writes=[b_onb])
                            for a_ in range(4):
                                t.op("pe", lambda e, a_=a_: e.transpose(psb[1][:, a_ * 128:(a_ + 1) * 128], onb[:, a_ * 128:(a_ + 1) * 128], identb[:]),
                                     reads=[b_onb, b_idb], writes=[pb[1]], inc=(a_ == 3))
                            t.op("act", lambda e: e.activation(out=OT[:, 8 + 2 * h0:8 + 2 * h0 + 4, i * 128:(i + 1) * 128],
                                                               in_=psb[1][:, 0:512].rearrange("p (a n) -> p a n", n=128), func=AF.Copy),
                                 reads=[pb[1]], writes=[b_OT[i]])

                        def upd(i):
                            t.op("pool", lambda e: e.tensor_tensor(out=Kz[:].rearrange("p (a n) -> p a n", n=256),
                                                                   in0=kB[:, i, :].rearrange("p (a n) -> p a n", n=256),
                                                                   in1=rkz[:, h0:h0 + 2].unsqueeze(2).broadcast_to([128, 2, 256]), op=ALU.mult),
                                 reads=[b_kB[i], b_rc], writes=[b_Kz])
                            for hh in range(2):
                                hs = slice(hh * 256, (hh + 1) * 256)
                                sbank = 7 if hh == 0 else 3
                                for dc in range(2):
                                    t.op("pe", lambda e, dc=dc: e.matmul(ps[sbank][:, dc * 256:(dc + 1) * 256], Kz[:, hh * 256 + dc * 128:hh * 256 + (dc + 1) * 128],
                                                                         vB[:, i, hs], start=True, stop=True),
                                         reads=[b_Kz, b_vB[i]], writes=[pb[sbank]], inc=(dc == 1))
                            for hh in range(2):
                                sbank = 7 if hh == 0 else 3
                                t.op("dve", lambda e: e.scalar_tensor_tensor(out=Sf[:, hh, :], in0=Sf[:, hh, :], scalar=float(gam[h0 + hh] ** 128), in1=ps[sbank][:],
                                                                             op0=ALU.mult, op1=ALU.add), reads=[pb[sbank], b_Sf[hh]], writes=[b_Sf[hh]])
                            for hh in range(2):
                                t.op("act", lambda e: e.activation(out=Sb[:, hh, :], in_=Sf[:, hh, :], func=AF.Copy), reads=[b_Sf[hh]], writes=[b_Sb[hh]])

                        if sg == 3:
                            partA(0)
                            for i in range(8):
                                if i + 1 < 8:
                                    partA(i + 1)
                                partB(i)
                                upd(i)
                        else:
                            for i in range(8):
                                upd(i)
                t.barrier()

            with contextlib.ExitStack() as eso:
                garow = self.sb("garow", [128, D], F32, eso)
                xo = [self.sb(f"xo{i}", [128, D], F32, eso) for i in range(2)]
                tmpo = [self.sb(f"tmpo{i}", [128, 512], F32, eso) for i in range(2)]
                b_ga = Buf("garow")
                b_xo = [Buf("xo0"), Buf("xo1")]
                b_tmpo = [Buf("tmpo0"), Buf("tmpo1")]
                t.dma("sp", garow[:], self.modrows[2:3, :].partition_broadcast(128), reads=[self.b_modrows[2]], writes=[b_ga])
                nb = 0
                def load_xo(i):
                    if i < NT:
                        t.dma("sp", xo[i % 2][:], I["xs"][(24 + i) * 128:(25 + i) * 128, :], writes=[b_xo[i % 2]])
                load_xo(0)
                load_xo(1)
                for i in range(NT):
                    x_, bx = xo[i % 2], b_xo[i % 2]
                    for cg in range(4):
                        bank = nb % 4
                        tm, btm = tmpo[nb % 2], b_tmpo[nb % 2]
                        nb += 1
                        for c in range(NKC):
                            t.op("pe", lambda e, c=c: e.matmul(ps[bank][:], OT[:, c, i * 128:(i + 1) * 128], ring[cg][:, c * 512:(c + 1) * 512],
                                                               start=(c == 0), stop=(c == NKC - 1)),
                                 reads=[b_OT[i], rb[cg]], writes=[pb[bank]], inc=(c == NKC - 1))
                        t.op("dve", lambda e: e.tensor_tensor(out=tm[:], in0=ps[bank][:], in1=garow[:, cg * 512:(cg + 1) * 512], op=ALU.mult),
                             reads=[pb[bank], b_ga], writes=[btm])
                        t.op("pool", lambda e: e.tensor_tensor(out=x_[:, cg * 512:(cg + 1) * 512], in0=x_[:, cg * 512:(cg + 1) * 512], in1=tm[:], op=ALU.add),
                             reads=[btm, bx], writes=[bx])
                    t.dma("sp", self.x1s[i * 128:(i + 1) * 128, :], x_[:], reads=[bx], writes=[self.b_x1s])
                    load_xo(i + 2)
                t.barrier()

    def build(self):
        nc = self.nc
        I = self.ins
        ne = self.ne
        self.din("c_col", [128, NKC])
        self.din("w_ada", [D, 6 * D])
        self.din("b_ada", [1, 6 * D])
        self.din("norm_mix", [1, D])
        self.din("norm_ffn", [1, D])
        self.din("norm_out", [1, D])
        self.din("w_router", [D, 64])
        self.din("router_bias", [1, 64])
        self.din("w_gate", [ne, D, 512])
        self.din("w_up", [ne, D, 512])
        self.din("w_down", [ne, 512, D])
        self.din("w_sh_gate", [D, 512])
        self.din("w_sh_up", [D, 512])
        self.din("w_sh_down", [512, D])
        self.din("ident", [128, 128])
        if self.mode in ("testA", "full"):
            self.din("xs", [NSLOT_T * 128, D])
            self.din("pos_i", [128, NSLOT_T], mybir.dt.int32)
            self.din("vmask", [128, NSLOT_T])
            self.din("w_in", [D, 7168])
            self.din("w_out", [D, D])
            self.din("invfA", [128, 16])
            self.din("invfR", [128, 128])
            self.din("gatebias", [128, 4, 16])
            self.din("diag", [128, 4, 16])
            self.din("oh16", [16, 16, 128])
            self.din("causal", [128, 4, 512])
            self.din("rettbl", [128, 4, 128])
            self.din("retxi", [128, 4])
            self.din("retkz", [128, 4])
        if self.mode == "testB":
            self.x1s = self.din("x1s", [NT * 128, D])
        elif self.mode == "testA":
            self.x1s = nc.dram_tensor("x1s", [NT * 128, D], F32, kind="ExternalOutput").ap()
        else:
            self.x1s = nc.dram_tensor("x1s", [NT * 128, D], F32, kind="Internal").ap()
        self.b_x1s = Buf("x1s")
        self.modrows = nc.dram_tensor("modrows", [8, D], F32, kind="Internal").ap()
        self.b_modrows = [Buf(f"modrows{v}") for v in range(6)]
        if self.mode != "testA":
            self.out = nc.dram_tensor("out", [NT * 128, D], F32, kind="ExternalOutput").ap()
        es = self.es
        ps = [es.enter_context(nc.psum_tensor(f"ps{i}", [128, 512], F32)) for i in range(8)]
        pb = [Buf(f"ps{i}") for i in range(8)]
        ring = [self.sb(f"ring{i}", [128, NKC * 512], BF16) for i in range(4)]
        rb = [Buf(f"ring{i}") for i in range(4)]
        ident32 = self.sb("ident32", [128, 128], F32)
        b_id = Buf("ident")
        t = Trk(nc)
        self.t = t
        t.dma("sp", ident32[:], I["ident"][:, :], writes=[b_id])
        if self.mode not in ("testA", "full"):
            with contextlib.ExitStack() as es0:
                for _ in self.phase0_gen(t, ps, pb, ring, rb, es0):
                    pass
                t.barrier()
        if self.mode == "test0":
            bo = Buf("o")
            t.dma("sp", self.out[0:6, :], self.modrows[0:6, :], reads=self.b_modrows, writes=[bo])
            t.drain("sp", [bo])
        if self.mode in ("testA", "full"):
            self.phaseA(t, ps, pb, ring, rb, ident32, b_id)
        if self.mode in ("testB", "full"):
            self.phaseB(t, ps, pb, ring, rb, ident32, b_id)
        t.close()
        es.close()
        return nc


def _common_inputs(inputs, b, ne=NE):
    f = np.ascontiguousarray
    return {
        "c_col": f(inputs["c"][b].reshape(NKC, 128).T),
        "w_ada": inputs["w_ada"][0],
        "b_ada": inputs["b_ada"][0:1],
        "norm_mix": inputs["norm_mix"][0:1],
        "norm_ffn": inputs["norm_ffn"][0:1],
        "norm_out": inputs["norm_out"].reshape(1, D),
        "w_router": inputs["w_router"][0],
        "router_bias": inputs["router_bias"][0:1],
        "w_gate": inputs["w_gate"][0][:ne],
        "w_up": inputs["w_up"][0][:ne],
        "w_down": inputs["w_down"][0][:ne],
        "w_sh_gate": inputs["w_sh_gate"][0],
        "w_sh_up": inputs["w_sh_up"][0],
        "w_sh_down": inputs["w_sh_down"][0],
        "ident": np.eye(128, dtype=np.float32),
    }


def _phaseA_inputs(inputs, b, j):
    f32 = np.float32
    own_end = 1024 * (j + 1)
    start = own_end - 4096
    lo = max(start, 0)
    xs = np.zeros((4096, D), f32)
    xs[lo - start:] = inputs["x"][b, lo:own_end]
    pos = np.zeros((4096,), np.int32)
    pos[lo - start:] = inputs["positions"][b, lo:own_end]
    tile_valid = ((np.arange(NSLOT_T) * 128 + start) >= 0).astype(f32)
    r = np.arange(4)[:, None]
    kb = np.arange(16)[None, :]
    gatebias = np.where((kb < 12 + r) & (kb >= 12 - 4 * j), 0.0, -1e30).astype(f32)
    diag = (kb >= 12 + r).astype(f32)
    oh16 = (np.arange(16)[:, None, None] == np.arange(16)[None, :, None]) * np.ones((1, 1, 128))
    pp = np.arange(128)[:, None, None]
    dd = np.arange(4)[None, :, None]
    cc = np.arange(512)[None, None, :]
    causal = np.where(128 * dd + pp <= cc, 0.0, -30000.0)
    invfA = (np.float32(500000.0) ** (-(np.arange(16, dtype=f32) * f32(2.0) / f32(32.0)))).astype(f32)
    invfR = (np.float32(10000.0) ** (-np.linspace(0.0, 1.0, 128, dtype=f32))).astype(f32)
    gam = 1.0 - 2.0 ** (-5.0 - np.arange(4))
    m = np.arange(128)
    rettbl = (gam[None, :, None] ** (-(m[:, None, None] + 1.0))) / 16.0 * (m[None, None, :] >= m[:, None, None])
    retxi = gam[None, :] ** (m[:, None] + 1.0)
    retkz = gam[None, :] ** (127.0 - m[:, None]) / 16.0
    c = np.ascontiguousarray
    return {
        "xs": xs,
        "pos_i": c(pos.reshape(NSLOT_T, 128).T),
        "vmask": c(np.broadcast_to(tile_valid[None, :], (128, NSLOT_T))).astype(f32),
        "w_in": inputs["w_in"][0],
        "w_out": inputs["w_out"][0],
        "invfA": c(np.broadcast_to(invfA[None, :], (128, 16))).astype(f32),
        "invfR": c(np.broadcast_to(invfR[None, :], (128, 128))).astype(f32),
        "gatebias": c(np.broadcast_to(gatebias[None], (128, 4, 16))).astype(f32),
        "diag": c(np.broadcast_to(diag[None], (128, 4, 16))).astype(f32),
        "oh16": c(oh16).astype(f32),
        "causal": c(causal).astype(f32),
        "rettbl": c(rettbl).astype(f32),
        "retxi": c(retxi).astype(f32),
        "retkz": c(retkz).astype(f32),
    }


_PROG = {}


def kernel(**inputs):
    inputs = {k: np.asarray(v) for k, v in inputs.items()}
    if "full" not in _PROG:
        _PROG["full"] = Prog(mode="full").build()
    nc = _PROG["full"]
    in_maps = []
    for core in range(8):
        b, j = core // 4, core % 4
        im = _common_inputs(inputs, b)
        im.update(_phaseA_inputs(inputs, b, j))
        in_maps.append(im)
    res = run_bass_kernel_spmd(nc, in_maps, core_ids=list(range(8)))
    out = np.empty((2, 4096, D), np.float32)
    for core in range(8):
        b, j = core // 4, core % 4
        out[b, 1024 * j:1024 * (j + 1)] = res.results[core]["out"]
    return out
```

```python
import contextlib
import numpy as np
import concourse.bass as bass
import concourse.mybir as mybir
from concourse.bass_utils import run_bass_kernel_spmd

F32 = mybir.dt.float32
BF16 = mybir.dt.bfloat16
AF = mybir.ActivationFunctionType
ALU = mybir.AluOpType
AX = mybir.AxisListType

D = 2048
NKC = 16
NE = 64
EPS = 1e-6
NT = 8
NSLOT_T = 32


class Buf:
    __slots__ = ("name", "w", "r")

    def __init__(self, name):
        self.name = name
        self.w = None
        self.r = []


class Trk:
    def __init__(self, nc, n_dma_sems=28, n_fixed=8):
        self.nc = nc
        self.sems = {}
        self.seen = {}
        self.engines = {"pe": nc.tensor, "act": nc.scalar, "dve": nc.vector, "pool": nc.gpsimd, "sp": nc.sync}
        self._ctx = []
        for k in ("pe", "act", "dve", "pool"):
            self._mk(k)
        self.n_dma = 0
        self.n_dma_pool = 0
        self.n_fixed = n_fixed
        self.n_dma_sems = n_dma_sems
        for i in range(n_dma_sems):
            self._mk(("dma", i))

    def _mk(self, key):
        nm = "s_" + "".join(ch for ch in str(key) if ch.isalnum())
        cm = self.nc.semaphore(nm)
        h = cm.__enter__()
        self._ctx.append(cm)
        self.sems[key] = [h, 0]
        for e in self.engines:
            self.seen.setdefault(e, {})[key] = 0

    def close(self):
        for cm in reversed(self._ctx):
            cm.__exit__(None, None, None)

    def _wait(self, ename, dep):
        if dep is None:
            return
        key, val = dep
        if self.seen[ename].get(key, 0) >= val:
            return
        self.engines[ename].wait_ge(self.sems[key][0], val)
        self.seen[ename][key] = val

    def _deps(self, ename, reads, writes):
        for b in reads:
            if b.w is not None and not (ename == "pe" and b.w[0] == "pe"):
                self._wait(ename, b.w)
        for b in writes:
            if b.w is not None and not (ename == "pe" and b.w[0] == "pe"):
                self._wait(ename, b.w)
            for d in b.r:
                if not (ename == "pe" and d[0] == "pe"):
                    self._wait(ename, d)

    def _record(self, key, dep, reads, writes):
        for b in writes:
            b.w = dep
            b.r = []
        for b in reads:
            b.r = [d for d in b.r if d[0] != key] + [dep]

    def op(self, ename, fn, reads=(), writes=(), inc=True):
        self._deps(ename, reads, writes)
        ins = fn(self.engines[ename])
        s = self.sems[ename]
        if inc:
            s[1] += 1
            ins.then_inc(s[0], 1)
            val = s[1]
        else:
            val = s[1] + 1
        self._record(ename, (ename, val), reads, writes)
        return ins

    def dma(self, qname, out, in_, reads=(), writes=(), sem=None):
        if sem is None:
            if qname == "pool":
                sem = 4 + self.n_dma_pool % 4
                self.n_dma_pool += 1
            else:
                sem = self.n_fixed + self.n_dma % (self.n_dma_sems - self.n_fixed)
                self.n_dma += 1
        key = ("dma", sem)
        s = self.sems[key]
        if s[1] > 0:
            self._wait(qname, (key, s[1]))
        self._deps(qname, reads, writes)
        ins = self.engines[qname].dma_start(out=out, in_=in_)
        s[1] += 16
        ins.then_inc(s[0], 16)
        self._record(key, (key, s[1]), reads, writes)
        return ins

    def drain(self, ename, bufs):
        for b in bufs:
            self._wait(ename, b.w)

    def barrier(self):
        for e in self.engines:
            for key, s in self.sems.items():
                if s[1] > 0 and key != e:
                    self._wait(e, (key, s[1]))


class Prog:
    def __init__(self, mode="full", ne=NE):
        self.mode = mode
        self.ne = ne
        self.nc = bass.Bass("TRN2", target_bir_lowering=False)
        self.es = contextlib.ExitStack()
        self.ins = {}

    def din(self, name, shape, dt=F32):
        ap = self.nc.dram_tensor(name, list(shape), dt, kind="ExternalInput").ap()
        self.ins[name] = ap
        return ap

    def sb(self, name, shape, dt, es=None):
        return (es or self.es).enter_context(self.nc.sbuf_tensor("sb_" + name, list(shape), dt))

    def phase0_gen(self, t, ps, pb, ring, rb, es):
        I = self.ins
        ccol = self.sb("ccol", [128, NKC], F32, es)
        csil = self.sb("csil", [128, NKC], F32, es)
        crep = self.sb("crep", [128, NKC, 128], BF16, es)
        rowt = [self.sb(f"rowt{i}", [128, D], F32, es) for i in range(2)]
        nrow = self.sb("nrow", [128, D], F32, es)
        b_c, b_crep = Buf("ccol"), Buf("crep")
        b_rowt = [Buf("rowt0"), Buf("rowt1")]
        b_nrow = Buf("nrow")
        t.dma("sp", ccol[:], I["c_col"][:, :], writes=[b_c])
        t.op("act", lambda e: e.activation(out=csil[:], in_=ccol[:], func=AF.Silu), reads=[b_c], writes=[b_c])
        t.op("dve", lambda e: e.tensor_copy(out=crep[:], in_=csil[:].unsqueeze(2).broadcast_to([128, NKC, 128])),
             reads=[b_c], writes=[b_crep])

        def load(i):
            col0 = i * 512
            t.dma("pool", ring[i % 4][:].rearrange("p (k n) -> p k n", n=512),
                  I["w_ada"][:, col0:col0 + 512].rearrange("(k p) n -> p k n", p=128),
                  writes=[rb[i % 4]], sem=i % 4)
        for i in range(3):
            load(i)
        for v in range(6):
            rt = rowt[v % 2]
            t.dma("sp", rt[:], I["b_ada"][0:1, v * D:(v + 1) * D].partition_broadcast(128), writes=[b_rowt[v % 2]])
            if v in (1, 4):
                src = I["norm_mix"] if v == 1 else I["norm_ffn"]
                t.dma("sp", nrow[:], src[0:1, :].partition_broadcast(128), writes=[b_nrow])
            for cg in range(4):
                i = v * 4 + cg
                if i + 3 < 24:
                    load(i + 3)
                slot = i % 4
                bank = 6 + cg % 2
                for k in range(NKC):
                    t.op("pe", lambda e, k=k: e.matmul(ps[bank][:], crep[:, k, :], ring[slot][:, k * 512:(k + 1) * 512],
                                                       start=(k == 0), stop=(k == NKC - 1)),
                         reads=[b_crep, rb[slot]], writes=[pb[bank]], inc=(k == NKC - 1))
                t.op("dve", lambda e: e.tensor_tensor(out=rt[:, cg * 512:(cg + 1) * 512], in0=ps[bank][:],
                                                      in1=rt[:, cg * 512:(cg + 1) * 512], op=ALU.add),
                     reads=[pb[bank], b_rowt[v % 2]], writes=[b_rowt[v % 2]])
                if cg == 3:
                    if v in (1, 4):
                        t.op("dve", lambda e: e.scalar_tensor_tensor(out=rt[:], in0=rt[:], scalar=1.0, in1=nrow[:],
                                                                     op0=ALU.add, op1=ALU.mult),
                             reads=[b_rowt[v % 2], b_nrow], writes=[b_rowt[v % 2]])
                    t.dma("sp", self.modrows[v:v + 1, :], rt[0:1, :], reads=[b_rowt[v % 2]], writes=[self.b_modrows[v]])
                yield (v, cg)

    def routing(self, t, R, lg_ap, bl, rbrow, b_rb, wn_out, bw):
        sc, bi, m1, eq, g2, m2, t8, gm, bm, e8, ws = (R[k] for k in ("sc", "bi", "m1", "eq", "g2", "m2", "t8", "gm", "bm", "e8", "ws"))
        B = R["B"]

        def v3(x):
            return x[:].rearrange("p (g k) -> p g k", k=8)

        def bc(x):
            return x[:].unsqueeze(2).broadcast_to([128, 8, 8])
        t.op("act", lambda e: e.activation(out=sc[:], in_=lg_ap, func=AF.Sigmoid), reads=[bl], writes=[B["sc"]])
        t.op("dve", lambda e: e.tensor_tensor(out=bi[:], in0=sc[:], in1=rbrow[:], op=ALU.add), reads=[B["sc"], b_rb], writes=[B["bi"]])
        t.op("dve", lambda e: e.tensor_reduce(out=m1[:], in_=v3(bi), axis=AX.X, op=ALU.max), reads=[B["bi"]], writes=[B["m1"]])
        t.op("dve", lambda e: e.tensor_tensor(out=v3(eq), in0=v3(bi), in1=bc(m1), op=ALU.is_equal), reads=[B["bi"], B["m1"]], writes=[B["eq"]])
        t.op("dve", lambda e: e.scalar_tensor_tensor(out=g2[:], in0=eq[:], scalar=-1e30, in1=bi[:], op0=ALU.mult, op1=ALU.add),
             reads=[B["eq"], B["bi"]], writes=[B["g2"]])
        t.op("dve", lambda e: e.tensor_reduce(out=m2[:], in_=v3(g2), axis=AX.X, op=ALU.max), reads=[B["g2"]], writes=[B["m2"]])
        t.op("dve", lambda e: e.tensor_tensor(out=m2[:], in0=m2[:], in1=m1[:], op=ALU.add), reads=[B["m2"], B["m1"]], writes=[B["m2"]])
        t.op("dve", lambda e: e.max(out=t8[:], in_=m2[:]), reads=[B["m2"]], writes=[B["t8"]])
        t.op("dve", lambda e: e.tensor_scalar(out=gm[:], in0=m2[:], scalar1=t8[:, 3:4], scalar2=None, op0=ALU.is_ge),
             reads=[B["m2"], B["t8"]], writes=[B["gm"]])
        t.op("dve", lambda e: e.tensor_scalar(out=t8[:], in0=gm[:], scalar1=-1.0, scalar2=1e30, op0=ALU.add, op1=ALU.mult),
             reads=[B["gm"]], writes=[B["t8"]])
        t.op("dve", lambda e: e.tensor_tensor(out=v3(bm), in0=v3(bi), in1=bc(gm), op=ALU.mult), reads=[B["bi"], B["gm"]], writes=[B["bm"]])
        t.op("dve", lambda e: e.tensor_tensor(out=v3(bm), in0=v3(bm), in1=bc(t8), op=ALU.add), reads=[B["bm"], B["t8"]], writes=[B["bm"]])
        t.op("dve", lambda e: e.max(out=e8[:], in_=bm[:]), reads=[B["bm"]], writes=[B["e8"]])
        t.op("dve", lambda e: e.tensor_scalar(out=eq[:], in0=bm[:], scalar1=e8[:, 7:8], scalar2=None, op0=ALU.is_ge),
             reads=[B["bm"], B["e8"]], writes=[B["eq"]])
        t.op("dve", lambda e: e.tensor_tensor(out=g2[:], in0=eq[:], in1=sc[:], op=ALU.mult), reads=[B["eq"], B["sc"]], writes=[B["g2"]])
        t.op("dve", lambda e: e.tensor_reduce(out=ws[:], in_=g2[:], axis=AX.X, op=ALU.add), reads=[B["g2"]], writes=[B["ws"]])
        t.op("dve", lambda e: e.reciprocal(out=ws[:], in_=ws[:]), reads=[B["ws"]], writes=[B["ws"]])
        t.op("dve", lambda e: e.tensor_scalar(out=wn_out, in0=g2[:], scalar1=ws[:, 0:1], scalar2=2.5, op0=ALU.mult, op1=ALU.mult),
             reads=[B["g2"], B["ws"]], writes=[bw])

    def phaseB(self, t, ps, pb, ring, rb, ident32, b_id):
        nc = self.nc
        I = self.ins
        ne = self.ne
        with contextlib.ExitStack() as es:
            h2T = self.sb("h2T", [128, NKC, NT * 128], BF16, es)
            wn = self.sb("wn", [128, NT, 64], F32, es)
            ones1 = self.sb("ones1", [128, 1], F32, es)
            b_y = [[Buf(f"y{i}_{c}") for c in range(4)] for i in range(NT)]
            b_h2T = [Buf(f"h2T{i}") for i in range(NT)]
            b_wn = [Buf(f"wn{i}") for i in range(NT)]
            b_act = [[Buf(f"act{h}_{f}") for f in range(4)] for h in range(2)]
            b_sil = [Buf("sil0"), Buf("sil1")]
            b_one = Buf("ones1")
            t.op("dve", lambda e: e.memset(ones1[:], 1.0), writes=[b_one])
            mats = []
            for e_ in range(ne):
                mats += [("g", I["w_gate"][e_]), ("g", I["w_up"][e_]), ("d", I["w_down"][e_])]
            mats += [("g", I["w_sh_gate"]), ("g", I["w_sh_up"]), ("d", I["w_sh_down"])]
            state = {"issued": 0, "consumed": 0}

            def pump():
                while state["issued"] < len(mats) and state["issued"] - 4 < state["consumed"]:
                    i = state["issued"]
                    kind, src = mats[i]
                    slot = i % 4
                    if kind == "g":
                        t.dma("pool", ring[slot][:].rearrange("p (k n) -> p k n", n=512),
                              src.rearrange("(k p) n -> p k n", p=128), writes=[rb[slot]], sem=slot)
                    else:
                        t.dma("pool", ring[slot][:].rearrange("p (k n) -> p k n", n=D),
                              src.rearrange("(k p) n -> p k n", p=128), writes=[rb[slot]], sem=slot)
                    state["issued"] += 1
            pump()
            with contextlib.ExitStack() as es1:
                g2row = self.sb("g2row", [128, D], F32, es1)
                shfrow = self.sb("shfrow", [128, D], F32, es1)
                xt = [self.sb(f"xtB{i}", [128, D], F32, es1) for i in range(2)]
                hf = self.sb("hfB", [128, D], F32, es1)
                sq = self.sb("sqB", [128, D], BF16, es1)
                hT32 = self.sb("hT32", [128, NKC, 128], F32, es1)
                wr32 = self.sb("wr32", [128, NKC, 64], F32, es1)
                rbrow = self.sb("rbrow", [128, 64], F32, es1)
                ss = self.sb("ssB", [128, 2], F32, es1)
                R = {k: self.sb("rt_" + k, [128, n], F32, es1) for k, n in
                     (("sc", 64), ("bi", 64), ("m1", 8), ("eq", 64), ("g2", 64), ("m2", 8), ("t8", 8), ("gm", 8),
                      ("bm", 64), ("e8", 8), ("ws", 1))}
                R["B"] = {k: Buf("rt_" + k) for k in ("sc", "bi", "m1", "eq", "g2", "m2", "t8", "gm", "bm", "e8", "ws")}
                b_g2, b_shf, b_wr, b_rbr = Buf("g2row"), Buf("shfrow"), Buf("wr32"), Buf("rbrow")
                b_xt = [Buf("xt0"), Buf("xt1")]
                b_hf, b_sq, b_hT32, b_ss = Buf("hf"), Buf("sq"), Buf("hT32"), Buf("ss")
                t.dma("sp", g2row[:], self.modrows[4:5, :].partition_broadcast(128), reads=[self.b_modrows[4]], writes=[b_g2])
                t.dma("sp", shfrow[:], self.modrows[3:4, :].partition_broadcast(128), reads=[self.b_modrows[3]], writes=[b_shf])
                t.dma("sp", wr32[:], I["w_router"].rearrange("(k p) n -> p k n", p=128), writes=[b_wr])
                t.dma("sp", rbrow[:], I["router_bias"][0:1, :].partition_broadcast(128), writes=[b_rbr])
                hfs = [hf, self.sb("hfB1", [128, D], F32, es1)]
                b_hfs = [b_hf, Buf("hf1")]
                ss4 = self.sb("ssB4", [128, 4], F32, es1)
                b_ss4 = [Buf("ssB40"), Buf("ssB41")]

                def normB(i):
                    if i >= NT:
                        return
                    x_, bx = xt[i % 2], b_xt[i % 2]
                    hf_, bhf = hfs[i % 2], b_hfs[i % 2]
                    sa, sb2, bss = ss4[:, 2 * (i % 2):2 * (i % 2) + 1], ss4[:, 2 * (i % 2) + 1:2 * (i % 2) + 2], b_ss4[i % 2]
                    t.dma("sp", x_[:], self.x1s[i * 128:(i + 1) * 128, :], reads=[self.b_x1s], writes=[bx])
                    t.op("act", lambda e: e.activation(out=sq[:], in_=x_[:], func=AF.Square, accum_out=sa), reads=[bx], writes=[b_sq, bss])
                    t.op("act", lambda e: e.activation(out=sb2, in_=sa, func=AF.Sqrt, bias=EPS, scale=1.0 / D), reads=[bss], writes=[bss])
                    t.op("dve", lambda e: e.reciprocal(out=sb2, in_=sb2), reads=[bss], writes=[bss])
                    t.op("dve", lambda e: e.scalar_tensor_tensor(out=hf_[:], in0=x_[:], scalar=sb2, in1=g2row[:], op0=ALU.mult, op1=ALU.mult),
                         reads=[bx, bss, b_g2], writes=[bhf])
                    t.op("pool", lambda e: e.tensor_tensor(out=hf_[:], in0=hf_[:], in1=shfrow[:], op=ALU.add), reads=[bhf, b_shf], writes=[bhf])
                normB(0)
                for i in range(NT):
                    hf, b_hf = hfs[i % 2], b_hfs[i % 2]
                    for j in range(4):
                        for r in range(4):
                            c = 4 * j + r
                            t.op("pe", lambda e, c=c, r=r: e.matmul(ps[j][:, r * 128:(r + 1) * 128],
                                                                    hf[:, c * 128:(c + 1) * 128], ident32[:],
                                                                    start=True, stop=True),
                                 reads=[b_hf, b_id], writes=[pb[j]], inc=(r == 3))
                        t.op("dve", lambda e: e.tensor_copy(out=hT32[:, 4 * j:4 * j + 4, :],
                                                            in_=ps[j][:].rearrange("p (r n) -> p r n", n=128)),
                             reads=[pb[j]], writes=[b_hT32])
                        t.op("act", lambda e: e.activation(out=h2T[:, 4 * j:4 * j + 4, i * 128:(i + 1) * 128],
                                                           in_=hT32[:, 4 * j:4 * j + 4, :], func=AF.Copy),
                             reads=[b_hT32], writes=[b_h2T[i]])
                    for c in range(NKC):
                        t.op("pe", lambda e, c=c: e.matmul(ps[4][:, 0:64], hT32[:, c, :], wr32[:, c, :],
                                                           start=(c == 0), stop=(c == NKC - 1)),
                             reads=[b_hT32, b_wr], writes=[pb[4]], inc=(c == NKC - 1))
                    normB(i + 1)
                    self.routing(t, R, ps[4][:, 0:64], pb[4], rbrow, b_rbr, wn[:, i, :], b_wn[i])
            t.barrier()
            yacc = self.sb("yacc", [128, NT, D], F32, es)
            act = self.sb("actT", [128, 4, NT * 128], BF16, es)
            sil = [self.sb(f"sil{i}", [128, 512], BF16, es) for i in range(2)]
            nyb = 0
            for e_ in range(ne + 1):
                sg, su, sd = (3 * e_) % 4, (3 * e_ + 1) % 4, (3 * e_ + 2) % 4
                wg, wu, wd = ring[sg], ring[su], ring[sd]
                nau = 0
                for half in range(2):
                    hbufs = b_h2T[4 * half:4 * half + 4]
                    for fc in range(4):
                        pa, pu = (nau % 2) * 2, (nau % 2) * 2 + 1
                        nau += 1
                        for w_, slot_, bank in ((wg, sg, pa), (wu, su, pu)):
                            for k in range(NKC):
                                t.op("pe", lambda e, k=k, w_=w_, bank=bank: e.matmul(
                                    ps[bank][:], w_[:, k * 512 + fc * 128:k * 512 + (fc + 1) * 128],
                                    h2T[:, k, half * 512:(half + 1) * 512], start=(k == 0), stop=(k == NKC - 1)),
                                    reads=[rb[slot_]] + hbufs, writes=[pb[bank]], inc=(k == NKC - 1))
                        sl, bsl = sil[nau % 2], b_sil[nau % 2]
                        t.op("act", lambda e: e.activation(out=sl[:], in_=ps[pa][:], func=AF.Silu), reads=[pb[pa]], writes=[bsl])
                        t.op("dve", lambda e: e.tensor_tensor(out=act[:, fc, half * 512:(half + 1) * 512], in0=sl[:],
                                                              in1=ps[pu][:], op=ALU.mult),
                             reads=[bsl, pb[pu]], writes=[b_act[half][fc]])
                state["consumed"] += 2
                pump()
                for i in range(NT):
                    for cg in range(4):
                        bank = 4 + nyb % 4
                        nyb += 1
                        for fc in range(4):
                            t.op("pe", lambda e, fc=fc: e.matmul(ps[bank][:], act[:, fc, i * 128:(i + 1) * 128],
                                                                 wd[:, fc * D + cg * 512:fc * D + (cg + 1) * 512],
                                                                 start=(fc == 0), stop=(fc == 3)),
                                 reads=[rb[sd], b_act[i // 4][fc]], writes=[pb[bank]], inc=(fc == 3))
                        ysl = yacc[:, i, cg * 512:(cg + 1) * 512]
                        wcol = wn[:, i, e_:e_ + 1] if e_ < ne else ones1[:, 0:1]
                        wbuf = b_wn[i] if e_ < ne else b_one
                        if e_ == 0:
                            t.op("dve", lambda e: e.tensor_scalar(out=ysl, in0=ps[bank][:], scalar1=wcol, scalar2=None, op0=ALU.mult),
                                 reads=[pb[bank], wbuf], writes=[b_y[i][cg]])
                        else:
                            t.op("dve", lambda e: e.scalar_tensor_tensor(out=ysl, in0=ps[bank][:], scalar=wcol, in1=ysl,
                                                                         op0=ALU.mult, op1=ALU.add),
                                 reads=[pb[bank], wbuf, b_y[i][cg]], writes=[b_y[i][cg]])
                state["consumed"] += 1
                pump()
            t.barrier()
            with contextlib.ExitStack() as es3:
                r0 = ring[0][:].bitcast(F32)
                r1 = ring[1][:].bitcast(F32)
                gfrow = r0[:, 0:D]
                norow = r0[:, D:2 * D]
                xt = [r1[:, 0:D], r1[:, D:2 * D]]
                sq = ring[2][:, 0:D]
                ss = self.sb("ssC", [128, 2 * NT], F32, es3)
                b_gf, b_no = Buf("gfrow"), Buf("norow")
                b_xt = [Buf("xtC0"), Buf("xtC1")]
                b_sq = Buf("sqC")
                b_ss = [Buf(f"ssC{i}") for i in range(NT)]
                t.dma("sp", gfrow[:], self.modrows[5:6, :].partition_broadcast(128), reads=[self.b_modrows[5]], writes=[b_gf])
                t.dma("sp", norow[:], I["norm_out"][0:1, :].partition_broadcast(128), writes=[b_no])
                outs = []

                def load_x1(i):
                    if i < NT:
                        t.dma("sp", xt[i % 2][:], self.x1s[i * 128:(i + 1) * 128, :], reads=[self.b_x1s], writes=[b_xt[i % 2]])
                load_x1(0)
                load_x1(1)
                for i in range(NT):
                    x_, bx = xt[i % 2], b_xt[i % 2]
                    yb = b_y[i]
                    t.op("pool", lambda e: e.tensor_tensor(out=yacc[:, i, :], in0=yacc[:, i, :], in1=gfrow[:], op=ALU.mult),
                         reads=yb + [b_gf], writes=yb)
                    t.op("dve", lambda e: e.tensor_tensor(out=yacc[:, i, :], in0=yacc[:, i, :], in1=x_[:], op=ALU.add),
                         reads=yb + [bx], writes=yb)
                    t.op("act", lambda e: e.activation(out=sq[:], in_=yacc[:, i, :], func=AF.Square, accum_out=ss[:, 2 * i:2 * i + 1]),
                         reads=yb, writes=[b_sq, b_ss[i]])
                    t.op("act", lambda e: e.activation(out=ss[:, 2 * i + 1:2 * i + 2], in_=ss[:, 2 * i:2 * i + 1], func=AF.Sqrt,
                                                       bias=EPS, scale=1.0 / D), reads=[b_ss[i]], writes=[b_ss[i]])
                    t.op("dve", lambda e: e.reciprocal(out=ss[:, 2 * i + 1:2 * i + 2], in_=ss[:, 2 * i + 1:2 * i + 2]),
                         reads=[b_ss[i]], writes=[b_ss[i]])
                    t.op("dve", lambda e: e.scalar_tensor_tensor(out=yacc[:, i, :], in0=yacc[:, i, :], scalar=ss[:, 2 * i + 1:2 * i + 2],
                                                                 in1=norow[:], op0=ALU.mult, op1=ALU.mult),
                         reads=yb + [b_ss[i], b_no], writes=yb)
                    bo = Buf(f"out{i}")
                    t.dma("sp", self.out[i * 128:(i + 1) * 128, :], yacc[:, i, :], reads=yb, writes=[bo])
                    outs.append(bo)
                    load_x1(i + 2)
                t.drain("sp", outs)
                t.barrier()

    def phaseA(self, t, ps, pb, ring, rb, ident32, b_id):
        nc = self.nc
        I = self.ins
        SCALE = 128.0 ** -0.5
        PI = float(np.pi)
        psb = [p_[:].bitcast(BF16) for p_ in ps]
        hTs = nc.dram_tensor("hTs", [NSLOT_T, 128, D], BF16, kind="Internal").ap()
        b_hTs = [Buf(f"hTs{i}") for i in range(NSLOT_T)]
        hTt = [ring[3][:, q * D:(q + 1) * D] for q in range(4)]
        b_hTt = [Buf(f"hTt{q}") for q in range(4)]

        def load_hT(tile):
            if 0 <= tile < NSLOT_T:
                t.dma("sp", hTt[tile % 4], hTs[tile], reads=[b_hTs[tile]], writes=[b_hTt[tile % 4]])

        def project(tile, wslot, bslot, ncols, bank):
            for c in range(NKC):
                t.op("pe", lambda e, c=c: e.matmul(ps[bank][:, 0:ncols], hTt[tile % 4][:, c * 128:(c + 1) * 128],
                                                   wslot[:, c * 512:c * 512 + ncols], start=(c == 0), stop=(c == NKC - 1)),
                     reads=[b_hTt[tile % 4], bslot], writes=[pb[bank]], inc=(c == NKC - 1))

        def load_w(slot, col0, ncols=512):
            t.dma("pool", ring[slot][:].rearrange("p (k n) -> p k n", n=512)[:, :, 0:ncols],
                  I["w_in"][:, col0:col0 + ncols].rearrange("(k p) n -> p k n", p=128), writes=[rb[slot]], sem=slot)

        with contextlib.ExitStack() as es:
            OT = self.sb("OT", [128, NKC, NT * 128], BF16, es)
            b_OT = [Buf(f"OT{i}") for i in range(NT)]
            identb = self.sb("identb", [128, 128], BF16, es)
            onesb = self.sb("onesb", [128, 128], BF16, es)
            posi = self.sb("posi", [128, NSLOT_T], mybir.dt.int32, es)
            posf = self.sb("posf", [128, NSLOT_T], F32, es)
            vmask = self.sb("vmask", [128, NSLOT_T], F32, es)
            b_idb, b_ones, b_pos, b_vm = (Buf(n) for n in ("identb", "onesb", "pos", "vmask"))
            t.op("dve", lambda e: e.tensor_copy(out=identb[:], in_=ident32[:]), reads=[b_id], writes=[b_idb])
            t.op("dve", lambda e: e.memset(onesb[:], 1.0), writes=[b_ones])
            t.dma("sp", posi[:], I["pos_i"][:, :], writes=[b_pos])
            t.op("dve", lambda e: e.tensor_copy(out=posf[:], in_=posi[:]), reads=[b_pos], writes=[b_pos])
            t.dma("sp", vmask[:], I["vmask"][:, :], writes=[b_vm])

            with contextlib.ExitStack() as es0:
                p0 = self.phase0_gen(t, ps, pb, ring, rb, es0)
                for _ in range(8):
                    next(p0)
                g1row = self.sb("g1row", [128, D], F32, es0)
                sharow = self.sb("sharow", [128, D], F32, es0)
                xt = [self.sb(f"xtA{i}", [128, D], F32, es0) for i in range(3)]
                hb = [self.sb(f"hbA{i}", [128, D], BF16, es0) for i in range(2)]
                hst = [self.sb(f"hstA{i}", [128, D], BF16, es0) for i in range(2)]
                sq = self.sb("sqA", [128, D], BF16, es0)
                ssA = self.sb("ssA", [128, 4], F32, es0)
                b_g1, b_sha, b_sq = Buf("g1row"), Buf("sharow"), Buf("sq")
                b_xt = [Buf("xtA0"), Buf("xtA1"), Buf("xtA2")]
                b_hb = [Buf("hb0"), Buf("hb1")]
                b_hst = [Buf("hst0"), Buf("hst1")]

                def load_x(tile):
                    if tile < NSLOT_T:
                        t.dma("sp", xt[tile % 3][:], I["xs"][tile * 128:(tile + 1) * 128, :], writes=[b_xt[tile % 3]])
                b_ss = [Buf("ssA0"), Buf("ssA1")]
                t.dma("sp", g1row[:], self.modrows[1:2, :].partition_broadcast(128), reads=[self.b_modrows[1]], writes=[b_g1])
                t.dma("sp", sharow[:], self.modrows[0:1, :].partition_broadcast(128), reads=[self.b_modrows[0]], writes=[b_sha])
                def front(tile):
                    if tile >= NSLOT_T:
                        return
                    k2 = tile % 2
                    x_, bx = xt[tile % 3], b_xt[tile % 3]
                    sa, sb2 = ssA[:, 2 * k2:2 * k2 + 1], ssA[:, 2 * k2 + 1:2 * k2 + 2]
                    t.op("act", lambda e: e.activation(out=sq[:], in_=x_[:], func=AF.Square, accum_out=sa), reads=[bx], writes=[b_sq, b_ss[k2]])
                    t.op("act", lambda e: e.activation(out=sb2, in_=sa, func=AF.Sqrt, bias=EPS, scale=1.0 / D), reads=[b_ss[k2]], writes=[b_ss[k2]])
                    t.op("dve", lambda e: e.reciprocal(out=sb2, in_=sb2), reads=[b_ss[k2]], writes=[b_ss[k2]])
                    t.op("dve", lambda e: e.scalar_tensor_tensor(out=x_[:], in0=x_[:], scalar=sb2, in1=g1row[:], op0=ALU.mult, op1=ALU.mult),
                         reads=[bx, b_ss[k2], b_g1], writes=[bx])
                    t.op("dve", lambda e: e.tensor_tensor(out=hb[k2][:], in0=x_[:], in1=sharow[:], op=ALU.add),
                         reads=[bx, b_sha], writes=[b_hb[k2]])

                def back(tile):
                    k2 = tile % 2
                    for j in range(2):
                        bank = 2 * k2 + j
                        for r in range(8):
                            c = 8 * j + r
                            t.op("pe", lambda e, c=c, r=r: e.transpose(psb[bank][:, r * 128:(r + 1) * 128], hb[k2][:, c * 128:(c + 1) * 128], identb[:]),
                                 reads=[b_hb[k2], b_idb], writes=[pb[bank]], inc=(r == 7))
                    t.op("act", lambda e: e.activation(out=hst[k2][:, 0:1024], in_=psb[2 * k2][:], func=AF.Copy), reads=[pb[2 * k2]], writes=[b_hst[k2]])
                    t.op("dve", lambda e: e.tensor_copy(out=hst[k2][:, 1024:2048], in_=psb[2 * k2 + 1][:]), reads=[pb[2 * k2 + 1]], writes=[b_hst[k2]])
                    load_x(tile + 3)
                    t.dma("sp", hTs[tile], hst[k2][:], reads=[b_hst[k2]], writes=[b_hTs[tile]])

                load_x(0)
                load_x(1)
                load_x(2)
                front(0)
                for tile in range(NSLOT_T):
                    if tile % 2 == 1:
                        next(p0, None)
                    front(tile + 1)
                    back(tile)
                for _ in p0:
                    pass
                t.barrier()

            def sincos(ang, n, sin_out, cos_out, b_ang, b_out):
                kf = self._sc_tmp[:, 0:n]
                ki = self._sc_ki[:, 0:n]
                bt = self._b_sc
                t.op("dve", lambda e: e.tensor_scalar(out=cos_out, in0=ang, scalar1=0.5 * PI, scalar2=None, op0=ALU.add),
                     reads=[b_ang], writes=[b_out])
                for r_, br in ((cos_out, b_out), (ang, b_ang)):
                    t.op("dve", lambda e: e.tensor_scalar(out=kf, in0=r_, scalar1=1.0 / (2 * PI), scalar2=None, op0=ALU.mult), reads=[br], writes=[bt])
                    t.op("dve", lambda e: e.tensor_copy(out=ki, in_=kf), reads=[bt], writes=[bt])
                    t.op("dve", lambda e: e.tensor_copy(out=kf, in_=ki), reads=[bt], writes=[bt])
                    t.op("dve", lambda e: e.scalar_tensor_tensor(out=r_, in0=kf, scalar=-2 * PI, in1=r_, op0=ALU.mult, op1=ALU.add),
                         reads=[bt, br], writes=[br])
                    t.op("dve", lambda e: e.tensor_scalar(out=kf, in0=r_, scalar1=PI, scalar2=-2 * PI, op0=ALU.is_gt, op1=ALU.mult), reads=[br], writes=[bt])
                    t.op("dve", lambda e: e.tensor_tensor(out=r_, in0=r_, in1=kf, op=ALU.add), reads=[bt, br], writes=[br])
                    t.op("dve", lambda e: e.tensor_scalar(out=r_, in0=r_, scalar1=-PI, scalar2=PI, op0=ALU.max, op1=ALU.min), reads=[br], writes=[br])
                t.op("act", lambda e: e.activation(out=cos_out, in_=cos_out, func=AF.Sin), reads=[b_out], writes=[b_out])
                t.op("act", lambda e: e.activation(out=sin_out, in_=ang, func=AF.Sin), reads=[b_ang], writes=[b_out])

            self._sc_ki = self.sb("sc_ki", [128, 1024], mybir.dt.int32, es)
            self._sc_tmp = self.sb("sc_tmp", [128, 1024], F32, es)
            self._b_sc = Buf("sc_tmp")

            NH = 4
            with contextlib.ExitStack() as esm:
                cosA = self.sb("cosA", [128, NSLOT_T, 16], F32, esm)
                sinA = self.sb("sinA", [128, NSLOT_T, 16], F32, esm)
                angA = self.sb("angA", [128, NSLOT_T, 16], F32, esm)
                invfA = self.sb("invfA", [128, 16], F32, esm)
                KT = self.sb("KT", [128, NH, NSLOT_T * 128], BF16, esm)
                V = self.sb("Vm", [128, NSLOT_T, NH * 128], BF16, esm)
                QT = self.sb("QT", [128, NH, NT * 128], BF16, esm)
                TT = self.sb("TT", [16, NH, NT * 128], BF16, esm)
                kbs = [self.sb(f"kbm{i}", [128, NH, 128], BF16, esm) for i in range(2)]
                tm1 = self.sb("tm1", [128, NH, 16], F32, esm)
                tm2 = self.sb("tm2", [128, NH, 16], F32, esm)
                kmT = self.sb("kmT", [128, NH, 16], F32, esm)
                kmTb = self.sb("kmTb", [128, NH, 16], BF16, esm)
                gbias = self.sb("gbias", [128, 4, 16], F32, esm)
                diag = self.sb("diag", [128, 4, 16], F32, esm)
                oh16 = self.sb("oh16", [16, 16, 128], BF16, esm)
                causal = self.sb("causal", [128, 4, 512], BF16, esm)
                gm = self.sb("gmA", [128, NH, 16], F32, esm)
                t8 = self.sb("t8A", [128, NH, 8], F32, esm)
                thr = self.sb("thrA", [128, NH], F32, esm)
                sel = self.sb("selA", [128, NH, 16], F32, esm)
                selb = self.sb("selbA", [128, NH, 16], BF16, esm)
                Pb = [self.sb(f"Pb{i}", [128, 512], BF16, esm) for i in range(3)]
                rl = self._sc_tmp[:, 0:512]
                b_cs, b_ang, b_ifa = Buf("cosinA"), Buf("angA"), Buf("invfA")
                b_KT = [Buf(f"KT{i}") for i in range(NSLOT_T)]
                b_V = [Buf(f"V{i}") for i in range(NSLOT_T)]
                b_QT = [Buf(f"QT{i}") for i in range(NT)]
                b_TT = [Buf(f"TT{i}") for i in range(NT)]
                b_kbs = [Buf("kb0"), Buf("kb1")]
                b_tm1, b_tm2, b_km, b_kmb = (Buf(n) for n in ("tm1", "tm2", "kmT", "kmTb"))
                b_gb, b_dg, b_oh, b_ca, b_gm, b_t8, b_thr, b_sel, b_selb, b_rl = (
                    Buf(n) for n in ("gbias", "diag", "oh16", "causal", "gm", "t8", "thr", "sel", "selb", "rl"))
                b_P = [Buf(f"P{i}") for i in range(3)]
                t.dma("sp", invfA[:], I["invfA"][:, :], writes=[b_ifa])
                t.dma("sp", gbias[:], I["gatebias"][:, :, :], writes=[b_gb])
                t.dma("sp", diag[:], I["diag"][:, :, :], writes=[b_dg])
                t.dma("pool", oh16[:], I["oh16"][:, :, :], writes=[b_oh])
                t.dma("pool", causal[:], I["causal"][:, :, :], writes=[b_ca])
                t.op("dve", lambda e: e.tensor_tensor(out=angA[:], in0=posf[:].unsqueeze(2).broadcast_to([128, NSLOT_T, 16]),
                                                      in1=invfA[:].unsqueeze(1).broadcast_to([128, NSLOT_T, 16]), op=ALU.mult),
                     reads=[b_pos, b_ifa], writes=[b_ang])
                sincos(angA[:].rearrange("p a b -> p (a b)"), NSLOT_T * 16, sinA[:].rearrange("p a b -> p (a b)"),
                       cosA[:].rearrange("p a b -> p (a b)"), b_ang, b_cs)

                def rotaryA(bank, tile, dst, b_dst):
                    src3 = ps[bank][:].rearrange("p (h d) -> p h d", d=128)
                    cb = cosA[:, tile, :].unsqueeze(1).broadcast_to([128, NH, 16])
                    sn = sinA[:, tile, :].unsqueeze(1).broadcast_to([128, NH, 16])
                    x1, x2 = src3[:, :, 0:16], src3[:, :, 16:32]
                    t.op("dve", lambda e: e.tensor_tensor(out=tm1[:], in0=x1, in1=cb, op=ALU.mult), reads=[pb[bank], b_cs], writes=[b_tm1])
                    t.op("dve", lambda e: e.tensor_tensor(out=tm2[:], in0=x2, in1=sn, op=ALU.mult), reads=[pb[bank], b_cs], writes=[b_tm2])
                    t.op("dve", lambda e: e.tensor_tensor(out=dst[:, :, 0:16], in0=tm1[:], in1=tm2[:], op=ALU.subtract),
                         reads=[b_tm1, b_tm2], writes=[b_dst])
                    t.op("dve", lambda e: e.tensor_tensor(out=tm1[:], in0=x2, in1=cb, op=ALU.mult), reads=[pb[bank], b_cs], writes=[b_tm1])
                    t.op("dve", lambda e: e.tensor_tensor(out=tm2[:], in0=x1, in1=sn, op=ALU.mult), reads=[pb[bank], b_cs], writes=[b_tm2])
                    t.op("dve", lambda e: e.tensor_tensor(out=dst[:, :, 16:32], in0=tm1[:], in1=tm2[:], op=ALU.add),
                         reads=[b_tm1, b_tm2], writes=[b_dst])
                    t.op("dve", lambda e: e.tensor_copy(out=dst[:, :, 32:128], in_=src3[:, :, 32:128]), reads=[pb[bank]], writes=[b_dst])

                def transp4(src, b_src, dstT, b_dstT):
                    for h in range(NH):
                        t.op("pe", lambda e, h=h: e.transpose(psb[4][:, h * 128:(h + 1) * 128], src[:, h, :], identb[:]),
                             reads=[b_src, b_idb], writes=[pb[4]], inc=(h == NH - 1))
                    t.op("act", lambda e: e.activation(out=dstT, in_=psb[4][:, 0:NH * 128].rearrange("p (h n) -> p h n", n=128), func=AF.Copy),
                         reads=[pb[4]], writes=[b_dstT])

                def load_moba_w(p):
                    load_w(0, 1024 + 512 * p)
                    load_w(1, 2048 + 512 * p)
                    load_w(2, 512 * p)
                load_moba_w(0)
                for p in range(2):
                    for q in range(3):
                        load_hT(q)

                    def post(tile):
                        k2 = tile % 2
                        rotaryA(k2, tile, kbs[0], b_kbs[0])
                        t.op("act", lambda e: e.activation(out=V[:, tile, :], in_=ps[2 + k2][:], func=AF.Copy), reads=[pb[2 + k2]], writes=[b_V[tile]])
                        transp4(kbs[0], b_kbs[0], KT[:, :, tile * 128:(tile + 1) * 128], b_KT[tile])
                        if tile >= 24:
                            i = tile - 24
                            rotaryA(5 + k2, tile, kbs[1], b_kbs[1])
                            transp4(kbs[1], b_kbs[1], QT[:, :, i * 128:(i + 1) * 128], b_QT[i])
                        if tile % 8 == 7:
                            sg = tile // 8
                            t.op("dve", lambda e: e.tensor_reduce(out=kmT[:, :, 4 * sg:4 * sg + 4],
                                                                  in_=KT[:, :, sg * 1024:(sg + 1) * 1024].rearrange("p h (b k) -> p h b k", k=256),
                                                                  axis=AX.X, op=ALU.add),
                                 reads=b_KT[sg * 8:(sg + 1) * 8], writes=[b_km])

                    for tile in range(NSLOT_T):
                        k2 = tile % 2
                        load_hT(tile + 3)
                        project(tile, ring[0], rb[0], 512, k2)
                        project(tile, ring[1], rb[1], 512, 2 + k2)
                        if tile >= 24:
                            project(tile, ring[2], rb[2], 512, 5 + k2)
                        if tile >= 1:
                            post(tile - 1)
                    post(NSLOT_T - 1)
                    t.op("dve", lambda e: e.tensor_scalar(out=kmTb[:], in0=kmT[:], scalar1=1.0 / 256, scalar2=None, op0=ALU.mult),
                         reads=[b_km], writes=[b_kmb])
                    if p == 0:
                        load_moba_w(1)
                    else:
                        load_w(0, 4096)
                        load_w(1, 5120)
                        load_w(2, 3072)
                    for i in range(NT):
                        r = i // 2
                        for h in range(NH):
                            t.op("pe", lambda e, h=h: e.matmul(ps[4][:, h * 16:(h + 1) * 16], QT[:, h, i * 128:(i + 1) * 128], kmTb[:, h, :],
                                                               start=True, stop=True),
                                 reads=[b_QT[i], b_kmb], writes=[pb[4]], inc=(h == NH - 1))
                        t.op("dve", lambda e: e.tensor_tensor(out=gm[:], in0=ps[4][:, 0:NH * 16].rearrange("p (h n) -> p h n", n=16),
                                                              in1=gbias[:, r, :].unsqueeze(1).broadcast_to([128, NH, 16]), op=ALU.add),
                             reads=[pb[4], b_gb], writes=[b_gm])
                        for h in range(NH):
                            t.op("dve", lambda e, h=h: e.max(out=t8[:, h, :], in_=gm[:, h, :]), reads=[b_gm], writes=[b_t8])
                        t.op("dve", lambda e: e.tensor_scalar(out=thr[:], in0=t8[:, :, 2], scalar1=-1e29, scalar2=None, op0=ALU.max),
                             reads=[b_t8], writes=[b_thr])
                        t.op("dve", lambda e: e.tensor_tensor(out=sel[:], in0=gm[:], in1=thr[:].unsqueeze(2).broadcast_to([128, NH, 16]),
                                                              op=ALU.is_ge), reads=[b_gm, b_thr], writes=[b_sel])
                        t.op("dve", lambda e: e.tensor_tensor(out=sel[:], in0=sel[:], in1=diag[:, r, :].unsqueeze(1).broadcast_to([128, NH, 16]),
                                                              op=ALU.max), reads=[b_sel, b_dg], writes=[b_sel])
                        t.op("dve", lambda e: e.tensor_scalar(out=selb[:], in0=sel[:], scalar1=-1.0, scalar2=30000.0, op0=ALU.add, op1=ALU.mult),
                             reads=[b_sel], writes=[b_selb])
                        for h in range(NH):
                            t.op("pe", lambda e, h=h: e.transpose(psb[4][0:16, 512 + h * 128:512 + (h + 1) * 128], selb[:, h, :], identb[:]),
                                 reads=[b_selb, b_idb], writes=[pb[4]], inc=(h == NH - 1))
                        t.op("act", lambda e: e.activation(out=TT[:, :, i * 128:(i + 1) * 128],
                                                           in_=psb[4][0:16, 512:512 + NH * 128].rearrange("p (h n) -> p h n", n=128), func=AF.Copy),
                             reads=[pb[4]], writes=[b_TT[i]])
                    nS = 0
                    for h in range(NH):
                        for half in range(2):
                            qsl = slice(half * 512, (half + 1) * 512)
                            bq = b_QT[4 * half:4 * half + 4]
                            btt = b_TT[4 * half:4 * half + 4]
                            nkt = 28 + 4 * half
                            ob, lb = (5, 6) if (2 * h + half) % 2 == 0 else (7, 4)
                            def emit_pv(kt, Pt, bP):
                                t.op("pe", lambda e: e.matmul(ps[ob][:], V[:, kt, h * 128:(h + 1) * 128], Pt[:], start=(kt == 0), stop=(kt == nkt - 1)),
                                     reads=[b_V[kt], bP], writes=[pb[ob]], inc=False)
                                t.op("pe", lambda e: e.matmul(ps[lb][:], onesb[:], Pt[:], start=(kt == 0), stop=(kt == nkt - 1)),
                                     reads=[b_ones, bP], writes=[pb[lb]], inc=True)
                            pend = None
                            for kt in range(nkt):
                                sb_ = nS % 4
                                Pt, bP = Pb[nS % 3], b_P[nS % 3]
                                nS += 1
                                dg = kt >= 24 + 4 * half
                                t.op("pe", lambda e: e.matmul(ps[sb_][:], KT[:, h, kt * 128:(kt + 1) * 128], QT[:, h, qsl], start=True, stop=False),
                                     reads=[b_KT[kt]] + bq, writes=[pb[sb_]], inc=False)
                                t.op("pe", lambda e: e.matmul(ps[sb_][:], oh16[:, kt // 2, :], TT[:, h, qsl], start=False, stop=not dg),
                                     reads=[b_oh] + btt, writes=[pb[sb_]], inc=not dg)
                                if dg:
                                    t.op("pe", lambda e: e.matmul(ps[sb_][:], identb[:], causal[:, kt - 24 - 4 * half, :], start=False, stop=True),
                                         reads=[b_idb, b_ca], writes=[pb[sb_]], inc=True)
                                t.op("act", lambda e: e.activation(out=Pt[:], in_=ps[sb_][:], func=AF.Exp, scale=SCALE), reads=[pb[sb_]], writes=[bP])
                                if pend is not None:
                                    emit_pv(*pend)
                                pend = (kt, Pt, bP)
                            emit_pv(*pend)
                            t.op("dve", lambda e: e.reciprocal(out=rl[:], in_=ps[lb][:]), reads=[pb[lb]], writes=[b_rl])
                            t.op("dve", lambda e: e.tensor_tensor(out=OT[:, 4 * p + h, qsl], in0=ps[ob][:], in1=rl[:], op=ALU.mult),
                                 reads=[pb[ob], b_rl], writes=b_OT[4 * half:4 * half + 4])
                t.barrier()

            with contextlib.ExitStack() as esr:
                cosRs = [self.sb(f"cosR{i}", [128, 8, 128], F32, esr) for i in range(2)]
                sinRs = [self.sb(f"sinR{i}", [128, 8, 128], F32, esr) for i in range(2)]
                rtabs = nc.dram_tensor("rtabs", [4, 2, 128, 1024], F32, kind="Internal").ap()
                b_rtabs = [Buf(f"rtabs{i}") for i in range(4)]
                angR = self.sb("angR", [128, 8, 128], F32, esr)
                invfR = self.sb("invfR", [128, 128], F32, esr)
                kB = self.sb("kB", [128, 8, 512], BF16, esr)
                vB = self.sb("vB", [128, 8, 512], BF16, esr)
                qB = self.sb("qB", [128, 8, 512], BF16, esr)
                gB = self.sb("gB", [128, 8, 512], BF16, esr)
                Sf = self.sb("Sf", [128, 2, 512], F32, esr)
                Sb = self.sb("Sb", [128, 2, 512], BF16, esr)
                rtbl = self.sb("rtbl", [128, 4, 128], F32, esr)
                rxi = self.sb("rxi", [128, 4], F32, esr)
                rkz = self.sb("rkz", [128, 4], F32, esr)
                r1 = self.sb("r1", [128, 2, 128], F32, esr)
                r2 = self.sb("r2", [128, 2, 128], F32, esr)
                r3 = self.sb("r3", [128, 2, 128], F32, esr)
                r4 = self.sb("r4", [128, 2, 128], F32, esr)
                kqTs = [self.sb(f"kqT{i}", [128, 8, 128], BF16, esr) for i in range(2)]
                ATps = [self.sb(f"ATp{i}", [128, 2, 128], BF16, esr) for i in range(2)]
                osb = self.sb("osb", [128, 512], F32, esr)
                osq = self.sb("osq", [128, 512], BF16, esr)
                onb = self.sb("onb", [128, 512], BF16, esr)
                Kz = self.sb("Kz", [128, 512], BF16, esr)
                st = self.sb("stR", [128, 5, 2], F32, esr)
                b_csrs, b_angr, b_ifr = [Buf("cosinR0"), Buf("cosinR1")], Buf("angR"), Buf("invfR")
                b_kB = [Buf(f"kB{i}") for i in range(8)]
                b_vB = [Buf(f"vB{i}") for i in range(8)]
                b_qB = [Buf(f"qB{i}") for i in range(8)]
                b_gB = [Buf(f"gB{i}") for i in range(8)]
                b_Sf = [Buf("Sf0"), Buf("Sf1")]
                b_Sb = [Buf("Sb0"), Buf("Sb1")]
                b_rc = Buf("retconst")
                b_r = [Buf(f"r{i}") for i in range(4)]
                b_osb, b_osq, b_onb, b_Kz, b_st = (Buf(n) for n in ("osb", "osq", "onb", "Kz", "stR"))
                b_kqTs = [Buf("kqT0"), Buf("kqT1")]
                b_ATps = [Buf("ATp0"), Buf("ATp1")]
                t.dma("sp", invfR[:], I["invfR"][:, :], writes=[b_ifr])
                t.dma("sp", rtbl[:], I["rettbl"][:, :, :], writes=[b_rc])
                t.dma("sp", rxi[:], I["retxi"][:, :], writes=[b_rc])
                t.dma("sp", rkz[:], I["retkz"][:, :], writes=[b_rc])
                gam = [1.0 - 2.0 ** (-5 - h) for h in range(4)]

                def make_tables(rp, sg):
                    if sg > 3:
                        return
                    cR, sR, bcs = cosRs[sg % 2], sinRs[sg % 2], b_csrs[sg % 2]
                    cf, sf = cR[:].rearrange("p a b -> p (a b)"), sR[:].rearrange("p a b -> p (a b)")
                    if rp == 0:
                        t.op("dve", lambda e: e.tensor_tensor(out=angR[:], in0=posf[:, sg * 8:(sg + 1) * 8].unsqueeze(2).broadcast_to([128, 8, 128]),
                                                              in1=invfR[:].unsqueeze(1).broadcast_to([128, 8, 128]), op=ALU.mult),
                             reads=[b_pos, b_ifr], writes=[b_angr])
                        sincos(angR[:].rearrange("p a b -> p (a b)"), 1024, sf, cf, b_angr, bcs)
                        t.dma("sp", rtabs[sg, 0], cf, reads=[bcs], writes=[b_rtabs[sg]])
                        t.dma("sp", rtabs[sg, 1], sf, reads=[bcs], writes=[b_rtabs[sg]])
                    else:
                        t.dma("sp", cf, rtabs[sg, 0], reads=[b_rtabs[sg]], writes=[bcs])
                        t.dma("sp", sf, rtabs[sg, 1], reads=[b_rtabs[sg]], writes=[bcs])

                def rotaryR(bank, tile_in_group, dst, b_dst, sg):
                    cosR, sinR, b_csr = cosRs[sg % 2], sinRs[sg % 2], b_csrs[sg % 2]
                    src = ps[bank][:].rearrange("p (h s d) -> p h s d", s=2, d=128)
                    d4 = dst.rearrange("p (h s d) -> p h s d", s=2, d=128)
                    cb = cosR[:, tile_in_group, :].unsqueeze(1).broadcast_to([128, 2, 128])
                    sn = sinR[:, tile_in_group, :].unsqueeze(1).broadcast_to([128, 2, 128])
                    x1, x2 = src[:, :, 0, :], src[:, :, 1, :]
                    t.op("dve", lambda e: e.tensor_tensor(out=r1[:], in0=x1, in1=cb, op=ALU.mult), reads=[pb[bank], b_csr], writes=[b_r[0]])
                    t.op("dve", lambda e: e.tensor_tensor(out=r2[:], in0=x2, in1=sn, op=ALU.mult), reads=[pb[bank], b_csr], writes=[b_r[1]])
                    t.op("dve", lambda e: e.tensor_tensor(out=r3[:], in0=x2, in1=cb, op=ALU.mult), reads=[pb[bank], b_csr], writes=[b_r[2]])
                    t.op("dve", lambda e: e.tensor_tensor(out=r4[:], in0=x1, in1=sn, op=ALU.mult), reads=[pb[bank], b_csr], writes=[b_r[3]])
                    t.op("pool", lambda e: e.tensor_tensor(out=d4[:, :, 0, :], in0=r1[:], in1=r2[:], op=ALU.subtract),
                         reads=[b_r[0], b_r[1]], writes=[b_dst])
                    t.op("pool", lambda e: e.tensor_tensor(out=d4[:, :, 1, :], in0=r3[:], in1=r4[:], op=ALU.add),
                         reads=[b_r[2], b_r[3]], writes=[b_dst])

                for rp in range(2):
                    t.op("dve", lambda e: e.memset(Sf[:], 0.0), writes=b_Sf)
                    t.op("dve", lambda e: e.memset(Sb[:], 0.0), writes=b_Sb)
                    for q in range(3):
                        load_hT(q)
                    make_tables(rp, 0)
                    for sg in range(4):

                        def postkv(i, tile):
                            k2 = tile % 2
                            rotaryR(k2, i, kB[:, i, :], b_kB[i], sg)
                            t.op("act", lambda e: e.activation(out=vB[:, i, :], in_=ps[2 + k2][:], func=AF.Copy, scale=vmask[:, tile:tile + 1]),
                                 reads=[pb[2 + k2], b_vm], writes=[b_vB[i]])

                        def postqg(i, tile):
                            k2 = tile % 2
                            rotaryR(k2, i, qB[:, i, :], b_qB[i], sg)
                            t.op("act", lambda e: e.activation(out=gB[:, i, :], in_=ps[2 + k2][:], func=AF.Silu), reads=[pb[2 + k2]], writes=[b_gB[i]])

                        for i in range(8):
                            tile = sg * 8 + i
                            k2 = tile % 2
                            load_hT(tile + 3 if not (sg == 3 and i >= 5) else -1)
                            project(tile, ring[0], rb[0], 512, k2)
                            project(tile, ring[1], rb[1], 512, 2 + k2)
                            if i >= 1:
                                postkv(i - 1, tile - 1)
                            if i == 3:
                                make_tables(rp, sg + 1)
                        postkv(7, sg * 8 + 7)
                        if sg == 3:
                            load_w(0, 6144 + 512 * rp)
                            for q in range(3):
                                load_hT(24 + q)
                            for i in range(8):
                                tile = 24 + i
                                k2 = tile % 2
                                load_hT(tile + 3)
                                project(tile, ring[2], rb[2], 512, k2)
                                project(tile, ring[0], rb[0], 512, 2 + k2)
                                if i >= 1:
                                    postqg(i - 1, tile - 1)
                            postqg(7, 31)
                            if rp == 0:
                                load_w(0, 4096 + 512)
                                load_w(1, 5120 + 512)
                                load_w(2, 3072 + 512)
                            else:
                                for cg in range(4):
                                    t.dma("pool", ring[cg][:].rearrange("p (k n) -> p k n", n=512),
                                          I["w_out"][:, cg * 512:(cg + 1) * 512].rearrange("(k p) n -> p k n", p=128),
                                          writes=[rb[cg]] + (b_hTt if cg == 3 else []), sem=cg)
                        h0 = 2 * rp

                        def partA(i):
                            for hh in range(2):
                                for q_, (src_, bsrc) in enumerate(((kB, b_kB[i]), (qB, b_qB[i]))):
                                    for dc in range(2):
                                        col = (4 * hh + 2 * q_ + dc) * 128
                                        t.op("pe", lambda e, col=col, dc=dc, src_=src_, hh=hh: e.transpose(
                                            psb[4][:, col:col + 128], src_[:, i, hh * 256 + dc * 128:hh * 256 + (dc + 1) * 128], identb[:]),
                                            reads=[bsrc, b_idb], writes=[pb[4]], inc=(hh == 1 and q_ == 1 and dc == 1))
                            t.op("act", lambda e: e.activation(out=kqTs[i % 2][:], in_=psb[4][:].rearrange("p (a n) -> p a n", n=128), func=AF.Copy),
                                 reads=[pb[4]], writes=[b_kqTs[i % 2]])
                            for hh in range(2):
                                for dc in range(2):
                                    t.op("pe", lambda e, dc=dc, hh=hh: e.matmul(ps[5][:, hh * 128:(hh + 1) * 128], kqTs[i % 2][:, 4 * hh + dc, :], kqTs[i % 2][:, 4 * hh + 2 + dc, :],
                                                                                start=(dc == 0), stop=(dc == 1)),
                                         reads=[b_kqTs[i % 2]], writes=[pb[5]], inc=(hh == 1 and dc == 1))
                            t.op("dve", lambda e: e.tensor_tensor(out=ATps[i % 2][:], in0=ps[5][:, 0:256].rearrange("p (a n) -> p a n", n=128),
                                                                  in1=rtbl[:, h0:h0 + 2, :], op=ALU.mult),
                                 reads=[pb[5], b_rc], writes=[b_ATps[i % 2]])

                        def partB(i):
                            for hh in range(2):
                                hs = slice(hh * 256, (hh + 1) * 256)
                                t.op("pe", lambda e: e.matmul(ps[6][:, hs], ATps[i % 2][:, hh, :], vB[:, i, hs], start=True, stop=False),
                                     reads=[b_ATps[i % 2], b_vB[i]], writes=[pb[6]], inc=False)
                                for dc in range(2):
                                    t.op("pe", lambda e, dc=dc: e.matmul(ps[6][:, hs], kqTs[i % 2][:, 4 * hh + 2 + dc, :], Sb[:, hh, dc * 256:(dc + 1) * 256],
                                                                         start=False, stop=(dc == 1)),
                                         reads=[b_kqTs[i % 2], b_Sb[hh]], writes=[pb[6]], inc=(dc == 1))
                            for hh in range(2):
                                hs = slice(hh * 256, (hh + 1) * 256)
                                t.op("act", lambda e: e.activation(out=osb[:, hs], in_=ps[6][:, hs], func=AF.Copy, scale=rxi[:, h0 + hh:h0 + hh + 1],
                                                                   accum_out=st[:, 0, hh:hh + 1]), reads=[pb[6], b_rc], writes=[b_osb, b_st])
                            for hh in range(2):
                                hs = slice(hh * 256, (hh + 1) * 256)
                                t.op("act", lambda e: e.activation(out=osq[:, hs], in_=osb[:, hs], func=AF.Square, accum_out=st[:, 1, hh:hh + 1]),
                                     reads=[b_osb], writes=[b_osq, b_st])
                            t.op("dve", lambda e: e.tensor_scalar(out=st[:, 2, :], in0=st[:, 0, :], scalar1=1.0 / 256, scalar2=None, op0=ALU.mult),
                                 reads=[b_st], writes=[b_st])
                            t.op("dve", lambda e: e.tensor_tensor(out=st[:, 3, :], in0=st[:, 2, :], in1=st[:, 2, :], op=ALU.mult),
                                 reads=[b_st], writes=[b_st])
                            t.op("dve", lambda e: e.scalar_tensor_tensor(out=st[:, 3, :], in0=st[:, 1, :], scalar=1.0 / 256, in1=st[:, 3, :],
                                                                         op0=ALU.mult, op1=ALU.subtract), reads=[b_st], writes=[b_st])
                            t.op("act", lambda e: e.activation(out=st[:, 4, :], in_=st[:, 3, :], func=AF.Sqrt, bias=EPS, scale=1.0),
                                 reads=[b_st], writes=[b_st])
                            t.op("dve", lambda e: e.reciprocal(out=st[:, 4, :], in_=st[:, 4, :]), reads=[b_st], writes=[b_st])
                            for hh in range(2):
                                hs = slice(hh * 256, (hh + 1) * 256)
                                t.op("dve", lambda e: e.tensor_scalar(out=osb[:, hs], in0=osb[:, hs], scalar1=st[:, 2, hh:hh + 1], scalar2=st[:, 4, hh:hh + 1],
                                                                      op0=ALU.subtract, op1=ALU.mult), reads=[b_osb, b_st], writes=[b_osb])
                            t.op("pool", lambda e: e.tensor_tensor(out=onb[:], in0=osb[:], in1=gB[:, i, :], op=ALU.mult),
                                 reads=[b_osb, b_gB[i]], writes=[b_onb])
                            for a_ in range(4):
                                t.op("pe", lambda e, a_=a_: e.transpose(psb[1][:, a_ * 128:(a_ + 1) * 128], onb[:, a_ * 128:(a_ + 1) * 128], identb[:]),
                                     reads=[b_onb, b_idb], writes=[pb[1]], inc=(a_ == 3))
                            t.op("act", lambda e: e.activation(out=OT[:, 8 + 2 * h0:8 + 2 * h0 + 4, i * 128:(i + 1) * 128],
                                                               in_=psb[1][:, 0:512].rearrange("p (a n) -> p a n", n=128), func=AF.Copy),
                                 reads=[pb[1]], writes=[b_OT[i]])

                        def upd(i):
                            t.op("pool", lambda e: e.tensor_tensor(out=Kz[:].rearrange("p (a n) -> p a n", n=256),
                                                                   in0=kB[:, i, :].rearrange("p (a n) -> p a n", n=256),
                                                                   in1=rkz[:, h0:h0 + 2].unsqueeze(2).broadcast_to([128, 2, 256]), op=ALU.mult),
                                 reads=[b_kB[i], b_rc], writes=[b_Kz])
                            for hh in range(2):
                                hs = slice(hh * 256, (hh + 1) * 256)
                                sbank = 7 if hh == 0 else 3
                                for dc in range(2):
                                    t.op("pe", lambda e, dc=dc: e.matmul(ps[sbank][:, dc * 256:(dc + 1) * 256], Kz[:, hh * 256 + dc * 128:hh * 256 + (dc + 1) * 128],
                                                                         vB[:, i, hs], start=True, stop=True),
                                         reads=[b_Kz, b_vB[i]], writes=[pb[sbank]], inc=(dc == 1))
                            for hh in range(2):
                                sbank = 7 if hh == 0 else 3
                                t.op("dve", lambda e: e.scalar_tensor_tensor(out=Sf[:, hh, :], in0=Sf[:, hh, :], scalar=float(gam[h0 + hh] ** 128), in1=ps[sbank][:],
                                                                             op0=ALU.mult, op1=ALU.add), reads=[pb[sbank], b_Sf[hh]], writes=[b_Sf[hh]])
                            for hh in range(2):
                                t.op("act", lambda e: e.activation(out=Sb[:, hh, :], in_=Sf[:, hh, :], func=AF.Copy), reads=[b_Sf[hh]], writes=[b_Sb[hh]])

                        if sg == 3:
                            partA(0)
                            for i in range(8):
                                if i + 1 < 8:
                                    partA(i + 1)
                                partB(i)
                                upd(i)
                        else:
                            for i in range(8):
                                upd(i)
                t.barrier()

            with contextlib.ExitStack() as eso:
                garow = self.sb("garow", [128, D], F32, eso)
                xo = [self.sb(f"xo{i}", [128, D], F32, eso) for i in range(2)]
                tmpo = [self.sb(f"tmpo{i}", [128, 512], F32, eso) for i in range(2)]
                b_ga = Buf("garow")
                b_xo = [Buf("xo0"), Buf("xo1")]
                b_tmpo = [Buf("tmpo0"), Buf("tmpo1")]
                t.dma("sp", garow[:], self.modrows[2:3, :].partition_broadcast(128), reads=[self.b_modrows[2]], writes=[b_ga])
                nb = 0
                def load_xo(i):
                    if i < NT:
                        t.dma("sp", xo[i % 2][:], I["xs"][(24 + i) * 128:(25 + i) * 128, :], writes=[b_xo[i % 2]])
                load_xo(0)
                load_xo(1)
                for i in range(NT):
                    x_, bx = xo[i % 2], b_xo[i % 2]
                    for cg in range(4):
                        bank = nb % 4
                        tm, btm = tmpo[nb % 2], b_tmpo[nb % 2]
                        nb += 1
                        for c in range(NKC):
                            t.op("pe", lambda e, c=c: e.matmul(ps[bank][:], OT[:, c, i * 128:(i + 1) * 128], ring[cg][:, c * 512:(c + 1) * 512],
                                                               start=(c == 0), stop=(c == NKC - 1)),
                                 reads=[b_OT[i], rb[cg]], writes=[pb[bank]], inc=(c == NKC - 1))
                        t.op("dve", lambda e: e.tensor_tensor(out=tm[:], in0=ps[bank][:], in1=garow[:, cg * 512:(cg + 1) * 512], op=ALU.mult),
                             reads=[pb[bank], b_ga], writes=[btm])
                        t.op("pool", lambda e: e.tensor_tensor(out=x_[:, cg * 512:(cg + 1) * 512], in0=x_[:, cg * 512:(cg + 1) * 512], in1=tm[:], op=ALU.add),
                             reads=[btm, bx], writes=[bx])
                    t.dma("sp", self.x1s[i * 128:(i + 1) * 128, :], x_[:], reads=[bx], writes=[self.b_x1s])
                    load_xo(i + 2)
                t.barrier()

    def build(self):
        nc = self.nc
        I = self.ins
        ne = self.ne
        self.din("c_col", [128, NKC])
        self.din("w_ada", [D, 6 * D])
        self.din("b_ada", [1, 6 * D])
        self.din("norm_mix", [1, D])
        self.din("norm_ffn", [1, D])
        self.din("norm_out", [1, D])
        self.din("w_router", [D, 64])
        self.din("router_bias", [1, 64])
        self.din("w_gate", [ne, D, 512])
        self.din("w_up", [ne, D, 512])
        self.din("w_down", [ne, 512, D])
        self.din("w_sh_gate", [D, 512])
        self.din("w_sh_up", [D, 512])
        self.din("w_sh_down", [512, D])
        self.din("ident", [128, 128])
        if self.mode in ("testA", "full"):
            self.din("xs", [NSLOT_T * 128, D])
            self.din("pos_i", [128, NSLOT_T], mybir.dt.int32)
            self.din("vmask", [128, NSLOT_T])
            self.din("w_in", [D, 7168])
            self.din("w_out", [D, D])
            self.din("invfA", [128, 16])
            self.din("invfR", [128, 128])
            self.din("gatebias", [128, 4, 16])
            self.din("diag", [128, 4, 16])
            self.din("oh16", [16, 16, 128])
            self.din("causal", [128, 4, 512])
            self.din("rettbl", [128, 4, 128])
            self.din("retxi", [128, 4])
            self.din("retkz", [128, 4])
        if self.mode == "testB":
            self.x1s = self.din("x1s", [NT * 128, D])
        elif self.mode == "testA":
            self.x1s = nc.dram_tensor("x1s", [NT * 128, D], F32, kind="ExternalOutput").ap()
        else:
            self.x1s = nc.dram_tensor("x1s", [NT * 128, D], F32, kind="Internal").ap()
        self.b_x1s = Buf("x1s")
        self.modrows = nc.dram_tensor("modrows", [8, D], F32, kind="Internal").ap()
        self.b_modrows = [Buf(f"modrows{v}") for v in range(6)]
        if self.mode != "testA":
            self.out = nc.dram_tensor("out", [NT * 128, D], F32, kind="ExternalOutput").ap()
        es = self.es
        ps = [es.enter_context(nc.psum_tensor(f"ps{i}", [128, 512], F32)) for i in range(8)]
        pb = [Buf(f"ps{i}") for i in range(8)]
        ring = [self.sb(f"ring{i}", [128, NKC * 512], BF16) for i in range(4)]
        rb = [Buf(f"ring{i}") for i in range(4)]
        ident32 = self.sb("ident32", [128, 128], F32)
        b_id = Buf("ident")
        t = Trk(nc)
        self.t = t
        t.dma("sp", ident32[:], I["ident"][:, :], writes=[b_id])
        if self.mode not in ("testA", "full"):
            with contextlib.ExitStack() as es0:
                for _ in self.phase0_gen(t, ps, pb, ring, rb, es0):
                    pass
                t.barrier()
        if self.mode == "test0":
            bo = Buf("o")
            t.dma("sp", self.out[0:6, :], self.modrows[0:6, :], reads=self.b_modrows, writes=[bo])
            t.drain("sp", [bo])
        if self.mode in ("testA", "full"):
            self.phaseA(t, ps, pb, ring, rb, ident32, b_id)
        if self.mode in ("testB", "full"):
            self.phaseB(t, ps, pb, ring, rb, ident32, b_id)
        t.close()
        es.close()
        return nc


def _common_inputs(inputs, b, ne=NE):
    f = np.ascontiguousarray
    return {
        "c_col": f(inputs["c"][b].reshape(NKC, 128).T),
        "w_ada": inputs["w_ada"][0],
        "b_ada": inputs["b_ada"][0:1],
        "norm_mix": inputs["norm_mix"][0:1],
        "norm_ffn": inputs["norm_ffn"][0:1],
        "norm_out": inputs["norm_out"].reshape(1, D),
        "w_router": inputs["w_router"][0],
        "router_bias": inputs["router_bias"][0:1],
        "w_gate": inputs["w_gate"][0][:ne],
        "w_up": inputs["w_up"][0][:ne],
        "w_down": inputs["w_down"][0][:ne],
        "w_sh_gate": inputs["w_sh_gate"][0],
        "w_sh_up": inputs["w_sh_up"][0],
        "w_sh_down": inputs["w_sh_down"][0],
        "ident": np.eye(128, dtype=np.float32),
    }


def _phaseA_inputs(inputs, b, j):
    f32 = np.float32
    own_end = 1024 * (j + 1)
    start = own_end - 4096
    lo = max(start, 0)
    xs = np.zeros((4096, D), f32)
    xs[lo - start:] = inputs["x"][b, lo:own_end]
    pos = np.zeros((4096,), np.int32)
    pos[lo - start:] = inputs["positions"][b, lo:own_end]
    tile_valid = ((np.arange(NSLOT_T) * 128 + start) >= 0).astype(f32)
    r = np.arange(4)[:, None]
    kb = np.arange(16)[None, :]
    gatebias = np.where((kb < 12 + r) & (kb >= 12 - 4 * j), 0.0, -1e30).astype(f32)
    diag = (kb >= 12 + r).astype(f32)
    oh16 = (np.arange(16)[:, None, None] == np.arange(16)[None, :, None]) * np.ones((1, 1, 128))
    pp = np.arange(128)[:, None, None]
    dd = np.arange(4)[None, :, None]
    cc = np.arange(512)[None, None, :]
    causal = np.where(128 * dd + pp <= cc, 0.0, -30000.0)
    invfA = (np.float32(500000.0) ** (-(np.arange(16, dtype=f32) * f32(2.0) / f32(32.0)))).astype(f32)
    invfR = (np.float32(10000.0) ** (-np.linspace(0.0, 1.0, 128, dtype=f32))).astype(f32)
    gam = 1.0 - 2.0 ** (-5.0 - np.arange(4))
    m = np.arange(128)
    rettbl = (gam[None, :, None] ** (-(m[:, None, None] + 1.0))) / 16.0 * (m[None, None, :] >= m[:, None, None])
    retxi = gam[None, :] ** (m[:, None] + 1.0)
    retkz = gam[None, :] ** (127.0 - m[:, None]) / 16.0
    c = np.ascontiguousarray
    return {
        "xs": xs,
        "pos_i": c(pos.reshape(NSLOT_T, 128).T),
        "vmask": c(np.broadcast_to(tile_valid[None, :], (128, NSLOT_T))).astype(f32),
        "w_in": inputs["w_in"][0],
        "w_out": inputs["w_out"][0],
        "invfA": c(np.broadcast_to(invfA[None, :], (128, 16))).astype(f32),
        "invfR": c(np.broadcast_to(invfR[None, :], (128, 128))).astype(f32),
        "gatebias": c(np.broadcast_to(gatebias[None], (128, 4, 16))).astype(f32),
        "diag": c(np.broadcast_to(diag[None], (128, 4, 16))).astype(f32),
        "oh16": c(oh16).astype(f32),
        "causal": c(causal).astype(f32),
        "rettbl": c(rettbl).astype(f32),
        "retxi": c(retxi).astype(f32),
        "retkz": c(retkz).astype(f32),
    }


_PROG = {}


def kernel(**inputs):
    inputs = {k: np.asarray(v) for k, v in inputs.items()}
    if "full" not in _PROG:
        _PROG["full"] = Prog(mode="full").build()
    nc = _PROG["full"]
    in_maps = []
    for core in range(8):
        b, j = core // 4, core % 4
        im = _common_inputs(inputs, b)
        im.update(_phaseA_inputs(inputs, b, j))
        in_maps.append(im)
    res = run_bass_kernel_spmd(nc, in_maps, core_ids=list(range(8)))
    out = np.empty((2, 4096, D), np.float32)
    for core in range(8):
        b, j = core // 4, core % 4
        out[b, 1024 * j:1024 * (j + 1)] = res.results[core]["out"]
    return out
```

```python
import contextlib
import numpy as np
import concourse.bass as bass
import concourse.mybir as mybir
from concourse.bass_utils import run_bass_kernel_spmd

F32 = mybir.dt.float32
BF16 = mybir.dt.bfloat16
AF = mybir.ActivationFunctionType
ALU = mybir.AluOpType
AX = mybir.AxisListType

D = 2048
NKC = 16
NE = 64
EPS = 1e-6
NT = 8
NSLOT_T = 32


class Buf:
    __slots__ = ("name", "w", "r")

    def __init__(self, name):
        self.name = name
        self.w = None
        self.r = []


class Trk:
    def __init__(self, nc, n_dma_sems=28, n_fixed=8):
        self.nc = nc
        self.sems = {}
        self.seen = {}
        self.engines = {"pe": nc.tensor, "act": nc.scalar, "dve": nc.vector, "pool": nc.gpsimd, "sp": nc.sync}
        self._ctx = []
        for k in ("pe", "act", "dve", "pool"):
            self._mk(k)
        self.n_dma = 0
        self.n_dma_pool = 0
        self.n_fixed = n_fixed
        self.n_dma_sems = n_dma_sems
        for i in range(n_dma_sems):
            self._mk(("dma", i))

    def _mk(self, key):
        nm = "s_" + "".join(ch for ch in str(key) if ch.isalnum())
        cm = self.nc.semaphore(nm)
        h = cm.__enter__()
        self._ctx.append(cm)
        self.sems[key] = [h, 0]
        for e in self.engines:
            self.seen.setdefault(e, {})[key] = 0

    def close(self):
        for cm in reversed(self._ctx):
            cm.__exit__(None, None, None)

    def _wait(self, ename, dep):
        if dep is None:
            return
        key, val = dep
        if self.seen[ename].get(key, 0) >= val:
            return
        self.engines[ename].wait_ge(self.sems[key][0], val)
        self.seen[ename][key] = val

    def _deps(self, ename, reads, writes):
        for b in reads:
            if b.w is not None and not (ename == "pe" and b.w[0] == "pe"):
                self._wait(ename, b.w)
        for b in writes:
            if b.w is not None and not (ename == "pe" and b.w[0] == "pe"):
                self._wait(ename, b.w)
            for d in b.r:
                if not (ename == "pe" and d[0] == "pe"):
                    self._wait(ename, d)

    def _record(self, key, dep, reads, writes):
        for b in writes:
            b.w = dep
            b.r = []
        for b in reads:
            b.r = [d for d in b.r if d[0] != key] + [dep]

    def op(self, ename, fn, reads=(), writes=(), inc=True):
        self._deps(ename, reads, writes)
        ins = fn(self.engines[ename])
        s = self.sems[ename]
        if inc:
            s[1] += 1
            ins.then_inc(s[0], 1)
            val = s[1]
        else:
            val = s[1] + 1
        self._record(ename, (ename, val), reads, writes)
        return ins

    def dma(self, qname, out, in_, reads=(), writes=(), sem=None):
        if sem is None:
            if qname == "pool":
                sem = 4 + self.n_dma_pool % 4
                self.n_dma_pool += 1
            else:
                sem = self.n_fixed + self.n_dma % (self.n_dma_sems - self.n_fixed)
                self.n_dma += 1
        key = ("dma", sem)
        s = self.sems[key]
        if s[1] > 0:
            self._wait(qname, (key, s[1]))
        self._deps(qname, reads, writes)
        ins = self.engines[qname].dma_start(out=out, in_=in_)
        s[1] += 16
        ins.then_inc(s[0], 16)
        self._record(key, (key, s[1]), reads, writes)
        return ins

    def drain(self, ename, bufs):
        for b in bufs:
            self._wait(ename, b.w)

    def barrier(self):
        for e in self.engines:
            for key, s in self.sems.items():
                if s[1] > 0 and key != e:
                    self._wait(e, (key, s[1]))


class Prog:
    def __init__(self, mode="full", ne=NE):
        self.mode = mode
        self.ne = ne
        self.nc = bass.Bass("TRN2", target_bir_lowering=False)
        self.es = contextlib.ExitStack()
        self.ins = {}

    def din(self, name, shape, dt=F32):
        ap = self.nc.dram_tensor(name, list(shape), dt, kind="ExternalInput").ap()
        self.ins[name] = ap
        return ap

    def sb(self, name, shape, dt, es=None):
        return (es or self.es).enter_context(self.nc.sbuf_tensor("sb_" + name, list(shape), dt))

    def phase0_gen(self, t, ps, pb, ring, rb, es):
        I = self.ins
        ccol = self.sb("ccol", [128, NKC], F32, es)
        csil = self.sb("csil", [128, NKC], F32, es)
        crep = self.sb("crep", [128, NKC, 128], BF16, es)
        rowt = [self.sb(f"rowt{i}", [128, D], F32, es) for i in range(2)]
        nrow = self.sb("nrow", [128, D], F32, es)
        b_c, b_crep = Buf("ccol"), Buf("crep")
        b_rowt = [Buf("rowt0"), Buf("rowt1")]
        b_nrow = Buf("nrow")
        t.dma("sp", ccol[:], I["c_col"][:, :], writes=[b_c])
        t.op("act", lambda e: e.activation(out=csil[:], in_=ccol[:], func=AF.Silu), reads=[b_c], writes=[b_c])
        t.op("dve", lambda e: e.tensor_copy(out=crep[:], in_=csil[:].unsqueeze(2).broadcast_to([128, NKC, 128])),
             reads=[b_c], writes=[b_crep])

        def load(i):
            col0 = i * 512
            t.dma("pool", ring[i % 4][:].rearrange("p (k n) -> p k n", n=512),
                  I["w_ada"][:, col0:col0 + 512].rearrange("(k p) n -> p k n", p=128),
                  writes=[rb[i % 4]], sem=i % 4)
        for i in range(3):
            load(i)
        for v in range(6):
            rt = rowt[v % 2]
            t.dma("sp", rt[:], I["b_ada"][0:1, v * D:(v + 1) * D].partition_broadcast(128), writes=[b_rowt[v % 2]])
            if v in (1, 4):
                src = I["norm_mix"] if v == 1 else I["norm_ffn"]
                t.dma("sp", nrow[:], src[0:1, :].partition_broadcast(128), writes=[b_nrow])
            for cg in range(4):
                i = v * 4 + cg
                if i + 3 < 24:
                    load(i + 3)
                slot = i % 4
                bank = 6 + cg % 2
                for k in range(NKC):
                    t.op("pe", lambda e, k=k: e.matmul(ps[bank][:], crep[:, k, :], ring[slot][:, k * 512:(k + 1) * 512],
                                                       start=(k == 0), stop=(k == NKC - 1)),
                         reads=[b_crep, rb[slot]], writes=[pb[bank]], inc=(k == NKC - 1))
                t.op("dve", lambda e: e.tensor_tensor(out=rt[:, cg * 512:(cg + 1) * 512], in0=ps[bank][:],
                                                      in1=rt[:, cg * 512:(cg + 1) * 512], op=ALU.add),
                     reads=[pb[bank], b_rowt[v % 2]], writes=[b_rowt[v % 2]])
                if cg == 3:
                    if v in (1, 4):
                        t.op("dve", lambda e: e.scalar_tensor_tensor(out=rt[:], in0=rt[:], scalar=1.0, in1=nrow[:],
                                                                     op0=ALU.add, op1=ALU.mult),
                             reads=[b_rowt[v % 2], b_nrow], writes=[b_rowt[v % 2]])
                    t.dma("sp", self.modrows[v:v + 1, :], rt[0:1, :], reads=[b_rowt[v % 2]], writes=[self.b_modrows[v]])
                yield (v, cg)

    def routing(self, t, R, lg_ap, bl, rbrow, b_rb, wn_out, bw):
        sc, bi, m1, eq, g2, m2, t8, gm, bm, e8, ws = (R[k] for k in ("sc", "bi", "m1", "eq", "g2", "m2", "t8", "gm", "bm", "e8", "ws"))
        B = R["B"]

        def v3(x):
            return x[:].rearrange("p (g k) -> p g k", k=8)

        def bc(x):
            return x[:].unsqueeze(2).broadcast_to([128, 8, 8])
        t.op("act", lambda e: e.activation(out=sc[:], in_=lg_ap, func=AF.Sigmoid), reads=[bl], writes=[B["sc"]])
        t.op("dve", lambda e: e.tensor_tensor(out=bi[:], in0=sc[:], in1=rbrow[:], op=ALU.add), reads=[B["sc"], b_rb], writes=[B["bi"]])
        t.op("dve", lambda e: e.tensor_reduce(out=m1[:], in_=v3(bi), axis=AX.X, op=ALU.max), reads=[B["bi"]], writes=[B["m1"]])
        t.op("dve", lambda e: e.tensor_tensor(out=v3(eq), in0=v3(bi), in1=bc(m1), op=ALU.is_equal), reads=[B["bi"], B["m1"]], writes=[B["eq"]])
        t.op("dve", lambda e: e.scalar_tensor_tensor(out=g2[:], in0=eq[:], scalar=-1e30, in1=bi[:], op0=ALU.mult, op1=ALU.add),
             reads=[B["eq"], B["bi"]], writes=[B["g2"]])
        t.op("dve", lambda e: e.tensor_reduce(out=m2[:], in_=v3(g2), axis=AX.X, op=ALU.max), reads=[B["g2"]], writes=[B["m2"]])
        t.op("dve", lambda e: e.tensor_tensor(out=m2[:], in0=m2[:], in1=m1[:], op=ALU.add), reads=[B["m2"], B["m1"]], writes=[B["m2"]])
        t.op("dve", lambda e: e.max(out=t8[:], in_=m2[:]), reads=[B["m2"]], writes=[B["t8"]])
        t.op("dve", lambda e: e.tensor_scalar(out=gm[:], in0=m2[:], scalar1=t8[:, 3:4], scalar2=None, op0=ALU.is_ge),
             reads=[B["m2"], B["t8"]], writes=[B["gm"]])
        t.op("dve", lambda e: e.tensor_scalar(out=t8[:], in0=gm[:], scalar1=-1.0, scalar2=1e30, op0=ALU.add, op1=ALU.mult),
             reads=[B["gm"]], writes=[B["t8"]])
        t.op("dve", lambda e: e.tensor_tensor(out=v3(bm), in0=v3(bi), in1=bc(gm), op=ALU.mult), reads=[B["bi"], B["gm"]], writes=[B["bm"]])
        t.op("dve", lambda e: e.tensor_tensor(out=v3(bm), in0=v3(bm), in1=bc(t8), op=ALU.add), reads=[B["bm"], B["t8"]], writes=[B["bm"]])
        t.op("dve", lambda e: e.max(out=e8[:], in_=bm[:]), reads=[B["bm"]], writes=[B["e8"]])
        t.op("dve", lambda e: e.tensor_scalar(out=eq[:], in0=bm[:], scalar1=e8[:, 7:8], scalar2=None, op0=ALU.is_ge),
             reads=[B["bm"], B["e8"]], writes=[B["eq"]])
        t.op("dve", lambda e: e.tensor_tensor(out=g2[:], in0=eq[:], in1=sc[:], op=ALU.mult), reads=[B["eq"], B["sc"]], writes=[B["g2"]])
        t.op("dve", lambda e: e.tensor_reduce(out=ws[:], in_=g2[:], axis=AX.X, op=ALU.add), reads=[B["g2"]], writes=[B["ws"]])
        t.op("dve", lambda e: e.reciprocal(out=ws[:], in_=ws[:]), reads=[B["ws"]], writes=[B["ws"]])
        t.op("dve", lambda e: e.tensor_scalar(out=wn_out, in0=g2[:], scalar1=ws[:, 0:1], scalar2=2.5, op0=ALU.mult, op1=ALU.mult),
             reads=[B["g2"], B["ws"]], writes=[bw])

    def phaseB(self, t, ps, pb, ring, rb, ident32, b_id):
        nc = self.nc
        I = self.ins
        ne = self.ne
        with contextlib.ExitStack() as es:
            h2T = self.sb("h2T", [128, NKC, NT * 128], BF16, es)
            wn = self.sb("wn", [128, NT, 64], F32, es)
            ones1 = self.sb("ones1", [128, 1], F32, es)
            b_y = [[Buf(f"y{i}_{c}") for c in range(4)] for i in range(NT)]
            b_h2T = [Buf(f"h2T{i}") for i in range(NT)]
            b_wn = [Buf(f"wn{i}") for i in range(NT)]
            b_act = [[Buf(f"act{h}_{f}") for f in range(4)] for h in range(2)]
            b_sil = [Buf("sil0"), Buf("sil1")]
            b_one = Buf("ones1")
            t.op("dve", lambda e: e.memset(ones1[:], 1.0), writes=[b_one])
            mats = []
            for e_ in range(ne):
                mats += [("g", I["w_gate"][e_]), ("g", I["w_up"][e_]), ("d", I["w_down"][e_])]
            mats += [("g", I["w_sh_gate"]), ("g", I["w_sh_up"]), ("d", I["w_sh_down"])]
            state = {"issued": 0, "consumed": 0}

            def pump():
                while state["issued"] < len(mats) and state["issued"] - 4 < state["consumed"]:
                    i = state["issued"]
                    kind, src = mats[i]
                    slot = i % 4
                    if kind == "g":
                        t.dma("pool", ring[slot][:].rearrange("p (k n) -> p k n", n=512),
                              src.rearrange("(k p) n -> p k n", p=128), writes=[rb[slot]], sem=slot)
                    else:
                        t.dma("pool", ring[slot][:].rearrange("p (k n) -> p k n", n=D),
                              src.rearrange("(k p) n -> p k n", p=128), writes=[rb[slot]], sem=slot)
                    state["issued"] += 1
            pump()
            with contextlib.ExitStack() as es1:
                g2row = self.sb("g2row", [128, D], F32, es1)
                shfrow = self.sb("shfrow", [128, D], F32, es1)
                xt = [self.sb(f"xtB{i}", [128, D], F32, es1) for i in range(2)]
                hf = self.sb("hfB", [128, D], F32, es1)
                sq = self.sb("sqB", [128, D], BF16, es1)
                hT32 = self.sb("hT32", [128, NKC, 128], F32, es1)
                wr32 = self.sb("wr32", [128, NKC, 64], F32, es1)
                rbrow = self.sb("rbrow", [128, 64], F32, es1)
                ss = self.sb("ssB", [128, 2], F32, es1)
                R = {k: self.sb("rt_" + k, [128, n], F32, es1) for k, n in
                     (("sc", 64), ("bi", 64), ("m1", 8), ("eq", 64), ("g2", 64), ("m2", 8), ("t8", 8), ("gm", 8),
                      ("bm", 64), ("e8", 8), ("ws", 1))}
                R["B"] = {k: Buf("rt_" + k) for k in ("sc", "bi", "m1", "eq", "g2", "m2", "t8", "gm", "bm", "e8", "ws")}
                b_g2, b_shf, b_wr, b_rbr = Buf("g2row"), Buf("shfrow"), Buf("wr32"), Buf("rbrow")
                b_xt = [Buf("xt0"), Buf("xt1")]
                b_hf, b_sq, b_hT32, b_ss = Buf("hf"), Buf("sq"), Buf("hT32"), Buf("ss")
                t.dma("sp", g2row[:], self.modrows[4:5, :].partition_broadcast(128), reads=[self.b_modrows[4]], writes=[b_g2])
                t.dma("sp", shfrow[:], self.modrows[3:4, :].partition_broadcast(128), reads=[self.b_modrows[3]], writes=[b_shf])
                t.dma("sp", wr32[:], I["w_router"].rearrange("(k p) n -> p k n", p=128), writes=[b_wr])
                t.dma("sp", rbrow[:], I["router_bias"][0:1, :].partition_broadcast(128), writes=[b_rbr])
                hfs = [hf, self.sb("hfB1", [128, D], F32, es1)]
                b_hfs = [b_hf, Buf("hf1")]
                ss4 = self.sb("ssB4", [128, 4], F32, es1)
                b_ss4 = [Buf("ssB40"), Buf("ssB41")]

                def normB(i):
                    if i >= NT:
                        return
                    x_, bx = xt[i % 2], b_xt[i % 2]
                    hf_, bhf = hfs[i % 2], b_hfs[i % 2]
                    sa, sb2, bss = ss4[:, 2 * (i % 2):2 * (i % 2) + 1], ss4[:, 2 * (i % 2) + 1:2 * (i % 2) + 2], b_ss4[i % 2]
                    t.dma("sp", x_[:], self.x1s[i * 128:(i + 1) * 128, :], reads=[self.b_x1s], writes=[bx])
                    t.op("act", lambda e: e.activation(out=sq[:], in_=x_[:], func=AF.Square, accum_out=sa), reads=[bx], writes=[b_sq, bss])
                    t.op("act", lambda e: e.activation(out=sb2, in_=sa, func=AF.Sqrt, bias=EPS, scale=1.0 / D), reads=[bss], writes=[bss])
                    t.op("dve", lambda e: e.reciprocal(out=sb2, in_=sb2), reads=[bss], writes=[bss])
                    t.op("dve", lambda e: e.scalar_tensor_tensor(out=hf_[:], in0=x_[:], scalar=sb2, in1=g2row[:], op0=ALU.mult, op1=ALU.mult),
                         reads=[bx, bss, b_g2], writes=[bhf])
                    t.op("pool", lambda e: e.tensor_tensor(out=hf_[:], in0=hf_[:], in1=shfrow[:], op=ALU.add), reads=[bhf, b_shf], writes=[bhf])
                normB(0)
                for i in range(NT):
                    hf, b_hf = hfs[i % 2], b_hfs[i % 2]
                    for j in range(4):
                        for r in range(4):
                            c = 4 * j + r
                            t.op("pe", lambda e, c=c, r=r: e.matmul(ps[j][:, r * 128:(r + 1) * 128],
                                                                    hf[:, c * 128:(c + 1) * 128], ident32[:],
                                                                    start=True, stop=True),
                                 reads=[b_hf, b_id], writes=[pb[j]], inc=(r == 3))
                        t.op("dve", lambda e: e.tensor_copy(out=hT32[:, 4 * j:4 * j + 4, :],
                                                            in_=ps[j][:].rearrange("p (r n) -> p r n", n=128)),
                             reads=[pb[j]], writes=[b_hT32])
                        t.op("act", lambda e: e.activation(out=h2T[:, 4 * j:4 * j + 4, i * 128:(i + 1) * 128],
                                                           in_=hT32[:, 4 * j:4 * j + 4, :], func=AF.Copy),
                             reads=[b_hT32], writes=[b_h2T[i]])
                    for c in range(NKC):
                        t.op("pe", lambda e, c=c: e.matmul(ps[4][:, 0:64], hT32[:, c, :], wr32[:, c, :],
                                                           start=(c == 0), stop=(c == NKC - 1)),
                             reads=[b_hT32, b_wr], writes=[pb[4]], inc=(c == NKC - 1))
                    normB(i + 1)
                    self.routing(t, R, ps[4][:, 0:64], pb[4], rbrow, b_rbr, wn[:, i, :], b_wn[i])
            t.barrier()
            yacc = self.sb("yacc", [128, NT, D], F32, es)
            act = self.sb("actT", [128, 4, NT * 128], BF16, es)
            sil = [self.sb(f"sil{i}", [128, 512], BF16, es) for i in range(2)]
            nyb = 0
            for e_ in range(ne + 1):
                sg, su, sd = (3 * e_) % 4, (3 * e_ + 1) % 4, (3 * e_ + 2) % 4
                wg, wu, wd = ring[sg], ring[su], ring[sd]
                nau = 0
                for half in range(2):
                    hbufs = b_h2T[4 * half:4 * half + 4]
                    for fc in range(4):
                        pa, pu = (nau % 2) * 2, (nau % 2) * 2 + 1
                        nau += 1
                        for w_, slot_, bank in ((wg, sg, pa), (wu, su, pu)):
                            for k in range(NKC):
                                t.op("pe", lambda e, k=k, w_=w_, bank=bank: e.matmul(
                                    ps[bank][:], w_[:, k * 512 + fc * 128:k * 512 + (fc + 1) * 128],
                                    h2T[:, k, half * 512:(half + 1) * 512], start=(k == 0), stop=(k == NKC - 1)),
                                    reads=[rb[slot_]] + hbufs, writes=[pb[bank]], inc=(k == NKC - 1))
                        sl, bsl = sil[nau % 2], b_sil[nau % 2]
                        t.op("act", lambda e: e.activation(out=sl[:], in_=ps[pa][:], func=AF.Silu), reads=[pb[pa]], writes=[bsl])
                        t.op("dve", lambda e: e.tensor_tensor(out=act[:, fc, half * 512:(half + 1) * 512], in0=sl[:],
                                                              in1=ps[pu][:], op=ALU.mult),
                             reads=[bsl, pb[pu]], writes=[b_act[half][fc]])
                state["consumed"] += 2
                pump()
                for i in range(NT):
                    for cg in range(4):
                        bank = 4 + nyb % 4
                        nyb += 1
                        for fc in range(4):
                            t.op("pe", lambda e, fc=fc: e.matmul(ps[bank][:], act[:, fc, i * 128:(i + 1) * 128],
                                                                 wd[:, fc * D + cg * 512:fc * D + (cg + 1) * 512],
                                                                 start=(fc == 0), stop=(fc == 3)),
                                 reads=[rb[sd], b_act[i // 4][fc]], writes=[pb[bank]], inc=(fc == 3))
                        ysl = yacc[:, i, cg * 512:(cg + 1) * 512]
                        wcol = wn[:, i, e_:e_ + 1] if e_ < ne else ones1[:, 0:1]
                        wbuf = b_wn[i] if e_ < ne else b_one
                        if e_ == 0:
                            t.op("dve", lambda e: e.tensor_scalar(out=ysl, in0=ps[bank][:], scalar1=wcol, scalar2=None, op0=ALU.mult),
                                 reads=[pb[bank], wbuf], writes=[b_y[i][cg]])
                        else:
                            t.op("dve", lambda e: e.scalar_tensor_tensor(out=ysl, in0=ps[bank][:], scalar=wcol, in1=ysl,
                                                                         op0=ALU.mult, op1=ALU.add),
                                 reads=[pb[bank], wbuf, b_y[i][cg]], writes=[b_y[i][cg]])
                state["consumed"] += 1
                pump()
            t.barrier()
            with contextlib.ExitStack() as es3:
                r0 = ring[0][:].bitcast(F32)
                r1 = ring[1][:].bitcast(F32)
                gfrow = r0[:, 0:D]
                norow = r0[:, D:2 * D]
                xt = [r1[:, 0:D], r1[:, D:2 * D]]
                sq = ring[2][:, 0:D]
                ss = self.sb("ssC", [128, 2 * NT], F32, es3)
                b_gf, b_no = Buf("gfrow"), Buf("norow")
                b_xt = [Buf("xtC0"), Buf("xtC1")]
                b_sq = Buf("sqC")
                b_ss = [Buf(f"ssC{i}") for i in range(NT)]
                t.dma("sp", gfrow[:], self.modrows[5:6, :].partition_broadcast(128), reads=[self.b_modrows[5]], writes=[b_gf])
                t.dma("sp", norow[:], I["norm_out"][0:1, :].partition_broadcast(128), writes=[b_no])
                outs = []

                def load_x1(i):
                    if i < NT:
                        t.dma("sp", xt[i % 2][:], self.x1s[i * 128:(i + 1) * 128, :], reads=[self.b_x1s], writes=[b_xt[i % 2]])
                def frontC(i):
                    if i >= NT:
                        return
                    x_, bx = xt[i % 2], b_xt[i % 2]
                    yb = b_y[i]
                    t.op("pool", lambda e: e.tensor_tensor(out=yacc[:, i, :], in0=yacc[:, i, :], in1=gfrow[:], op=ALU.mult),
                         reads=yb + [b_gf], writes=yb)
                    t.op("dve", lambda e: e.tensor_tensor(out=yacc[:, i, :], in0=yacc[:, i, :], in1=x_[:], op=ALU.add),
                         reads=yb + [bx], writes=yb)
                    t.op("act", lambda e: e.activation(out=sq[:], in_=yacc[:, i, :], func=AF.Square, accum_out=ss[:, 2 * i:2 * i + 1]),
                         reads=yb, writes=[b_sq, b_ss[i]])
                    t.op("act", lambda e: e.activation(out=ss[:, 2 * i + 1:2 * i + 2], in_=ss[:, 2 * i:2 * i + 1], func=AF.Sqrt,
                                                       bias=EPS, scale=1.0 / D), reads=[b_ss[i]], writes=[b_ss[i]])

                def backC(i):
                    yb = b_y[i]
                    t.op("dve", lambda e: e.reciprocal(out=ss[:, 2 * i + 1:2 * i + 2], in_=ss[:, 2 * i + 1:2 * i + 2]),
                         reads=[b_ss[i]], writes=[b_ss[i]])
                    t.op("dve", lambda e: e.scalar_tensor_tensor(out=yacc[:, i, :], in0=yacc[:, i, :], scalar=ss[:, 2 * i + 1:2 * i + 2],
                                                                 in1=norow[:], op0=ALU.mult, op1=ALU.mult),
                         reads=yb + [b_ss[i], b_no], writes=yb)
                    bo = Buf(f"out{i}")
                    t.dma("sp", self.out[i * 128:(i + 1) * 128, :], yacc[:, i, :], reads=yb, writes=[bo])
                    outs.append(bo)
                    load_x1(i + 2)

                load_x1(0)
                load_x1(1)
                frontC(0)
                for i in range(NT):
                    frontC(i + 1)
                    backC(i)
                t.drain("sp", outs)
                t.barrier()

    def phaseA(self, t, ps, pb, ring, rb, ident32, b_id):
        nc = self.nc
        I = self.ins
        SCALE = 128.0 ** -0.5
        PI = float(np.pi)
        psb = [p_[:].bitcast(BF16) for p_ in ps]
        hTs = nc.dram_tensor("hTs", [NSLOT_T, 128, D], BF16, kind="Internal").ap()
        b_hTs = [Buf(f"hTs{i}") for i in range(NSLOT_T)]
        hTt = [ring[3][:, q * D:(q + 1) * D] for q in range(4)]
        b_hTt = [Buf(f"hTt{q}") for q in range(4)]

        def load_hT(tile):
            if 0 <= tile < NSLOT_T:
                t.dma("sp", hTt[tile % 4], hTs[tile], reads=[b_hTs[tile]], writes=[b_hTt[tile % 4]])

        def project(tile, wslot, bslot, ncols, bank):
            for c in range(NKC):
                t.op("pe", lambda e, c=c: e.matmul(ps[bank][:, 0:ncols], hTt[tile % 4][:, c * 128:(c + 1) * 128],
                                                   wslot[:, c * 512:c * 512 + ncols], start=(c == 0), stop=(c == NKC - 1)),
                     reads=[b_hTt[tile % 4], bslot], writes=[pb[bank]], inc=(c == NKC - 1))

        def load_w(slot, col0, ncols=512):
            t.dma("pool", ring[slot][:].rearrange("p (k n) -> p k n", n=512)[:, :, 0:ncols],
                  I["w_in"][:, col0:col0 + ncols].rearrange("(k p) n -> p k n", p=128), writes=[rb[slot]], sem=slot)

        with contextlib.ExitStack() as es:
            OT = self.sb("OT", [128, NKC, NT * 128], BF16, es)
            b_OT = [Buf(f"OT{i}") for i in range(NT)]
            identb = self.sb("identb", [128, 128], BF16, es)
            onesb = self.sb("onesb", [128, 128], BF16, es)
            posi = self.sb("posi", [128, NSLOT_T], mybir.dt.int32, es)
            posf = self.sb("posf", [128, NSLOT_T], F32, es)
            vmask = self.sb("vmask", [128, NSLOT_T], F32, es)
            b_idb, b_ones, b_pos, b_vm = (Buf(n) for n in ("identb", "onesb", "pos", "vmask"))
            t.op("dve", lambda e: e.tensor_copy(out=identb[:], in_=ident32[:]), reads=[b_id], writes=[b_idb])
            t.op("dve", lambda e: e.memset(onesb[:], 1.0), writes=[b_ones])
            t.dma("sp", posi[:], I["pos_i"][:, :], writes=[b_pos])
            t.op("dve", lambda e: e.tensor_copy(out=posf[:], in_=posi[:]), reads=[b_pos], writes=[b_pos])
            t.dma("sp", vmask[:], I["vmask"][:, :], writes=[b_vm])

            with contextlib.ExitStack() as es0:
                p0 = self.phase0_gen(t, ps, pb, ring, rb, es0)
                for _ in range(8):
                    next(p0)
                g1row = self.sb("g1row", [128, D], F32, es0)
                sharow = self.sb("sharow", [128, D], F32, es0)
                xt = [self.sb(f"xtA{i}", [128, D], F32, es0) for i in range(3)]
                hb = [self.sb(f"hbA{i}", [128, D], BF16, es0) for i in range(2)]
                hst = [self.sb(f"hstA{i}", [128, D], BF16, es0) for i in range(2)]
                sq = self.sb("sqA", [128, D], BF16, es0)
                ssA = self.sb("ssA", [128, 4], F32, es0)
                b_g1, b_sha, b_sq = Buf("g1row"), Buf("sharow"), Buf("sq")
                b_xt = [Buf("xtA0"), Buf("xtA1"), Buf("xtA2")]
                b_hb = [Buf("hb0"), Buf("hb1")]
                b_hst = [Buf("hst0"), Buf("hst1")]

                def load_x(tile):
                    if tile < NSLOT_T:
                        t.dma("sp", xt[tile % 3][:], I["xs"][tile * 128:(tile + 1) * 128, :], writes=[b_xt[tile % 3]])
                b_ss = [Buf("ssA0"), Buf("ssA1")]
                t.dma("sp", g1row[:], self.modrows[1:2, :].partition_broadcast(128), reads=[self.b_modrows[1]], writes=[b_g1])
                t.dma("sp", sharow[:], self.modrows[0:1, :].partition_broadcast(128), reads=[self.b_modrows[0]], writes=[b_sha])
                def front(tile):
                    if tile >= NSLOT_T:
                        return
                    k2 = tile % 2
                    x_, bx = xt[tile % 3], b_xt[tile % 3]
                    sa, sb2 = ssA[:, 2 * k2:2 * k2 + 1], ssA[:, 2 * k2 + 1:2 * k2 + 2]
                    t.op("act", lambda e: e.activation(out=sq[:], in_=x_[:], func=AF.Square, accum_out=sa), reads=[bx], writes=[b_sq, b_ss[k2]])
                    t.op("act", lambda e: e.activation(out=sb2, in_=sa, func=AF.Sqrt, bias=EPS, scale=1.0 / D), reads=[b_ss[k2]], writes=[b_ss[k2]])
                    t.op("dve", lambda e: e.reciprocal(out=sb2, in_=sb2), reads=[b_ss[k2]], writes=[b_ss[k2]])
                    t.op("dve", lambda e: e.scalar_tensor_tensor(out=x_[:], in0=x_[:], scalar=sb2, in1=g1row[:], op0=ALU.mult, op1=ALU.mult),
                         reads=[bx, b_ss[k2], b_g1], writes=[bx])
                    t.op("dve", lambda e: e.tensor_tensor(out=hb[k2][:], in0=x_[:], in1=sharow[:], op=ALU.add),
                         reads=[bx, b_sha], writes=[b_hb[k2]])

                def back(tile):
                    k2 = tile % 2
                    for j in range(2):
                        bank = 2 * k2 + j
                        for r in range(8):
                            c = 8 * j + r
                            t.op("pe", lambda e, c=c, r=r: e.transpose(psb[bank][:, r * 128:(r + 1) * 128], hb[k2][:, c * 128:(c + 1) * 128], identb[:]),
                                 reads=[b_hb[k2], b_idb], writes=[pb[bank]], inc=(r == 7))
                    t.op("act", lambda e: e.activation(out=hst[k2][:, 0:1024], in_=psb[2 * k2][:], func=AF.Copy), reads=[pb[2 * k2]], writes=[b_hst[k2]])
                    t.op("dve", lambda e: e.tensor_copy(out=hst[k2][:, 1024:2048], in_=psb[2 * k2 + 1][:]), reads=[pb[2 * k2 + 1]], writes=[b_hst[k2]])
                    load_x(tile + 3)
                    t.dma("sp", hTs[tile], hst[k2][:], reads=[b_hst[k2]], writes=[b_hTs[tile]])

                load_x(0)
                load_x(1)
                load_x(2)
                front(0)
                for tile in range(NSLOT_T):
                    if tile % 2 == 1:
                        next(p0, None)
                    front(tile + 1)
                    back(tile)
                for _ in p0:
                    pass
                t.barrier()

            def sincos(ang, n, sin_out, cos_out, b_ang, b_out):
                kf = self._sc_tmp[:, 0:n]
                ki = self._sc_ki[:, 0:n]
                bt = self._b_sc
                t.op("dve", lambda e: e.tensor_scalar(out=cos_out, in0=ang, scalar1=0.5 * PI, scalar2=None, op0=ALU.add),
                     reads=[b_ang], writes=[b_out])
                for r_, br in ((cos_out, b_out), (ang, b_ang)):
                    t.op("dve", lambda e: e.tensor_scalar(out=kf, in0=r_, scalar1=1.0 / (2 * PI), scalar2=None, op0=ALU.mult), reads=[br], writes=[bt])
                    t.op("dve", lambda e: e.tensor_copy(out=ki, in_=kf), reads=[bt], writes=[bt])
                    t.op("dve", lambda e: e.tensor_copy(out=kf, in_=ki), reads=[bt], writes=[bt])
                    t.op("dve", lambda e: e.scalar_tensor_tensor(out=r_, in0=kf, scalar=-2 * PI, in1=r_, op0=ALU.mult, op1=ALU.add),
                         reads=[bt, br], writes=[br])
                    t.op("dve", lambda e: e.tensor_scalar(out=kf, in0=r_, scalar1=PI, scalar2=-2 * PI, op0=ALU.is_gt, op1=ALU.mult), reads=[br], writes=[bt])
                    t.op("dve", lambda e: e.tensor_tensor(out=r_, in0=r_, in1=kf, op=ALU.add), reads=[bt, br], writes=[br])
                    t.op("dve", lambda e: e.tensor_scalar(out=r_, in0=r_, scalar1=-PI, scalar2=PI, op0=ALU.max, op1=ALU.min), reads=[br], writes=[br])
                t.op("act", lambda e: e.activation(out=cos_out, in_=cos_out, func=AF.Sin), reads=[b_out], writes=[b_out])
                t.op("act", lambda e: e.activation(out=sin_out, in_=ang, func=AF.Sin), reads=[b_ang], writes=[b_out])

            self._sc_ki = self.sb("sc_ki", [128, 1024], mybir.dt.int32, es)
            self._sc_tmp = self.sb("sc_tmp", [128, 1024], F32, es)
            self._b_sc = Buf("sc_tmp")

            NH = 4
            with contextlib.ExitStack() as esm:
                cosA = self.sb("cosA", [128, NSLOT_T, 16], F32, esm)
                sinA = self.sb("sinA", [128, NSLOT_T, 16], F32, esm)
                angA = self.sb("angA", [128, NSLOT_T, 16], F32, esm)
                invfA = self.sb("invfA", [128, 16], F32, esm)
                KT = self.sb("KT", [128, NH, NSLOT_T * 128], BF16, esm)
                V = self.sb("Vm", [128, NSLOT_T, NH * 128], BF16, esm)
                QT = self.sb("QT", [128, NH, NT * 128], BF16, esm)
                TT = self.sb("TT", [16, NH, NT * 128], BF16, esm)
                kbs = [self.sb(f"kbm{i}", [128, NH, 128], BF16, esm) for i in range(2)]
                tm1 = self.sb("tm1", [128, NH, 16], F32, esm)
                tm2 = self.sb("tm2", [128, NH, 16], F32, esm)
                kmT = self.sb("kmT", [128, NH, 16], F32, esm)
                kmTb = self.sb("kmTb", [128, NH, 16], BF16, esm)
                gbias = self.sb("gbias", [128, 4, 16], F32, esm)
                diag = self.sb("diag", [128, 4, 16], F32, esm)
                oh16 = self.sb("oh16", [16, 16, 128], BF16, esm)
                causal = self.sb("causal", [128, 4, 512], BF16, esm)
                gm = self.sb("gmA", [128, NH, 16], F32, esm)
                t8 = self.sb("t8A", [128, NH, 8], F32, esm)
                thr = self.sb("thrA", [128, NH], F32, esm)
                sel = self.sb("selA", [128, NH, 16], F32, esm)
                selb = self.sb("selbA", [128, NH, 16], BF16, esm)
                Pb = [self.sb(f"Pb{i}", [128, 512], BF16, esm) for i in range(3)]
                rl = self._sc_tmp[:, 0:512]
                b_cs, b_ang, b_ifa = Buf("cosinA"), Buf("angA"), Buf("invfA")
                b_KT = [Buf(f"KT{i}") for i in range(NSLOT_T)]
                b_V = [Buf(f"V{i}") for i in range(NSLOT_T)]
                b_QT = [Buf(f"QT{i}") for i in range(NT)]
                b_TT = [Buf(f"TT{i}") for i in range(NT)]
                b_kbs = [Buf("kb0"), Buf("kb1")]
                b_tm1, b_tm2, b_km, b_kmb = (Buf(n) for n in ("tm1", "tm2", "kmT", "kmTb"))
                b_gb, b_dg, b_oh, b_ca, b_gm, b_t8, b_thr, b_sel, b_selb, b_rl = (
                    Buf(n) for n in ("gbias", "diag", "oh16", "causal", "gm", "t8", "thr", "sel", "selb", "rl"))
                b_P = [Buf(f"P{i}") for i in range(3)]
                t.dma("sp", invfA[:], I["invfA"][:, :], writes=[b_ifa])
                t.dma("sp", gbias[:], I["gatebias"][:, :, :], writes=[b_gb])
                t.dma("sp", diag[:], I["diag"][:, :, :], writes=[b_dg])
                t.dma("pool", oh16[:], I["oh16"][:, :, :], writes=[b_oh])
                t.dma("pool", causal[:], I["causal"][:, :, :], writes=[b_ca])
                t.op("dve", lambda e: e.tensor_tensor(out=angA[:], in0=posf[:].unsqueeze(2).broadcast_to([128, NSLOT_T, 16]),
                                                      in1=invfA[:].unsqueeze(1).broadcast_to([128, NSLOT_T, 16]), op=ALU.mult),
                     reads=[b_pos, b_ifa], writes=[b_ang])
                sincos(angA[:].rearrange("p a b -> p (a b)"), NSLOT_T * 16, sinA[:].rearrange("p a b -> p (a b)"),
                       cosA[:].rearrange("p a b -> p (a b)"), b_ang, b_cs)

                def rotaryA(bank, tile, dst, b_dst):
                    src3 = ps[bank][:].rearrange("p (h d) -> p h d", d=128)
                    cb = cosA[:, tile, :].unsqueeze(1).broadcast_to([128, NH, 16])
                    sn = sinA[:, tile, :].unsqueeze(1).broadcast_to([128, NH, 16])
                    x1, x2 = src3[:, :, 0:16], src3[:, :, 16:32]
                    t.op("dve", lambda e: e.tensor_tensor(out=tm1[:], in0=x1, in1=cb, op=ALU.mult), reads=[pb[bank], b_cs], writes=[b_tm1])
                    t.op("dve", lambda e: e.tensor_tensor(out=tm2[:], in0=x2, in1=sn, op=ALU.mult), reads=[pb[bank], b_cs], writes=[b_tm2])
                    t.op("dve", lambda e: e.tensor_tensor(out=dst[:, :, 0:16], in0=tm1[:], in1=tm2[:], op=ALU.subtract),
                         reads=[b_tm1, b_tm2], writes=[b_dst])
                    t.op("dve", lambda e: e.tensor_tensor(out=tm1[:], in0=x2, in1=cb, op=ALU.mult), reads=[pb[bank], b_cs], writes=[b_tm1])
                    t.op("dve", lambda e: e.tensor_tensor(out=tm2[:], in0=x1, in1=sn, op=ALU.mult), reads=[pb[bank], b_cs], writes=[b_tm2])
                    t.op("dve", lambda e: e.tensor_tensor(out=dst[:, :, 16:32], in0=tm1[:], in1=tm2[:], op=ALU.add),
                         reads=[b_tm1, b_tm2], writes=[b_dst])
                    t.op("dve", lambda e: e.tensor_copy(out=dst[:, :, 32:128], in_=src3[:, :, 32:128]), reads=[pb[bank]], writes=[b_dst])

                def transp4(src, b_src, dstT, b_dstT):
                    for h in range(NH):
                        t.op("pe", lambda e, h=h: e.transpose(psb[4][:, h * 128:(h + 1) * 128], src[:, h, :], identb[:]),
                             reads=[b_src, b_idb], writes=[pb[4]], inc=(h == NH - 1))
                    t.op("act", lambda e: e.activation(out=dstT, in_=psb[4][:, 0:NH * 128].rearrange("p (h n) -> p h n", n=128), func=AF.Copy),
                         reads=[pb[4]], writes=[b_dstT])

                def load_moba_w(p):
                    load_w(0, 1024 + 512 * p)
                    load_w(1, 2048 + 512 * p)
                    load_w(2, 512 * p)
                load_moba_w(0)
                for p in range(2):
                    for q in range(3):
                        load_hT(q)

                    def post(tile):
                        k2 = tile % 2
                        rotaryA(k2, tile, kbs[0], b_kbs[0])
                        t.op("act", lambda e: e.activation(out=V[:, tile, :], in_=ps[2 + k2][:], func=AF.Copy), reads=[pb[2 + k2]], writes=[b_V[tile]])
                        transp4(kbs[0], b_kbs[0], KT[:, :, tile * 128:(tile + 1) * 128], b_KT[tile])
                        if tile >= 24:
                            i = tile - 24
                            rotaryA(5 + k2, tile, kbs[1], b_kbs[1])
                            transp4(kbs[1], b_kbs[1], QT[:, :, i * 128:(i + 1) * 128], b_QT[i])
                        if tile % 8 == 7:
                            sg = tile // 8
                            t.op("dve", lambda e: e.tensor_reduce(out=kmT[:, :, 4 * sg:4 * sg + 4],
                                                                  in_=KT[:, :, sg * 1024:(sg + 1) * 1024].rearrange("p h (b k) -> p h b k", k=256),
                                                                  axis=AX.X, op=ALU.add),
                                 reads=b_KT[sg * 8:(sg + 1) * 8], writes=[b_km])

                    for tile in range(NSLOT_T):
                        k2 = tile % 2
                        load_hT(tile + 3)
                        project(tile, ring[0], rb[0], 512, k2)
                        project(tile, ring[1], rb[1], 512, 2 + k2)
                        if tile >= 24:
                            project(tile, ring[2], rb[2], 512, 5 + k2)
                        if tile >= 1:
                            post(tile - 1)
                    post(NSLOT_T - 1)
                    t.op("dve", lambda e: e.tensor_scalar(out=kmTb[:], in0=kmT[:], scalar1=1.0 / 256, scalar2=None, op0=ALU.mult),
                         reads=[b_km], writes=[b_kmb])
                    if p == 0:
                        load_moba_w(1)
                    else:
                        load_w(0, 4096)
                        load_w(1, 5120)
                        load_w(2, 3072)
                    for i in range(NT):
                        r = i // 2
                        for h in range(NH):
                            t.op("pe", lambda e, h=h: e.matmul(ps[4][:, h * 16:(h + 1) * 16], QT[:, h, i * 128:(i + 1) * 128], kmTb[:, h, :],
                                                               start=True, stop=True),
                                 reads=[b_QT[i], b_kmb], writes=[pb[4]], inc=(h == NH - 1))
                        t.op("dve", lambda e: e.tensor_tensor(out=gm[:], in0=ps[4][:, 0:NH * 16].rearrange("p (h n) -> p h n", n=16),
                                                              in1=gbias[:, r, :].unsqueeze(1).broadcast_to([128, NH, 16]), op=ALU.add),
                             reads=[pb[4], b_gb], writes=[b_gm])
                        for h in range(NH):
                            t.op("dve", lambda e, h=h: e.max(out=t8[:, h, :], in_=gm[:, h, :]), reads=[b_gm], writes=[b_t8])
                        t.op("dve", lambda e: e.tensor_scalar(out=thr[:], in0=t8[:, :, 2], scalar1=-1e29, scalar2=None, op0=ALU.max),
                             reads=[b_t8], writes=[b_thr])
                        t.op("dve", lambda e: e.tensor_tensor(out=sel[:], in0=gm[:], in1=thr[:].unsqueeze(2).broadcast_to([128, NH, 16]),
                                                              op=ALU.is_ge), reads=[b_gm, b_thr], writes=[b_sel])
                        t.op("dve", lambda e: e.tensor_tensor(out=sel[:], in0=sel[:], in1=diag[:, r, :].unsqueeze(1).broadcast_to([128, NH, 16]),
                                                              op=ALU.max), reads=[b_sel, b_dg], writes=[b_sel])
                        t.op("dve", lambda e: e.tensor_scalar(out=selb[:], in0=sel[:], scalar1=-1.0, scalar2=30000.0, op0=ALU.add, op1=ALU.mult),
                             reads=[b_sel], writes=[b_selb])
                        for h in range(NH):
                            t.op("pe", lambda e, h=h: e.transpose(psb[4][0:16, 512 + h * 128:512 + (h + 1) * 128], selb[:, h, :], identb[:]),
                                 reads=[b_selb, b_idb], writes=[pb[4]], inc=(h == NH - 1))
                        t.op("act", lambda e: e.activation(out=TT[:, :, i * 128:(i + 1) * 128],
                                                           in_=psb[4][0:16, 512:512 + NH * 128].rearrange("p (h n) -> p h n", n=128), func=AF.Copy),
                             reads=[pb[4]], writes=[b_TT[i]])
                    nS = 0
                    for h in range(NH):
                        for half in range(2):
                            qsl = slice(half * 512, (half + 1) * 512)
                            bq = b_QT[4 * half:4 * half + 4]
                            btt = b_TT[4 * half:4 * half + 4]
                            nkt = 28 + 4 * half
                            ob, lb = (5, 6) if (2 * h + half) % 2 == 0 else (7, 4)
                            def emit_pv(kt, Pt, bP):
                                t.op("pe", lambda e: e.matmul(ps[ob][:], V[:, kt, h * 128:(h + 1) * 128], Pt[:], start=(kt == 0), stop=(kt == nkt - 1)),
                                     reads=[b_V[kt], bP], writes=[pb[ob]], inc=False)
                                t.op("pe", lambda e: e.matmul(ps[lb][:], onesb[:], Pt[:], start=(kt == 0), stop=(kt == nkt - 1)),
                                     reads=[b_ones, bP], writes=[pb[lb]], inc=True)
                            pend = None
                            for kt in range(nkt):
                                sb_ = nS % 4
                                Pt, bP = Pb[nS % 3], b_P[nS % 3]
                                nS += 1
                                dg = kt >= 24 + 4 * half
                                t.op("pe", lambda e: e.matmul(ps[sb_][:], KT[:, h, kt * 128:(kt + 1) * 128], QT[:, h, qsl], start=True, stop=False),
                                     reads=[b_KT[kt]] + bq, writes=[pb[sb_]], inc=False)
                                t.op("pe", lambda e: e.matmul(ps[sb_][:], oh16[:, kt // 2, :], TT[:, h, qsl], start=False, stop=not dg),
                                     reads=[b_oh] + btt, writes=[pb[sb_]], inc=not dg)
                                if dg:
                                    t.op("pe", lambda e: e.matmul(ps[sb_][:], identb[:], causal[:, kt - 24 - 4 * half, :], start=False, stop=True),
                                         reads=[b_idb, b_ca], writes=[pb[sb_]], inc=True)
                                t.op("act", lambda e: e.activation(out=Pt[:], in_=ps[sb_][:], func=AF.Exp, scale=SCALE), reads=[pb[sb_]], writes=[bP])
                                if pend is not None:
                                    emit_pv(*pend)
                                pend = (kt, Pt, bP)
                            emit_pv(*pend)
                            t.op("dve", lambda e: e.reciprocal(out=rl[:], in_=ps[lb][:]), reads=[pb[lb]], writes=[b_rl])
                            t.op("dve", lambda e: e.tensor_tensor(out=OT[:, 4 * p + h, qsl], in0=ps[ob][:], in1=rl[:], op=ALU.mult),
                                 reads=[pb[ob], b_rl], writes=b_OT[4 * half:4 * half + 4])
                t.barrier()

            with contextlib.ExitStack() as esr:
                cosRs = [self.sb(f"cosR{i}", [128, 8, 128], F32, esr) for i in range(2)]
                sinRs = [self.sb(f"sinR{i}", [128, 8, 128], F32, esr) for i in range(2)]
                rtabs = nc.dram_tensor("rtabs", [4, 2, 128, 1024], F32, kind="Internal").ap()
                b_rtabs = [Buf(f"rtabs{i}") for i in range(4)]
                angR = self.sb("angR", [128, 8, 128], F32, esr)
                invfR = self.sb("invfR", [128, 128], F32, esr)
                kB = self.sb("kB", [128, 8, 512], BF16, esr)
                vB = self.sb("vB", [128, 8, 512], BF16, esr)
                qB = self.sb("qB", [128, 8, 512], BF16, esr)
                gB = self.sb("gB", [128, 8, 512], BF16, esr)
                Sf = self.sb("Sf", [128, 2, 512], F32, esr)
                Sb = self.sb("Sb", [128, 2, 512], BF16, esr)
                rtbl = self.sb("rtbl", [128, 4, 128], F32, esr)
                rxi = self.sb("rxi", [128, 4], F32, esr)
                rkz = self.sb("rkz", [128, 4], F32, esr)
                r1 = self.sb("r1", [128, 2, 128], F32, esr)
                r2 = self.sb("r2", [128, 2, 128], F32, esr)
                r3 = self.sb("r3", [128, 2, 128], F32, esr)
                r4 = self.sb("r4", [128, 2, 128], F32, esr)
                kqTs = [self.sb(f"kqT{i}", [128, 8, 128], BF16, esr) for i in range(2)]
                ATps = [self.sb(f"ATp{i}", [128, 2, 128], BF16, esr) for i in range(2)]
                osb = self.sb("osb", [128, 512], F32, esr)
                osq = self.sb("osq", [128, 512], BF16, esr)
                onb = self.sb("onb", [128, 512], BF16, esr)
                Kz = self.sb("Kz", [128, 512], BF16, esr)
                st = self.sb("stR", [128, 5, 2], F32, esr)
                b_csrs, b_angr, b_ifr = [Buf("cosinR0"), Buf("cosinR1")], Buf("angR"), Buf("invfR")
                b_kB = [Buf(f"kB{i}") for i in range(8)]
                b_vB = [Buf(f"vB{i}") for i in range(8)]
                b_qB = [Buf(f"qB{i}") for i in range(8)]
                b_gB = [Buf(f"gB{i}") for i in range(8)]
                b_Sf = [Buf("Sf0"), Buf("Sf1")]
                b_Sb = [Buf("Sb0"), Buf("Sb1")]
                b_rc = Buf("retconst")
                b_r = [Buf(f"r{i}") for i in range(4)]
                b_osb, b_osq, b_onb, b_Kz, b_st = (Buf(n) for n in ("osb", "osq", "onb", "Kz", "stR"))
                b_kqTs = [Buf("kqT0"), Buf("kqT1")]
                b_ATps = [Buf("ATp0"), Buf("ATp1")]
                t.dma("sp", invfR[:], I["invfR"][:, :], writes=[b_ifr])
                t.dma("sp", rtbl[:], I["rettbl"][:, :, :], writes=[b_rc])
                t.dma("sp", rxi[:], I["retxi"][:, :], writes=[b_rc])
                t.dma("sp", rkz[:], I["retkz"][:, :], writes=[b_rc])
                gam = [1.0 - 2.0 ** (-5 - h) for h in range(4)]

                def make_tables(rp, sg):
                    if sg > 3:
                        return
                    cR, sR, bcs = cosRs[sg % 2], sinRs[sg % 2], b_csrs[sg % 2]
                    cf, sf = cR[:].rearrange("p a b -> p (a b)"), sR[:].rearrange("p a b -> p (a b)")
                    if rp == 0:
                        t.op("dve", lambda e: e.tensor_tensor(out=angR[:], in0=posf[:, sg * 8:(sg + 1) * 8].unsqueeze(2).broadcast_to([128, 8, 128]),
                                                              in1=invfR[:].unsqueeze(1).broadcast_to([128, 8, 128]), op=ALU.mult),
                             reads=[b_pos, b_ifr], writes=[b_angr])
                        sincos(angR[:].rearrange("p a b -> p (a b)"), 1024, sf, cf, b_angr, bcs)
                        t.dma("sp", rtabs[sg, 0], cf, reads=[bcs], writes=[b_rtabs[sg]])
                        t.dma("sp", rtabs[sg, 1], sf, reads=[bcs], writes=[b_rtabs[sg]])
                    else:
                        t.dma("sp", cf, rtabs[sg, 0], reads=[b_rtabs[sg]], writes=[bcs])
                        t.dma("sp", sf, rtabs[sg, 1], reads=[b_rtabs[sg]], writes=[bcs])

                def rotaryR(bank, tile_in_group, dst, b_dst, sg):
                    cosR, sinR, b_csr = cosRs[sg % 2], sinRs[sg % 2], b_csrs[sg % 2]
                    src = ps[bank][:].rearrange("p (h s d) -> p h s d", s=2, d=128)
                    d4 = dst.rearrange("p (h s d) -> p h s d", s=2, d=128)
                    cb = cosR[:, tile_in_group, :].unsqueeze(1).broadcast_to([128, 2, 128])
                    sn = sinR[:, tile_in_group, :].unsqueeze(1).broadcast_to([128, 2, 128])
                    x1, x2 = src[:, :, 0, :], src[:, :, 1, :]
                    t.op("dve", lambda e: e.tensor_tensor(out=r1[:], in0=x1, in1=cb, op=ALU.mult), reads=[pb[bank], b_csr], writes=[b_r[0]])
                    t.op("dve", lambda e: e.tensor_tensor(out=r2[:], in0=x2, in1=sn, op=ALU.mult), reads=[pb[bank], b_csr], writes=[b_r[1]])
                    t.op("dve", lambda e: e.tensor_tensor(out=r3[:], in0=x2, in1=cb, op=ALU.mult), reads=[pb[bank], b_csr], writes=[b_r[2]])
                    t.op("dve", lambda e: e.tensor_tensor(out=r4[:], in0=x1, in1=sn, op=ALU.mult), reads=[pb[bank], b_csr], writes=[b_r[3]])
                    t.op("pool", lambda e: e.tensor_tensor(out=d4[:, :, 0, :], in0=r1[:], in1=r2[:], op=ALU.subtract),
                         reads=[b_r[0], b_r[1]], writes=[b_dst])
                    t.op("pool", lambda e: e.tensor_tensor(out=d4[:, :, 1, :], in0=r3[:], in1=r4[:], op=ALU.add),
                         reads=[b_r[2], b_r[3]], writes=[b_dst])

                for rp in range(2):
                    t.op("dve", lambda e: e.memset(Sf[:], 0.0), writes=b_Sf)
                    t.op("dve", lambda e: e.memset(Sb[:], 0.0), writes=b_Sb)
                    for q in range(3):
                        load_hT(q)
                    make_tables(rp, 0)
                    for sg in range(4):

                        def postkv(i, tile):
                            k2 = tile % 2
                            rotaryR(k2, i, kB[:, i, :], b_kB[i], sg)
                            t.op("act", lambda e: e.activation(out=vB[:, i, :], in_=ps[2 + k2][:], func=AF.Copy, scale=vmask[:, tile:tile + 1]),
                                 reads=[pb[2 + k2], b_vm], writes=[b_vB[i]])

                        def postqg(i, tile):
                            k2 = tile % 2
                            rotaryR(k2, i, qB[:, i, :], b_qB[i], sg)
                            t.op("act", lambda e: e.activation(out=gB[:, i, :], in_=ps[2 + k2][:], func=AF.Silu), reads=[pb[2 + k2]], writes=[b_gB[i]])

                        for i in range(8):
                            tile = sg * 8 + i
                            k2 = tile % 2
                            load_hT(tile + 3 if not (sg == 3 and i >= 5) else -1)
                            project(tile, ring[0], rb[0], 512, k2)
                            project(tile, ring[1], rb[1], 512, 2 + k2)
                            if i >= 1:
                                postkv(i - 1, tile - 1)
                            if i == 3:
                                make_tables(rp, sg + 1)
                        postkv(7, sg * 8 + 7)
                        if sg == 3:
                            load_w(0, 6144 + 512 * rp)
                            for q in range(3):
                                load_hT(24 + q)
                            for i in range(8):
                                tile = 24 + i
                                k2 = tile % 2
                                load_hT(tile + 3)
                                project(tile, ring[2], rb[2], 512, k2)
                                project(tile, ring[0], rb[0], 512, 2 + k2)
                                if i >= 1:
                                    postqg(i - 1, tile - 1)
                            postqg(7, 31)
                            if rp == 0:
                                load_w(0, 4096 + 512)
                                load_w(1, 5120 + 512)
                                load_w(2, 3072 + 512)
                            else:
                                for cg in range(4):
                                    t.dma("pool", ring[cg][:].rearrange("p (k n) -> p k n", n=512),
                                          I["w_out"][:, cg * 512:(cg + 1) * 512].rearrange("(k p) n -> p k n", p=128),
                                          writes=[rb[cg]] + (b_hTt if cg == 3 else []), sem=cg)
                        h0 = 2 * rp

                        def partA(i):
                            for hh in range(2):
                                for q_, (src_, bsrc) in enumerate(((kB, b_kB[i]), (qB, b_qB[i]))):
                                    for dc in range(2):
                                        col = (4 * hh + 2 * q_ + dc) * 128
                                        t.op("pe", lambda e, col=col, dc=dc, src_=src_, hh=hh: e.transpose(
                                            psb[4][:, col:col + 128], src_[:, i, hh * 256 + dc * 128:hh * 256 + (dc + 1) * 128], identb[:]),
                                            reads=[bsrc, b_idb], writes=[pb[4]], inc=(hh == 1 and q_ == 1 and dc == 1))
                            t.op("act", lambda e: e.activation(out=kqTs[i % 2][:], in_=psb[4][:].rearrange("p (a n) -> p a n", n=128), func=AF.Copy),
                                 reads=[pb[4]], writes=[b_kqTs[i % 2]])
                            for hh in range(2):
                                for dc in range(2):
                                    t.op("pe", lambda e, dc=dc, hh=hh: e.matmul(ps[5][:, hh * 128:(hh + 1) * 128], kqTs[i % 2][:, 4 * hh + dc, :], kqTs[i % 2][:, 4 * hh + 2 + dc, :],
                                                                                start=(dc == 0), stop=(dc == 1)),
                                         reads=[b_kqTs[i % 2]], writes=[pb[5]], inc=(hh == 1 and dc == 1))
                            t.op("dve", lambda e: e.tensor_tensor(out=ATps[i % 2][:], in0=ps[5][:, 0:256].rearrange("p (a n) -> p a n", n=128),
                                                                  in1=rtbl[:, h0:h0 + 2, :], op=ALU.mult),
                                 reads=[pb[5], b_rc], writes=[b_ATps[i % 2]])

                        def partB(i):
                            for hh in range(2):
                                hs = slice(hh * 256, (hh + 1) * 256)
                                t.op("pe", lambda e: e.matmul(ps[6][:, hs], ATps[i % 2][:, hh, :], vB[:, i, hs], start=True, stop=False),
                                     reads=[b_ATps[i % 2], b_vB[i]], writes=[pb[6]], inc=False)
                                for dc in range(2):
                                    t.op("pe", lambda e, dc=dc: e.matmul(ps[6][:, hs], kqTs[i % 2][:, 4 * hh + 2 + dc, :], Sb[:, hh, dc * 256:(dc + 1) * 256],
                                                                         start=False, stop=(dc == 1)),
                                         reads=[b_kqTs[i % 2], b_Sb[hh]], writes=[pb[6]], inc=(dc == 1))
                            for hh in range(2):
                                hs = slice(hh * 256, (hh + 1) * 256)
                                t.op("act", lambda e: e.activation(out=osb[:, hs], in_=ps[6][:, hs], func=AF.Copy, scale=rxi[:, h0 + hh:h0 + hh + 1],
                                                                   accum_out=st[:, 0, hh:hh + 1]), reads=[pb[6], b_rc], writes=[b_osb, b_st])
                            for hh in range(2):
                                hs = slice(hh * 256, (hh + 1) * 256)
                                t.op("act", lambda e: e.activation(out=osq[:, hs], in_=osb[:, hs], func=AF.Square, accum_out=st[:, 1, hh:hh + 1]),
                                     reads=[b_osb], writes=[b_osq, b_st])
                            t.op("dve", lambda e: e.tensor_scalar(out=st[:, 2, :], in0=st[:, 0, :], scalar1=1.0 / 256, scalar2=None, op0=ALU.mult),
                                 reads=[b_st], writes=[b_st])
                            t.op("dve", lambda e: e.tensor_tensor(out=st[:, 3, :], in0=st[:, 2, :], in1=st[:, 2, :], op=ALU.mult),
                                 reads=[b_st], writes=[b_st])
                            t.op("dve", lambda e: e.scalar_tensor_tensor(out=st[:, 3, :], in0=st[:, 1, :], scalar=1.0 / 256, in1=st[:, 3, :],
                                                                         op0=ALU.mult, op1=ALU.subtract), reads=[b_st], writes=[b_st])
                            t.op("act", lambda e: e.activation(out=st[:, 4, :], in_=st[:, 3, :], func=AF.Sqrt, bias=EPS, scale=1.0),
                                 reads=[b_st], writes=[b_st])
                            t.op("dve", lambda e: e.reciprocal(out=st[:, 4, :], in_=st[:, 4, :]), reads=[b_st], writes=[b_st])
                            for hh in range(2):
                                hs = slice(hh * 256, (hh + 1) * 256)
                                t.op("dve", lambda e: e.tensor_scalar(out=osb[:, hs], in0=osb[:, hs], scalar1=st[:, 2, hh:hh + 1], scalar2=st[:, 4, hh:hh + 1],
                                                                      op0=ALU.subtract, op1=ALU.mult), reads=[b_osb, b_st], writes=[b_osb])
                            t.op("pool", lambda e: e.tensor_tensor(out=onb[:], in0=osb[:], in1=gB[:, i, :], op=ALU.mult),
                                 reads=[b_osb, b_gB[i]], writes=[b_onb])
                            for a_ in range(4):
                                t.op("pe", lambda e, a_=a_: e.transpose(psb[1][:, a_ * 128:(a_ + 1) * 128], onb[:, a_ * 128:(a_ + 1) * 128], identb[:]),
                                     reads=[b_onb, b_idb], writes=[pb[1]], inc=(a_ == 3))
                            t.op("act", lambda e: e.activation(out=OT[:, 8 + 2 * h0:8 + 2 * h0 + 4, i * 128:(i + 1) * 128],
                                                               in_=psb[1][:, 0:512].rearrange("p (a n) -> p a n", n=128), func=AF.Copy),
                                 reads=[pb[1]], writes=[b_OT[i]])

                        def upd(i):
                            t.op("pool", lambda e: e.tensor_tensor(out=Kz[:].rearrange("p (a n) -> p a n", n=256),
                                                                   in0=kB[:, i, :].rearrange("p (a n) -> p a n", n=256),
                                                                   in1=rkz[:, h0:h0 + 2].unsqueeze(2).broadcast_to([128, 2, 256]), op=ALU.mult),
                                 reads=[b_kB[i], b_rc], writes=[b_Kz])
                            for hh in range(2):
                                hs = slice(hh * 256, (hh + 1) * 256)
                                sbank = 7 if hh == 0 else 3
                                for dc in range(2):
                                    t.op("pe", lambda e, dc=dc: e.matmul(ps[sbank][:, dc * 256:(dc + 1) * 256], Kz[:, hh * 256 + dc * 128:hh * 256 + (dc + 1) * 128],
                                                                         vB[:, i, hs], start=True, stop=True),
                                         reads=[b_Kz, b_vB[i]], writes=[pb[sbank]], inc=(dc == 1))
                            for hh in range(2):
                                sbank = 7 if hh == 0 else 3
                                t.op("dve", lambda e: e.scalar_tensor_tensor(out=Sf[:, hh, :], in0=Sf[:, hh, :], scalar=float(gam[h0 + hh] ** 128), in1=ps[sbank][:],
                                                                             op0=ALU.mult, op1=ALU.add), reads=[pb[sbank], b_Sf[hh]], writes=[b_Sf[hh]])
                            for hh in range(2):
                                t.op("act", lambda e: e.activation(out=Sb[:, hh, :], in_=Sf[:, hh, :], func=AF.Copy), reads=[b_Sf[hh]], writes=[b_Sb[hh]])

                        if sg == 3:
                            partA(0)
                            for i in range(8):
                                if i + 1 < 8:
                                    partA(i + 1)
                                partB(i)
                                upd(i)
                        else:
                            for i in range(8):
                                upd(i)
                t.barrier()

            with contextlib.ExitStack() as eso:
                garow = self.sb("garow", [128, D], F32, eso)
                xo = [self.sb(f"xo{i}", [128, D], F32, eso) for i in range(2)]
                tmpo = [self.sb(f"tmpo{i}", [128, 512], F32, eso) for i in range(2)]
                b_ga = Buf("garow")
                b_xo = [Buf("xo0"), Buf("xo1")]
                b_tmpo = [Buf("tmpo0"), Buf("tmpo1")]
                t.dma("sp", garow[:], self.modrows[2:3, :].partition_broadcast(128), reads=[self.b_modrows[2]], writes=[b_ga])
                nb = 0
                def load_xo(i):
                    if i < NT:
                        t.dma("sp", xo[i % 2][:], I["xs"][(24 + i) * 128:(25 + i) * 128, :], writes=[b_xo[i % 2]])
                load_xo(0)
                load_xo(1)
                for i in range(NT):
                    x_, bx = xo[i % 2], b_xo[i % 2]
                    for cg in range(4):
                        bank = nb % 4
                        tm, btm = tmpo[nb % 2], b_tmpo[nb % 2]
                        nb += 1
                        for c in range(NKC):
                            t.op("pe", lambda e, c=c: e.matmul(ps[bank][:], OT[:, c, i * 128:(i + 1) * 128], ring[cg][:, c * 512:(c + 1) * 512],
                                                               start=(c == 0), stop=(c == NKC - 1)),
                                 reads=[b_OT[i], rb[cg]], writes=[pb[bank]], inc=(c == NKC - 1))
                        t.op("dve", lambda e: e.tensor_tensor(out=tm[:], in0=ps[bank][:], in1=garow[:, cg * 512:(cg + 1) * 512], op=ALU.mult),
                             reads=[pb[bank], b_ga], writes=[btm])
                        t.op("pool", lambda e: e.tensor_tensor(out=x_[:, cg * 512:(cg + 1) * 512], in0=x_[:, cg * 512:(cg + 1) * 512], in1=tm[:], op=ALU.add),
                             reads=[btm, bx], writes=[bx])
                    t.dma("sp", self.x1s[i * 128:(i + 1) * 128, :], x_[:], reads=[bx], writes=[self.b_x1s])
                    load_xo(i + 2)
                t.barrier()

    def build(self):
        nc = self.nc
        I = self.ins
        ne = self.ne
        self.din("c_col", [128, NKC])
        self.din("w_ada", [D, 6 * D])
        self.din("b_ada", [1, 6 * D])
        self.din("norm_mix", [1, D])
        self.din("norm_ffn", [1, D])
        self.din("norm_out", [1, D])
        self.din("w_router", [D, 64])
        self.din("router_bias", [1, 64])
        self.din("w_gate", [ne, D, 512])
        self.din("w_up", [ne, D, 512])
        self.din("w_down", [ne, 512, D])
        self.din("w_sh_gate", [D, 512])
        self.din("w_sh_up", [D, 512])
        self.din("w_sh_down", [512, D])
        self.din("ident", [128, 128])
        if self.mode in ("testA", "full"):
            self.din("xs", [NSLOT_T * 128, D])
            self.din("pos_i", [128, NSLOT_T], mybir.dt.int32)
            self.din("vmask", [128, NSLOT_T])
            self.din("w_in", [D, 7168])
            self.din("w_out", [D, D])
            self.din("invfA", [128, 16])
            self.din("invfR", [128, 128])
            self.din("gatebias", [128, 4, 16])
            self.din("diag", [128, 4, 16])
            self.din("oh16", [16, 16, 128])
            self.din("causal", [128, 4, 512])
            self.din("rettbl", [128, 4, 128])
            self.din("retxi", [128, 4])
            self.din("retkz", [128, 4])
        if self.mode == "testB":
            self.x1s = self.din("x1s", [NT * 128, D])
        elif self.mode == "testA":
            self.x1s = nc.dram_tensor("x1s", [NT * 128, D], F32, kind="ExternalOutput").ap()
        else:
            self.x1s = nc.dram_tensor("x1s", [NT * 128, D], F32, kind="Internal").ap()
        self.b_x1s = Buf("x1s")
        self.modrows = nc.dram_tensor("modrows", [8, D], F32, kind="Internal").ap()
        self.b_modrows = [Buf(f"modrows{v}") for v in range(6)]
        if self.mode != "testA":
            self.out = nc.dram_tensor("out", [NT * 128, D], F32, kind="ExternalOutput").ap()
        es = self.es
        ps = [es.enter_context(nc.psum_tensor(f"ps{i}", [128, 512], F32)) for i in range(8)]
        pb = [Buf(f"ps{i}") for i in range(8)]
        ring = [self.sb(f"ring{i}", [128, NKC * 512], BF16) for i in range(4)]
        rb = [Buf(f"ring{i}") for i in range(4)]
        ident32 = self.sb("ident32", [128, 128], F32)
        b_id = Buf("ident")
        t = Trk(nc)
        self.t = t
        t.dma("sp", ident32[:], I["ident"][:, :], writes=[b_id])
        if self.mode not in ("testA", "full"):
            with contextlib.ExitStack() as es0:
                for _ in self.phase0_gen(t, ps, pb, ring, rb, es0):
                    pass
                t.barrier()
        if self.mode == "test0":
            bo = Buf("o")
            t.dma("sp", self.out[0:6, :], self.modrows[0:6, :], reads=self.b_modrows, writes=[bo])
            t.drain("sp", [bo])
        if self.mode in ("testA", "full"):
            self.phaseA(t, ps, pb, ring, rb, ident32, b_id)
        if self.mode in ("testB", "full"):
            self.phaseB(t, ps, pb, ring, rb, ident32, b_id)
        t.close()
        es.close()
        return nc


def _common_inputs(inputs, b, ne=NE):
    f = np.ascontiguousarray
    return {
        "c_col": f(inputs["c"][b].reshape(NKC, 128).T),
        "w_ada": inputs["w_ada"][0],
        "b_ada": inputs["b_ada"][0:1],
        "norm_mix": inputs["norm_mix"][0:1],
        "norm_ffn": inputs["norm_ffn"][0:1],
        "norm_out": inputs["norm_out"].reshape(1, D),
        "w_router": inputs["w_router"][0],
        "router_bias": inputs["router_bias"][0:1],
        "w_gate": inputs["w_gate"][0][:ne],
        "w_up": inputs["w_up"][0][:ne],
        "w_down": inputs["w_down"][0][:ne],
        "w_sh_gate": inputs["w_sh_gate"][0],
        "w_sh_up": inputs["w_sh_up"][0],
        "w_sh_down": inputs["w_sh_down"][0],
        "ident": np.eye(128, dtype=np.float32),
    }


def _phaseA_inputs(inputs, b, j):
    f32 = np.float32
    own_end = 1024 * (j + 1)
    start = own_end - 4096
    lo = max(start, 0)
    xs = np.zeros((4096, D), f32)
    xs[lo - start:] = inputs["x"][b, lo:own_end]
    pos = np.zeros((4096,), np.int32)
    pos[lo - start:] = inputs["positions"][b, lo:own_end]
    tile_valid = ((np.arange(NSLOT_T) * 128 + start) >= 0).astype(f32)
    r = np.arange(4)[:, None]
    kb = np.arange(16)[None, :]
    gatebias = np.where((kb < 12 + r) & (kb >= 12 - 4 * j), 0.0, -1e30).astype(f32)
    diag = (kb >= 12 + r).astype(f32)
    oh16 = (np.arange(16)[:, None, None] == np.arange(16)[None, :, None]) * np.ones((1, 1, 128))
    pp = np.arange(128)[:, None, None]
    dd = np.arange(4)[None, :, None]
    cc = np.arange(512)[None, None, :]
    causal = np.where(128 * dd + pp <= cc, 0.0, -30000.0)
    invfA = (np.float32(500000.0) ** (-(np.arange(16, dtype=f32) * f32(2.0) / f32(32.0)))).astype(f32)
    invfR = (np.float32(10000.0) ** (-np.linspace(0.0, 1.0, 128, dtype=f32))).astype(f32)
    gam = 1.0 - 2.0 ** (-5.0 - np.arange(4))
    m = np.arange(128)
    rettbl = (gam[None, :, None] ** (-(m[:, None, None] + 1.0))) / 16.0 * (m[None, None, :] >= m[:, None, None])
    retxi = gam[None, :] ** (m[:, None] + 1.0)
    retkz = gam[None, :] ** (127.0 - m[:, None]) / 16.0
    c = np.ascontiguousarray
    return {
        "xs": xs,
        "pos_i": c(pos.reshape(NSLOT_T, 128).T),
        "vmask": c(np.broadcast_to(tile_valid[None, :], (128, NSLOT_T))).astype(f32),
        "w_in": inputs["w_in"][0],
        "w_out": inputs["w_out"][0],
        "invfA": c(np.broadcast_to(invfA[None, :], (128, 16))).astype(f32),
        "invfR": c(np.broadcast_to(invfR[None, :], (128, 128))).astype(f32),
        "gatebias": c(np.broadcast_to(gatebias[None], (128, 4, 16))).astype(f32),
        "diag": c(np.broadcast_to(diag[None], (128, 4, 16))).astype(f32),
        "oh16": c(oh16).astype(f32),
        "causal": c(causal).astype(f32),
        "rettbl": c(rettbl).astype(f32),
        "retxi": c(retxi).astype(f32),
        "retkz": c(retkz).astype(f32),
    }


_PROG = {}


def kernel(**inputs):
    inputs = {k: np.asarray(v) for k, v in inputs.items()}
    if "full" not in _PROG:
        _PROG["full"] = Prog(mode="full").build()
    nc = _PROG["full"]
    in_maps = []
    for core in range(8):
        b, j = core // 4, core % 4
        im = _common_inputs(inputs, b)
        im.update(_phaseA_inputs(inputs, b, j))
        in_maps.append(im)
    res = run_bass_kernel_spmd(nc, in_maps, core_ids=list(range(8)))
    out = np.empty((2, 4096, D), np.float32)
    for core in range(8):
        b, j = core // 4, core % 4
        out[b, 1024 * j:1024 * (j + 1)] = res.results[core]["out"]
    return out
```
